# Optimizing a Trainium2 kernel written in Bass

```python
import math
import jax, jax.numpy as jnp
from jax import lax
import numpy as np


D_MODEL = 1024
BATCH = 8
SEQ = 4096
DEPTH = 1

M_HEADS = 4
M_DV = 128
M_DQK = 64
M_CHUNK = 64
M_CONV = 4
A_HEADS = 8
A_DH = 64
IDX_HEADS = 8
IDX_DIM = 64
TOPK_MAX = 256
Q_BLOCK = 128
REL_BUCKETS = 32
REL_MAX_DIST = 128
D_FF = 2816
FFN_CONV = 3
EPS = 1e-6

M_WIDTH = M_HEADS * M_DV
A_WIDTH = A_HEADS * A_DH
D_MIX = M_WIDTH + A_WIDTH
IN_SIZES = (M_HEADS * M_DQK, M_HEADS * M_DQK, M_WIDTH, M_WIDTH, M_HEADS, M_HEADS,
            A_WIDTH, A_WIDTH, A_WIDTH, IDX_HEADS * IDX_DIM, IDX_DIM, IDX_HEADS)
D_IN = 2 * M_HEADS * M_DQK + 2 * M_WIDTH + 2 * M_HEADS + 3 * A_WIDTH + IDX_HEADS * IDX_DIM + IDX_DIM + IDX_HEADS

kernel_name = 'hymba_mlstm_dsa_convffn'


def rms_norm(x, g):
    xf = x.astype(jnp.float32)
    y = xf * lax.rsqrt(jnp.mean(xf * xf, axis=-1, keepdims=True) + EPS)
    return (y * g.astype(jnp.float32)).astype(x.dtype)


def causal_dwconv(x, w, b):
    width = w.shape[0]
    s = x.shape[1]
    xp = jnp.pad(x, ((0, 0), (width - 1, 0), (0, 0)))
    y = b + xp[:, 0:s] * w[0]
    for j in range(1, width):
        y = y + xp[:, j:j + s] * w[j]
    return y


def split_cols(z, sizes):
    out, off = [], 0
    for sz in sizes:
        out.append(z[..., off:off + sz])
        off += sz
    return out


def rel_bucket(dist):
    max_exact = REL_BUCKETS // 2
    d = jnp.maximum(dist, 0)
    large = max_exact + (jnp.log(jnp.maximum(d, 1).astype(jnp.float32) / max_exact)
                         / math.log(REL_MAX_DIST / max_exact) * (REL_BUCKETS - max_exact)).astype(jnp.int32)
    large = jnp.minimum(large, REL_BUCKETS - 1)
    return jnp.where(d < max_exact, d, large)


def mlstm_chunkwise(q, k, v, i_pre, f_pre):
    bsz, s, nh, dqk = q.shape
    dv = v.shape[-1]
    L = M_CHUNK
    nc = s // L
    f32 = jnp.float32

    def heads_chunks(t):
        return t.astype(f32).reshape(bsz, nc, L, nh, t.shape[-1]).transpose(0, 3, 1, 2, 4)

    q = heads_chunks(q) * (dqk ** -0.5)
    k = heads_chunks(k)
    v = heads_chunks(v)
    ig = i_pre.astype(f32).reshape(bsz, nc, L, nh).transpose(0, 3, 1, 2)
    logf = jax.nn.log_sigmoid(f_pre.astype(f32)).reshape(bsz, nc, L, nh).transpose(0, 3, 1, 2)

    b = jnp.cumsum(logf, axis=-1)
    g = b[..., -1]
    a = g[..., None] - b + ig
    m_loc = jnp.max(a, axis=-1)
    wgt = jnp.exp(a - m_loc[..., None])
    kv_c = jnp.einsum('bhcl,bhcld,bhcle->bhcde', wgt, k, v)
    ks_c = jnp.einsum('bhcl,bhcld->bhcd', wgt, k)

    def step(carry, xs):
        c_st, n_st, m_st = carry
        kv, ks, gc, ml = xs
        m_new = jnp.maximum(gc + m_st, ml)
        decay = jnp.exp(gc + m_st - m_new)
        scale = jnp.exp(ml - m_new)
        c_new = decay[..., None, None] * c_st + scale[..., None, None] * kv
        n_new = decay[..., None] * n_st + scale[..., None] * ks
        return (c_new, n_new, m_new), (c_st, n_st, m_st)

    init = (jnp.zeros((bsz, nh, dqk, dv), f32), jnp.zeros((bsz, nh, dqk), f32), jnp.zeros((bsz, nh), f32))
    xs = (jnp.moveaxis(kv_c, 2, 0), jnp.moveaxis(ks_c, 2, 0), jnp.moveaxis(g, 2, 0), jnp.moveaxis(m_loc, 2, 0))
    _, (c_prev, n_prev, m_prev) = lax.scan(step, init, xs)
    c_prev = jnp.moveaxis(c_prev, 0, 2)
    n_prev = jnp.moveaxis(n_prev, 0, 2)
    m_prev = jnp.moveaxis(m_prev, 0, 2)

    tril = jnp.tril(jnp.ones((L, L), dtype=bool))
    dmat = b[..., :, None] - b[..., None, :] + ig[..., None, :]
    dmat = jnp.where(tril, dmat, -jnp.inf)
    inter_log = b + m_prev[..., None]
    m_t = jnp.maximum(inter_log, jnp.max(dmat, axis=-1))
    sc = jnp.einsum('bhcld,bhcsd->bhcls', q, k) * jnp.exp(dmat - m_t[..., None])
    inter_w = jnp.exp(inter_log - m_t)
    num = inter_w[..., None] * jnp.einsum('bhcld,bhcde->bhcle', q, c_prev) + jnp.einsum('bhcls,bhcse->bhcle', sc, v)
    den = inter_w * jnp.einsum('bhcld,bhcd->bhcl', q, n_prev) + jnp.sum(sc, axis=-1)
    h = num / jnp.maximum(jnp.abs(den), jnp.exp(-m_t))[..., None]
    return h.transpose(0, 2, 3, 1, 4).reshape(bsz, s, nh, dv)


def dsa_attention(q_att, k_att, v_att, q_idx, k_idx, w_idx, rel_bias):
    bsz, s = q_att.shape[0], q_att.shape[1]
    topk = min(TOPK_MAX, s // 4)
    nqb = s // Q_BLOCK
    key_pos = jnp.arange(s, dtype=jnp.int32)

    def to_blocks(t):
        return t.reshape(bsz, nqb, Q_BLOCK, *t.shape[2:]).swapaxes(0, 1)

    def one_block(args):
        qa, qi, wi, st = args
        qpos = st + jnp.arange(Q_BLOCK, dtype=jnp.int32)
        isc = jnp.einsum('bqhd,bsd->bqhs', qi, k_idx).astype(jnp.float32) * (IDX_DIM ** -0.5)
        score = jnp.einsum('bqhs,bqh->bqs', jax.nn.relu(isc), wi.astype(jnp.float32))
        score = jnp.where(key_pos[None, None, :] <= qpos[None, :, None], score, -jnp.inf)
        _, idx = lax.top_k(score, topk)
        valid = idx <= qpos[None, :, None]
        ks = jax.vmap(lambda kb, ib: kb[ib])(k_att, idx)
        vs = jax.vmap(lambda vb, ib: vb[ib])(v_att, idx)
        bias = rel_bias[rel_bucket(qpos[None, :, None] - idx)]
        logits = jnp.einsum('bqhd,bqkhd->bqhk', qa, ks).astype(jnp.float32) * (A_DH ** -0.5)
        logits = logits + bias.astype(jnp.float32).transpose(0, 1, 3, 2)
        logits = jnp.where(valid[:, :, None, :], logits, -jnp.inf)
        p = jax.nn.softmax(logits, axis=-1)
        return jnp.einsum('bqhk,bqkhd->bqhd', p.astype(vs.dtype), vs)

    starts = jnp.arange(nqb, dtype=jnp.int32) * Q_BLOCK
    out = lax.map(one_block, (to_blocks(q_att), to_blocks(q_idx), to_blocks(w_idx), starts))
    return out.swapaxes(0, 1).reshape(bsz, s, A_HEADS, A_DH)


def setup_inputs(seed: int = 0) -> dict:
    key = jax.random.key(seed)
    ks = jax.random.split(key, 20)
    f32 = jnp.float32
    nrm = lambda k, shape, scale: jax.random.normal(k, shape, f32) * scale
    return {
        'x': nrm(ks[0], (BATCH, SEQ, D_MODEL), 1.0),
        'norm_mix': 1.0 + nrm(ks[1], (DEPTH, D_MODEL), 0.01),
        'w_in': nrm(ks[2], (DEPTH, D_MODEL, D_IN), D_MODEL ** -0.5),
        'mlstm_conv_w': nrm(ks[3], (DEPTH, M_CONV, 2 * M_HEADS * M_DQK), M_CONV ** -0.5),
        'mlstm_conv_b': nrm(ks[4], (DEPTH, 2 * M_HEADS * M_DQK), 0.01),
        'i_bias': nrm(ks[5], (DEPTH, M_HEADS), 0.1),
        'f_bias': 3.0 + nrm(ks[6], (DEPTH, M_HEADS), 0.5),
        'mlstm_norm': 1.0 + nrm(ks[7], (DEPTH, M_WIDTH), 0.01),
        'idx_k_norm': 1.0 + nrm(ks[8], (DEPTH, IDX_DIM), 0.01),
        'rel_bias': nrm(ks[9], (REL_BUCKETS, A_HEADS), 0.5),
        'w_out': nrm(ks[10], (DEPTH, D_MIX, D_MODEL), D_MIX ** -0.5),
        'norm_ffn': 1.0 + nrm(ks[11], (DEPTH, D_MODEL), 0.01),
        'w_up': nrm(ks[12], (DEPTH, D_MODEL, 2 * D_FF), D_MODEL ** -0.5),
        'ffn_conv_w': nrm(ks[13], (DEPTH, FFN_CONV, 2 * D_FF), FFN_CONV ** -0.5),
        'ffn_conv_b': nrm(ks[14], (DEPTH, 2 * D_FF), 0.01),
        'w_down': nrm(ks[15], (DEPTH, D_FF, D_MODEL), D_FF ** -0.5),
        'norm_final': 1.0 + nrm(ks[16], (D_MODEL,), 0.01),
    }


def reference(x, norm_mix, w_in, mlstm_conv_w, mlstm_conv_b, i_bias, f_bias, mlstm_norm,
              idx_k_norm, rel_bias, w_out, norm_ffn, w_up, ffn_conv_w, ffn_conv_b, w_down, norm_final):
    bsz, s, _ = x.shape
    for l in range(DEPTH):
        h = rms_norm(x, norm_mix[l])
        z = h @ w_in[l]
        (m_q, m_k, m_v, m_o, m_i, m_f, a_q, a_k, a_v, i_q, i_k, i_w) = split_cols(z, IN_SIZES)

        qk = jax.nn.silu(causal_dwconv(jnp.concatenate([m_q, m_k], axis=-1), mlstm_conv_w[l], mlstm_conv_b[l]))
        m_q, m_k = qk[..., :M_HEADS * M_DQK], qk[..., M_HEADS * M_DQK:]
        hm = mlstm_chunkwise(m_q.reshape(bsz, s, M_HEADS, M_DQK), m_k.reshape(bsz, s, M_HEADS, M_DQK),
                             m_v.reshape(bsz, s, M_HEADS, M_DV), m_i + i_bias[l], m_f + f_bias[l])
        hm = rms_norm(hm, mlstm_norm[l].reshape(M_HEADS, M_DV)).astype(x.dtype)
        hm = jax.nn.sigmoid(m_o) * hm.reshape(bsz, s, M_WIDTH)

        ha = dsa_attention(a_q.reshape(bsz, s, A_HEADS, A_DH), a_k.reshape(bsz, s, A_HEADS, A_DH),
                           a_v.reshape(bsz, s, A_HEADS, A_DH), i_q.reshape(bsz, s, IDX_HEADS, IDX_DIM),
                           rms_norm(i_k, idx_k_norm[l]), i_w * (IDX_HEADS ** -0.5), rel_bias)
        ha = ha.reshape(bsz, s, A_WIDTH)

        x = x + jnp.concatenate([hm, ha], axis=-1) @ w_out[l]

        h = rms_norm(x, norm_ffn[l])
        u = causal_dwconv(h @ w_up[l], ffn_conv_w[l], ffn_conv_b[l])
        gate, val = u[..., :D_FF], u[..., D_FF:]
        x = x + (jax.nn.silu(gate) * val) @ w_down[l]
    return rms_norm(x, norm_final)
```

```python
import math
import numpy as np
import ml_dtypes
import concourse.bass as bass
import concourse.mybir as mybir
from concourse.bass_utils import run_bass_kernel_spmd

F32 = mybir.dt.float32
BF16 = mybir.dt.bfloat16
AF = mybir.ActivationFunctionType
ALU = mybir.AluOpType
AX = mybir.AxisListType

EPS = 1e-6
import os
DBG_NOOVERLAP = os.environ.get('DBG_NOOVERLAP', '0') == '1'
DBG_NOX1 = os.environ.get('DBG_NOX1', '0') == '1'
NITER = 16
NEG = -1.0e30

WIN_GROUPS = [
    [(0, 512)],
    [(512, 512)],
    [(1024, 512)],
    [(1544, 512)],
    [(2056, 512)],
    [(2568, 512)],
    [(3080, 512)],
    [(3592, 72), (1536, 8)],
]


class V:
    __slots__ = ("ap", "key")

    def __init__(self, ap, key):
        if type(ap).__name__.endswith("Handle"):
            ap = ap[:]
        self.ap = ap
        self.key = key

    def __getitem__(self, idx):
        return V(self.ap[idx], self.key)

    def f(self, fn):
        return V(fn(self.ap), self.key)


class Em:
    ENG = ("pe", "act", "dve", "pool", "sp")

    def __init__(self, nc, stack):
        self.nc = nc
        self.stack = stack
        self.q = {e: [] for e in self.ENG}
        self.cnt = {}
        self.sems = {}
        self.lastw = {}
        self.readers = {}
        self.waited = {e: {} for e in self.ENG}
        for e in ("pe", "act", "dve", "pool"):
            self._sem(e)

    def _sem(self, key):
        if key not in self.sems:
            self.sems[key] = self.stack.enter_context(self.nc.semaphore("s_" + key))
            self.cnt[key] = 0
        return self.sems[key]

    def _emit(self, eng, meth, kw, reads, writes, semkey, inc):
        deps = {}

        def add(d):
            if d is None:
                return
            k, v = d
            if deps.get(k, 0) < v:
                deps[k] = v
        for b in reads:
            add(self.lastw.get(b))
        for b in writes:
            add(self.lastw.get(b))
            for k, v in self.readers.get(b, {}).items():
                add((k, v))
        waits = []
        for k, v in deps.items():
            if eng == "pe" and k == "pe":
                continue
            if self.waited[eng].get(k, 0) >= v:
                continue
            self.waited[eng][k] = v
            waits.append((k, v))
        self._sem(semkey)
        self.cnt[semkey] += inc
        val = self.cnt[semkey]
        self.q[eng].append((meth, kw, waits, semkey, inc))
        for b in reads:
            if b in writes:
                continue
            self.readers.setdefault(b, {})[semkey] = val
        for b in writes:
            self.lastw[b] = (semkey, val)
            self.readers[b] = {}
        return val

    def op(self, eng, meth, **kw):
        reads, writes, real = [], [], {}
        for k, v in kw.items():
            if isinstance(v, V):
                real[k] = v.ap
                keys = v.key if isinstance(v.key, (list, tuple)) else [v.key]
                if k in ("out", "accum_out"):
                    writes.extend(keys)
                else:
                    reads.extend(keys)
            else:
                real[k] = v
        return self._emit(eng, meth, real, reads, writes, eng, 1)

    def dma(self, eng, sem, out, in_, **extra):
        okeys = out.key if isinstance(out.key, (list, tuple)) else [out.key]
        ikeys = in_.key if isinstance(in_.key, (list, tuple)) else [in_.key]
        kw = dict(out=out.ap, in_=in_.ap)
        kw.update(extra)
        return self._emit(eng, "dma_start", kw, list(ikeys), list(okeys), sem, 16)

    def barrier(self):
        cur = dict(self.cnt)
        for e in self.ENG:
            waits = []
            for k, v in cur.items():
                if v == 0 or (e == k):
                    continue
                if self.waited[e].get(k, 0) >= v:
                    continue
                self.waited[e][k] = v
                waits.append((k, v))
            if waits:
                self.q[e].append((None, None, waits, None, 0))

    def replay(self, eng, engobj):
        for meth, kw, waits, semkey, inc in self.q[eng]:
            for k, v in waits:
                engobj.wait_ge(self.sems[k], v)
            if meth is None:
                continue
            ins = getattr(engobj, meth)(**kw)
            ins.then_inc(self.sems[semkey], inc)

    def final_wait(self, engobj, eng):
        for k, v in self.cnt.items():
            if v > 0 and self.waited[eng].get(k, 0) < v:
                engobj.wait_ge(self.sems[k], v)


def build(NB, topk):
    from contextlib import ExitStack
    S = NB * 512
    NT = NB * 4
    nc = bass.Bass("TRN2", target_bir_lowering=False)
    stack = ExitStack()
    E = Em(nc, stack)

    def din(name, shape, dt=F32):
        return nc.dram_tensor(name, list(shape), dt, kind="ExternalInput").ap()

    x = din("x", [S, 1024])
    w_in = din("w_in", [1024, 3664])
    w_out = din("w_out", [1024, 1024])
    w_up = din("w_up", [1024, 5632])
    w_down = din("w_down", [2816, 1024])
    d_gmix = din("gmix", [128, 8])
    d_gffn = din("gffn", [128, 8])
    d_gfin = din("gfin", [128, 1024])
    d_mcw = din("mcw", [128, 16])
    d_mcb = din("mcb", [128, 4])
    d_gbias = din("gbias", [128, 8])
    d_mnorm = din("mnorm", [128, 512])
    d_gk = din("gk", [128, 64])
    d_relb = din("relb", [32, 8])
    d_b31 = din("b31", [8, 1])
    d_oh = din("oh", [32, 384])
    d_fcw = din("fcw", [128, 132])
    d_fcb = din("fcb", [128, 44])
    d_ident = din("ident", [128, 128])
    d_tri4 = din("tri4", [128, 512])
    d_cmask = din("cmask", [128, 128])
    d_hkc = din("hkc", [128, NITER + 1])
    d_jrev = din("jrev", [128, 128])
    y = nc.dram_tensor("y", [S, 1024], F32, kind="ExternalOutput").ap()

    def dscratch(name, shape, dt=BF16):
        return nc.dram_tensor(name, list(shape), dt, kind="Internal").ap()

    sc_in = [dscratch("sc_in%d" % g, [128, 8, 512]) for g in range(8)]
    sc_out = [dscratch("sc_out%d" % g, [128, 8, 512]) for g in range(2)]
    sc_up = [dscratch("sc_up%d" % g, [128, 8, 512]) for g in range(11)]
    sc_dn = [dscratch("sc_dn%d" % g, [128, 8, 512]) for g in range(6)]
    tvd = dscratch("tvd", [8, 384])

    def sb(name, shape, dt):
        return stack.enter_context(nc.sbuf_tensor(name, list(shape), dt))

    KT = V(sb("KT", [128, 4, S], BF16), "KT")
    VP_t = sb("VP", [128, NT, 8, 65], BF16)
    IK = V(sb("IK", [128, S], BF16), "IK")
    WT = V(sb("WT", [128, 8, 256], BF16), "WT")
    WS = [V(sb("WS%d" % i, [128, 8, 512], BF16), "WS%d" % i) for i in range(3)]
    X1_t = sb("X1", [128, 4, 1024], F32)
    X1 = [V(X1_t[:, t, :], "X1_%d" % t) for t in range(4)]
    MIXT = sb("MIXT", [128, 8, 512], BF16)
    GMIX = V(sb("GMIX", [128, 8], F32), "c_gmix")
    GFFN = V(sb("GFFN", [128, 8], F32), "c_gffn")
    MCW = V(sb("MCW", [128, 16], F32), "c_mcw")
    MCB = V(sb("MCB", [128, 4], F32), "c_mcb")
    GBIAS = V(sb("GBIAS", [128, 8], F32), "c_gbias")
    GKB = V(sb("GKB", [128, 64], F32), "c_gk")
    FCW = V(sb("FCW", [128, 132], F32), "c_fcw")
    FCB = V(sb("FCB", [128, 44], F32), "c_fcb")
    IDENT = V(sb("IDENT", [128, 128], F32), "c_ident")
    IDENTB = V(sb("IDENTB", [128, 128], BF16), "c_identb")
    CMASK = V(sb("CMASK", [128, 128], F32), "c_cmask")
    HKC = V(sb("HKC", [128, NITER + 1], F32), "c_hkc")
    DG = V(sb("DG", [128, 8, 128], BF16), "DG")
    RELB = V(sb("RELB", [32, 8], F32), "c_relb")
    B31 = V(sb("B31", [8, 1], F32), "c_b31")
    OH = V(sb("OH", [32, 384], F32), "c_oh")
    ONES = V(sb("ONES", [128, 64], F32), "c_ones")
    CST_t = sb("CST", [128, 2, 129], F32)
    CB_t = sb("CB", [128, 4, 129], BF16)
    HM_t = sb("HM", [128, 4, 3], F32)
    HF_t = sb("HF", [128, 44, 2], F32)
    ST_t = sb("ST", [128, 320], F32)
    ARF = 18432
    AR = sb("ARENA", [128, ARF], F32)

    st_off = [0]

    def st(name, n):
        o = st_off[0]
        st_off[0] += n
        assert st_off[0] <= 320
        return V(ST_t[:, o:o + n], "st_" + name)

    def arv(off_bytes, nbytes, dt, key, pat=None, **kw):
        a = AR[:, off_bytes // 4:(off_bytes + nbytes) // 4]
        if dt == BF16:
            a = a.bitcast(BF16)
        if pat is not None:
            a = a.rearrange(pat, **kw)
        return V(a, key)

    PS = [stack.enter_context(nc.psum_tensor("ps%d" % i, [128, 512], F32)) for i in range(8)]
    psrot = {"mm": [0, 0, 4], "acc": [4, 0, 4], "qk": [0, 0, 6], "acc2": [6, 0, 2]}

    def psum(pool):
        b0, i, n = psrot[pool]
        psrot[pool][1] = (i + 1) % n
        k = b0 + i
        return V(PS[k][:, :], "ps%d" % k)

    def bfv(p):
        return V(p.ap.bitcast(BF16), p.key)

    for dst, src in ((GMIX, d_gmix), (GFFN, d_gffn), (MCW, d_mcw), (MCB, d_mcb), (GBIAS, d_gbias), (GKB, d_gk),
                     (FCW, d_fcw), (FCB, d_fcb), (IDENT, d_ident), (CMASK, d_cmask), (HKC, d_hkc), (RELB, d_relb),
                     (B31, d_b31), (OH, d_oh)):
        E.dma("sp", "d_" + dst.key, out=dst[:], in_=V(src, "dram_" + dst.key))
    E.op("dve", "tensor_copy", out=IDENTB[:], in_=IDENT[:])
    E._emit("dve", "memset", dict(ap=ONES.ap, constant=1.0), [], [ONES.key], "dve", 1)
    E._emit("pool", "memset", dict(ap=CST_t[:, :, :], constant=0.0), [], ["CST0", "CST1"], "pool", 1)
    E._emit("pool", "memset", dict(ap=CB_t[:, :, :], constant=0.0), [], ["CB0", "CB1", "CB2", "CB3"], "pool", 1)
    E._emit("pool", "memset", dict(ap=VP_t[:, :, :, 64:65], constant=1.0), [], ["VPones"], "pool", 1)

    TVS = arv(0, 768, BF16, "tvs")
    NB31 = st("nb31", 1)
    E.op("dve", "tensor_scalar", out=NB31[0:8, :], in0=B31[:], scalar1=-1.0, scalar2=None, op0=ALU.mult)
    pt = psum("mm")
    E.op("pe", "matmul", out=pt[0:8, 0:384], lhsT=RELB[:], rhs=OH[:], start=True, stop=True)
    E.op("act", "activation", out=TVS[0:8, :], in_=pt[0:8, 0:384], func=AF.Exp, bias=NB31[0:8, :], scale=1.0)
    E._emit("dve", "memset", dict(ap=TVS.ap[0:8, 0:127], constant=0.0), ["tvs"], ["tvs"], "dve", 1)
    TVD = V(tvd, "tvd")
    E.dma("sp", "d_tvd", out=TVD[:, :], in_=TVS[0:8, :])
    WTR = arv(1024, 4096, BF16, "WTR", "p (h c) -> p h c", h=8)
    JREVB = arv(5120, 256, BF16, "JREVB")
    JREVF = arv(5632, 512, F32, "JREVF")
    E.dma("sp", "d_jrev", out=JREVF[:], in_=V(d_jrev, "dram_jrev"))
    E.op("dve", "tensor_copy", out=JREVB[:], in_=JREVF[:])
    for h in range(8):
        src = bass.AP(tensor=tvd.tensor, offset=h * 384, ap=[[1, 128], [1, 256]])
        E.dma("sp", "d_wt", out=WTR[:, h, :], in_=V(src, "tvd"))
    for q4 in range(4):
        pw = psum("mm")
        E.op("pe", "matmul", out=pw[:, :], lhsT=JREVB[:],
             rhs=WTR[:, 2 * q4:2 * q4 + 2, :].f(lambda a: a.rearrange("p h c -> p (h c)")), start=True, stop=True)
        E.op("dve", "tensor_copy", out=WT[:, 2 * q4:2 * q4 + 2, :],
             in_=pw[:, :].f(lambda a: a.rearrange("p (h c) -> p h c", h=2)))
    E.barrier()

    STG = [arv(1024 + i * 16384, 16384, F32, "stg%d" % i, "p (a b) -> p a b", a=8) for i in range(2)]
    STB = [arv(1024 + 32768 + i * 8192, 8192, BF16, "stb%d" % i, "p (a b) -> p a b", a=8) for i in range(2)]
    groups = []
    w_in_r = w_in.rearrange("(c p) n -> p c n", p=128)
    w_out_r = w_out.rearrange("(c p) n -> p c n", p=128)
    w_up_r = w_up.rearrange("(c p) n -> p c n", p=128)
    w_dn_r = w_down.rearrange("(c p) n -> p c n", p=128)
    for g, pieces in enumerate(WIN_GROUPS):
        pcs, c0 = [], 0
        for (s0, n) in pieces:
            pcs.append((w_in_r[:, :, s0:s0 + n], c0, n))
            c0 += n
        groups.append((sc_in[g], "sc_in%d" % g, pcs, 8, c0, GMIX))
    for g in range(2):
        groups.append((sc_out[g], "sc_out%d" % g, [(w_out_r[:, :, g * 512:(g + 1) * 512], 0, 512)], 8, 512, None))
    for g in range(11):
        groups.append((sc_up[g], "sc_up%d" % g,
                       [(w_up_r[:, :, 256 * g:256 * g + 256], 0, 256),
                        (w_up_r[:, :, 2816 + 256 * g:2816 + 256 * g + 256], 256, 256)], 8, 512, GFFN))
    for hf in range(2):
        for kg in range(3):
            nk = 8 if kg < 2 else 6
            groups.append((sc_dn[hf * 3 + kg], "sc_dn%d" % (hf * 3 + kg),
                           [(w_dn_r[:, 8 * kg:8 * kg + nk, hf * 512:(hf + 1) * 512], 0, 512)], nk, 512, None))
    cast_engs = ["dve", "act"]
    for gi, (dst, dkey, pcs, kc, ncols, scl) in enumerate(groups):
        sl = gi % 2
        for (src, c0, n) in pcs:
            E.dma("sp", "d_stg%d" % sl, out=STG[sl][:, 0:kc, c0:c0 + n], in_=V(src, "dram_w"))
        eng = cast_engs[gi % 2]
        if scl is None:
            if eng == "act":
                E.op("act", "activation", out=STB[sl][:, 0:kc, 0:ncols], in_=STG[sl][:, 0:kc, 0:ncols], func=AF.Copy)
            else:
                E.op(eng, "tensor_copy", out=STB[sl][:, 0:kc, 0:ncols], in_=STG[sl][:, 0:kc, 0:ncols])
        else:
            for c in range(kc):
                if eng == "act":
                    E.op("act", "activation", out=STB[sl][:, c, 0:ncols], in_=STG[sl][:, c, 0:ncols],
                         func=AF.Copy, scale=scl[:, c:c + 1])
                else:
                    E.op(eng, "tensor_scalar", out=STB[sl][:, c, 0:ncols], in0=STG[sl][:, c, 0:ncols],
                         scalar1=scl[:, c:c + 1], scalar2=None, op0=ALU.mult)
        E.dma("pool", "d_stb%d" % sl, out=V(dst[:, 0:kc, 0:ncols], dkey), in_=STB[sl][:, 0:kc, 0:ncols])
    E.barrier()

    wseq = []
    for I in range(NB):
        for g in range(8):
            wseq.append((sc_in[g], "sc_in%d" % g, 8, 512 if g < 7 else 80))
        for g in range(2):
            wseq.append((sc_out[g], "sc_out%d" % g, 8, 512))
        for g in range(11):
            wseq.append((sc_up[g], "sc_up%d" % g, 8, 512))
        for g in range(6):
            wseq.append((sc_dn[g], "sc_dn%d" % g, 8 if g % 3 < 2 else 6, 512))
    wstate = {"loaded": 0, "next": 0}

    def get_w():
        idx = wstate["next"]
        wstate["next"] += 1
        while wstate["loaded"] <= idx + 2 and wstate["loaded"] < len(wseq):
            li = wstate["loaded"]
            src, skey, kc, ncols = wseq[li]
            slot = li % 3
            E.dma("sp", "d_ws%d" % slot, out=WS[slot][:, 0:kc, 0:ncols], in_=V(src[:, 0:kc, 0:ncols], skey))
            wstate["loaded"] += 1
        return WS[idx % 3]

    HB = arv(0, 2048, BF16, "HB")
    HBA = [HB, arv(38400, 2048, BF16, "HBb")]
    HBE = [HB, arv(67584, 2048, BF16, "HBc")]
    HT = arv(2048, 8192, BF16, "HT", "p (a b) -> p a b", a=8)
    QKT = arv(10240, 4096, BF16, "QKT", "p (a b) -> p a b", a=4)
    YC = [arv(14336 + i * 2048, 2048, F32, "YC%d" % i) for i in range(2)]
    VM_full = AR[:, 18432 // 4:(18432 + 4160) // 4].bitcast(BF16)[:, 0:2064].rearrange("p (t h d) -> p t h d", t=4, h=4)
    VM = V(VM_full, "VM")
    OG = arv(22592, 4096, BF16, "OG", "p (a b) -> p a b", a=4)
    TRI4 = arv(26688, 2048, F32, "TRI4")
    MNORM = arv(28736, 2048, F32, "MNORM")
    IKN = arv(30784, 128, BF16, "IKN")
    KW = arv(30912, 512, BF16, "KW", "p (h d) -> p h d", h=4)
    PTB = [arv(37056 + i * 256, 256, BF16, "PT%d" % i) for i in range(4)]
    HMO = arv(31936, 1024, BF16, "HMO")
    JUNK = arv(32960, 4096, F32, "JUNK")
    AQT = arv(65536, 4096, BF16, "AQT", "p (a b) -> p a b", a=4)
    IQT = arv(69632, 4096, BF16, "IQT", "p (a b) -> p a b", a=4)
    SCK = [V(AR[:, (0 + kc * 2048) // 4:(0 + (kc + 1) * 2048) // 4], "SC%d" % kc) for kc in range(8)]
    SCall = V(AR[:, 0:4096], ["SC%d" % kc for kc in range(8)])
    MQ = arv(16384, 8192, BF16, "MQ")
    MT = arv(24576, 32768, BF16, "MT", "p (j q) -> p j q", j=32)
    EP = [arv(57344 + i * 1024, 1024, BF16, "EP%d" % i) for i in range(8)]
    FT = [arv(i * 2048, 2048, F32, "FT%d" % i) for i in range(4)]
    RB = [arv(61440 + i * 2048, 2048, F32, "RB%d" % i) for i in range(2)]
    RBF = [arv(61440 + i * 1024, 1024, BF16, "RBF%d" % i) for i in range(4)]
    YE = [arv(10240 + i * 2048, 2048, F32, "YE%d" % i) for i in range(4)]
    AT = arv(18432, 22528, BF16, "AT", "p (c q) -> p c q", c=22)
    GFIN = arv(40960, 4096, F32, "GFIN")
    OUTB = [arv(45056 + i * 4096, 4096, F32, "OUTB%d" % i) for i in range(2)]
    JUNK2 = arv(53248, 4096, F32, "JUNK2")
    UE = [arv(57344 + i * 2064, 2056, F32, "UE%d" % i) for i in range(4)]

    SS = [st("ss%d" % t_, 1) for t_ in range(4)]
    RSTD = [st("rstd%d" % t_, 1) for t_ in range(4)]
    SSK = [st("ssk%d" % t_, 1) for t_ in range(4)]
    RK = [st("rk%d" % t_, 1) for t_ in range(4)]
    GATES = [st("gates%d" % t, 8) for t in range(4)]
    WQ = [st("wq%d" % t, 8) for t in range(4)]
    E1 = st("e1", 4)
    NLF = st("nlf", 4)
    GS = st("gs", 16)
    EB8 = st("eb8", 4)
    T1 = st("t1", 4)
    EK = st("ek", 4)
    T2 = st("t2", 4)
    WKs = st("wk", 4)
    EG = st("eg", 8)
    DN = st("dn", 4)
    RR = st("rr", 4)
    MS = st("ms", 4)
    TT = st("tt", 4)
    SCL = st("scl", 4)
    MX = st("mx", 1)
    MN2 = [st("mn0", 1), st("mn1", 1)]
    W0 = st("w0", 1)
    NHK = st("nhk", NITER + 1)
    NM = st("nm", 1)
    SACC = st("sacc", 1)
    SG = st("sg", 1)
    LO = st("lo", 1)
    MID = st("mid", 1)
    CNT = st("cnt", 1)
    TMP = st("tmp", 1)
    SS2 = [st("ss2%d" % t_, 1) for t_ in range(4)]
    RSTD2 = [st("rstd2%d" % t_, 1) for t_ in range(4)]
    SS3 = [st("ss3%d" % t_, 1) for t_ in range(4)]
    RSTD3 = [st("rstd3%d" % t_, 1) for t_ in range(4)]

    CST = [V(CST_t[:, pp, :], "CST%d" % pp) for pp in range(2)]
    CBh = [V(CB_t[:, h, :], "CB%d" % h) for h in range(4)]
    HMh = [V(HM_t[:, c, :], "HM%d" % c) for c in range(4)]
    HFh = [V(HF_t[:, c, :], "HF%d" % c) for c in range(44)]
    MIXc = [V(MIXT[:, c, :], "MIX%d" % c) for c in range(8)]
    MIX03 = V(MIXT[:, 0:4, :], ["MIX0", "MIX1", "MIX2", "MIX3"])

    def rstd_from_ss(ssv, rv, n):
        E.op("dve", "tensor_scalar", out=rv, in0=ssv, scalar1=1.0 / n, scalar2=EPS, op0=ALU.mult, op1=ALU.add)
        E.op("act", "activation", out=rv, in_=rv, func=AF.Sqrt)
        E.op("dve", "reciprocal", out=rv, in_=rv)

    def transposes_to(dst_view, src, nchunk):
        p = psum("mm")
        pb = bfv(p)
        for c in range(nchunk):
            E.op("pe", "transpose", out=pb[:, c * 128:(c + 1) * 128], in_=src[:, c * 128:(c + 1) * 128],
                 identity=IDENTB[:])
        E.op("act", "activation", out=dst_view,
             in_=pb[:, 0:nchunk * 128].f(lambda a: a.rearrange("p (c q) -> p c q", c=nchunk)), func=AF.Copy)

    rr = {"i": 0}

    def alt(*engs):
        rr["i"] += 1
        return engs[rr["i"] % len(engs)]

    for I in range(NB):
        tok0 = I * 512
        E.dma("sp", "d_tri4", out=TRI4[:], in_=V(d_tri4, "dram_tri4"))
        E.dma("sp", "d_mnorm", out=MNORM[:], in_=V(d_mnorm, "dram_mnorm"))
        if I == 0:
            for t in range(4):
                E.dma("sp", "d_x%d" % t, out=X1[t][:], in_=V(x[tok0 + t * 128:tok0 + (t + 1) * 128, :], "dram_x"))
        for t in range(4):
            E.op("act", "activation", out=JUNK[:], in_=X1[t][:], func=AF.Square, accum_out=SS[t][:])
            rstd_from_ss(SS[t][:], RSTD[t][:], 1024.0)
            E.op("dve", "tensor_scalar", out=HBA[t % 2][:], in0=X1[t][:], scalar1=RSTD[t][:], scalar2=None, op0=ALU.mult)
            transposes_to(HT[:, :, t * 128:(t + 1) * 128], HBA[t % 2], 8)

        def fm_group(W, evac):
            for cc in range(4):
                p = psum("mm")
                for c in range(8):
                    E.op("pe", "matmul", out=p[:, :], lhsT=W[:, c, cc * 128:(cc + 1) * 128], rhs=HT[:, c, :],
                         start=(c == 0), stop=(c == 7))
                evac(cc, p)

        def tm_group(W, ncols, evac):
            for t in range(4):
                p = psum("mm")
                for c in range(8):
                    E.op("pe", "matmul", out=p[:, 0:ncols], lhsT=HT[:, c, t * 128:(t + 1) * 128], rhs=W[:, c, 0:ncols],
                         start=(c == 0), stop=(c == 7))
                evac(t, p)

        def ev_g0(cc, p):
            Y = YC[cc % 2]
            E.op("act", "activation", out=Y[:], in_=p[:, :], func=AF.Identity, scale=MCW[:, cc * 4 + 3:cc * 4 + 4],
                 bias=MCB[:, cc:cc + 1])
            for j, sh in ((2, 1), (1, 2), (0, 3)):
                E.op("dve", "scalar_tensor_tensor", out=Y[:, sh:512], in0=p[:, 0:512 - sh],
                     scalar=MCW[:, cc * 4 + j:cc * 4 + j + 1], in1=Y[:, sh:512], op0=ALU.mult, op1=ALU.add)
            if I > 0:
                H = HMh[cc]
                E.op("dve", "scalar_tensor_tensor", out=Y[:, 0:3], in0=H[:, 0:3], scalar=MCW[:, cc * 4:cc * 4 + 1],
                     in1=Y[:, 0:3], op0=ALU.mult, op1=ALU.add)
                E.op("dve", "scalar_tensor_tensor", out=Y[:, 0:2], in0=H[:, 1:3], scalar=MCW[:, cc * 4 + 1:cc * 4 + 2],
                     in1=Y[:, 0:2], op0=ALU.mult, op1=ALU.add)
                E.op("dve", "scalar_tensor_tensor", out=Y[:, 0:1], in0=H[:, 2:3], scalar=MCW[:, cc * 4 + 2:cc * 4 + 3],
                     in1=Y[:, 0:1], op0=ALU.mult, op1=ALU.add)
            E.op("act", "activation", out=HMh[cc][:], in_=p[:, 509:512], func=AF.Copy)
            E.op("act", "activation", out=QKT[:, cc, :], in_=Y[:], func=AF.Silu)
        fm_group(get_w(), ev_g0)

        def ev_g1(t, p):
            E.op("act", "activation", out=VM[:, t, :, 0:128],
                 in_=p[:, :].f(lambda a: a.rearrange("p (h d) -> p h d", h=4)), func=AF.Copy)
        E._emit("pool", "memset", dict(ap=VM.ap[:, :, :, 128:129], constant=1.0), [], ["VM"], "pool", 1)
        tm_group(get_w(), 512, ev_g1)

        def ev_g2(t, p):
            E.op("act", "activation", out=OG[:, t, :], in_=p[:, :], func=AF.Sigmoid)
            E.op("pool", "tensor_tensor", out=OG[:, t, :], in0=OG[:, t, :], in1=MNORM[:], op=ALU.mult)
        tm_group(get_w(), 512, ev_g2)

        def ev_g3(cc, p):
            E.op("act", "activation", out=AQT[:, cc, :], in_=p[:, :], func=AF.Copy, scale=0.125)
        fm_group(get_w(), ev_g3)

        def ev_g4(cc, p):
            E.op("dve", "tensor_copy", out=KT[:, cc, tok0:tok0 + 512], in_=p[:, :])
        fm_group(get_w(), ev_g4)

        def ev_g5(t, p):
            E.op("act", "activation", out=V(VP_t[:, I * 4 + t, :, 0:64], "VP"),
                 in_=p[:, :].f(lambda a: a.rearrange("p (h d) -> p h d", h=8)), func=AF.Copy)
        tm_group(get_w(), 512, ev_g5)

        def ev_g6(cc, p):
            E.op("dve", "tensor_copy", out=IQT[:, cc, :], in_=p[:, :])
        fm_group(get_w(), ev_g6)

        def ev_g7(t, p):
            E.op("act", "activation", out=JUNK[:, 0:64], in_=p[:, 0:64], func=AF.Square, accum_out=SSK[t][:])
            rstd_from_ss(SSK[t][:], RK[t][:], 64.0)
            E.op("dve", "scalar_tensor_tensor", out=IKN[:], in0=p[:, 0:64], scalar=RK[t][:], in1=GKB[:],
                 op0=ALU.mult, op1=ALU.mult)
            E.op("dve", "tensor_scalar", out=WQ[t][:], in0=p[:, 64:72], scalar1=float(8 ** -0.5 * 64 ** -0.5),
                 scalar2=None, op0=ALU.mult)
            E.op("dve", "tensor_tensor", out=GATES[t][:], in0=p[:, 72:80], in1=GBIAS[:], op=ALU.add)
            p2 = psum("mm")
            pb = bfv(p2)
            E.op("pe", "transpose", out=pb[0:64, 0:128], in_=IKN[:], identity=IDENTB[:])
            c0 = tok0 + t * 128
            E.op("dve", "tensor_copy", out=IK[0:64, c0:c0 + 128], in_=pb[0:64, 0:128])
            E.op("dve", "tensor_copy", out=IK[64:128, c0:c0 + 128], in_=pb[0:64, 0:128])
        tm_group(get_w(), 80, ev_g7)

        for t in range(4):
            c0 = t * 128
            pk = psum("mm")
            pkb = bfv(pk)
            for pp in range(2):
                E.op("pe", "transpose", out=pkb[:, pp * 128:(pp + 1) * 128], in_=QKT[:, 2 + pp, c0:c0 + 128],
                     identity=IDENTB[:])
            G = GATES[t]
            E.op("act", "activation", out=E1[:], in_=G[:, 4:8], func=AF.Exp, scale=-1.0)
            E.op("dve", "tensor_scalar", out=E1[:], in0=E1[:], scalar1=1.0, scalar2=None, op0=ALU.add)
            E.op("act", "activation", out=NLF[:], in_=E1[:], func=AF.Ln)
            pg = psum("mm")
            for k in range(4):
                E.op("pe", "matmul", out=pg[:, 4 * k:4 * k + 4], lhsT=TRI4[:, 128 * k:128 * (k + 1)], rhs=NLF[:],
                     start=True, stop=True)
            E.op("dve", "tensor_copy", out=GS[:], in_=pg[:, 0:16])
            E.op("act", "activation", out=EB8[:], in_=GS[:, 0:4], func=AF.Exp, scale=-1.0, bias=-math.log(8.0))
            E.op("dve", "tensor_tensor", out=T1[:], in0=G[:, 0:4], in1=GS[:, 0:4], op=ALU.add)
            E.op("act", "activation", out=EK[:], in_=T1[:], func=AF.Exp)
            E.op("dve", "tensor_tensor", out=T2[:], in0=T1[:], in1=GS[:, 4:8], op=ALU.subtract)
            E.op("act", "activation", out=WKs[:], in_=T2[:], func=AF.Exp)
            E.op("act", "activation", out=EG[:], in_=GS[:, 8:16], func=AF.Exp, scale=-1.0)
            E.op("dve", "tensor_tensor", out=KW[:],
                 in0=pkb[:, 0:256].f(lambda a: a.rearrange("p (h d) -> p h d", h=4)),
                 in1=WKs[:, 0:4].f(lambda a: a.unsqueeze(2).to_broadcast([128, 4, 64])), op=ALU.mult)
            npss = [psum("acc") for _ in range(4)]
            for h in range(4):
                pp, hh = h // 2, h % 2
                base, o = 64 * hh, 0
                pr = psum("mm")
                E.op("pe", "matmul", out=pr[:, 0:128], lhsT=QKT[base:base + 64, 2 + pp, c0:c0 + 128],
                     rhs=QKT[base:base + 64, pp, c0:c0 + 128], start=True, stop=True)
                PT = PTB[h]
                E.op("dve", "scalar_tensor_tensor", out=PT[:], in0=pr[:, 0:128], scalar=EK[:, h:h + 1],
                     in1=TRI4[:, 0:128], op0=ALU.mult, op1=ALU.mult)
                E.op("pe", "matmul", out=npss[h][:, o:o + 129], lhsT=PT[:], rhs=VM[:, t, h, :], start=True, stop=False,
                     skip_group_check=True)
            for ch in range(2):
                r0 = 64 * ch
                pkvs = []
                for h in range(4):
                    pp, hh = h // 2, h % 2
                    base, o = 64 * hh, 0
                    E.op("pe", "matmul", out=npss[h][r0:r0 + 64, o:o + 129], lhsT=QKT[:, pp, c0 + r0:c0 + r0 + 64],
                         rhs=CBh[h][:], start=False, stop=(ch == 1), skip_group_check=True)
                    pkv = psum("mm")
                    E.op("pe", "matmul", out=pkv[base:base + 64, 0:129], lhsT=KW[r0:r0 + 64, h, :],
                         rhs=VM[r0:r0 + 64, t, h, :], start=True, stop=True)
                    pkvs.append(pkv)
                for h in range(4):
                    pp, hh = h // 2, h % 2
                    base = 64 * hh
                    E.op("dve", "scalar_tensor_tensor", out=CST[pp][base:base + 64, :], in0=CST[pp][base:base + 64, :],
                         scalar=EG[base:base + 64, 4 * ch + h:4 * ch + h + 1], in1=pkvs[h][base:base + 64, 0:129],
                         op0=ALU.mult, op1=ALU.add)
                for h in range(4):
                    pp, hh = h // 2, h % 2
                    base = 64 * hh
                    E.op("act", "activation", out=CBh[h][base:base + 64, :], in_=CST[pp][base:base + 64, :],
                         func=AF.Copy)
            for h in range(4):
                E.op("dve", "tensor_tensor", out=DN[:, h:h + 1], in0=npss[h][:, 128:129], in1=EB8[:, h:h + 1], op=ALU.mult)
            E.op("act", "activation", out=DN[:], in_=DN[:], func=AF.Abs)
            E.op("dve", "tensor_scalar", out=DN[:], in0=DN[:], scalar1=1.0, scalar2=None, op0=ALU.max)
            E.op("dve", "reciprocal", out=DN[:], in_=DN[:])
            E.op("dve", "tensor_tensor", out=RR[:], in0=DN[:], in1=EB8[:, 0:4], op=ALU.mult)
            for h in range(4):
                E.op("act", "activation", out=JUNK[:, h * 128:(h + 1) * 128], in_=npss[h][:, 0:128], func=AF.Square,
                     accum_out=MS[:, h:h + 1])
            E.op("dve", "tensor_tensor", out=TT[:], in0=RR[:], in1=RR[:], op=ALU.mult)
            E.op("dve", "tensor_tensor", out=TT[:], in0=TT[:], in1=MS[:], op=ALU.mult)
            E.op("dve", "tensor_scalar", out=TT[:], in0=TT[:], scalar1=1.0 / 128.0, scalar2=EPS, op0=ALU.mult,
                 op1=ALU.add)
            E.op("act", "activation", out=TT[:], in_=TT[:], func=AF.Sqrt)
            E.op("dve", "reciprocal", out=TT[:], in_=TT[:])
            E.op("dve", "tensor_tensor", out=SCL[:], in0=TT[:], in1=RR[:], op=ALU.mult)
            for h in range(4):
                E.op("dve", "scalar_tensor_tensor", out=HMO[:, h * 128:(h + 1) * 128],
                     in0=npss[h][:, 0:128], scalar=SCL[:, h:h + 1], in1=OG[:, t, h * 128:(h + 1) * 128],
                     op0=ALU.mult, op1=ALU.mult)
            transposes_to(MIX03[:, :, c0:c0 + 128], HMO, 4)

        E.barrier()
        nkt = 4 * I + 4
        X1flat = X1_t[:].rearrange("p a b -> p (a b)")
        SCB = [
            ([SCK[kc] for kc in range(8)], SCall),
            ([V(X1_t[:, kc // 2, (kc % 2) * 512:(kc % 2) * 512 + 512], "X1_%d" % (kc // 2)) for kc in range(8)],
             V(X1flat, ["X1_0", "X1_1", "X1_2", "X1_3"])),
        ]

        def score_gen(t):
            i = 4 * I + t
            n = 128 * (i + 1)
            nch = (n + 511) // 512
            sck, scall = SCB[0 if DBG_NOX1 else t % 2]
            for h in range(8):
                E.op("dve", "tensor_scalar", out=DG[:, h, :], in0=IDENT[:], scalar1=WQ[t][:, h:h + 1], scalar2=None,
                     op0=ALU.mult)
            dk = i // 4
            for kc in range(nch):
                w = min(512, n - 512 * kc)
                pacc = psum("acc")
                pis = {}

                def emit_isc(h, kc=kc, w=w):
                    base, pp = 64 * (h % 2), h // 2
                    p = psum("mm")
                    E.op("pe", "matmul", out=p[:, 0:w], lhsT=IQT[base:base + 64, pp, t * 128:(t + 1) * 128],
                         rhs=IK[base:base + 64, 512 * kc:512 * kc + w], start=True, stop=True)
                    pis[h] = p
                for h in range(3):
                    emit_isc(h)
                for h in range(8):
                    if h + 3 < 8:
                        emit_isc(h + 3)
                    p = pis.pop(h)
                    R = RBF[h % 4]
                    if h % 2 == 0:
                        E.op("act", "activation", out=R[:, 0:w], in_=p[:, 0:w], func=AF.Relu)
                    else:
                        E.op("dve", "tensor_scalar", out=R[:, 0:w], in0=p[:, 0:w], scalar1=0.0, scalar2=None, op0=ALU.max)
                    E.op("pe", "matmul", out=pacc[:, 0:w], lhsT=DG[:, h, :], rhs=R[:, 0:w], start=(h == 0),
                         stop=(h == 7), skip_group_check=True)
                E.op("dve", "tensor_copy", out=sck[kc][:, 0:w], in_=pacc[:, 0:w])
                yield
            if n > topk:
                E.op("dve", "tensor_reduce", out=MN2[t % 2][:], in_=scall[:, 0:n], axis=AX.X, op=ALU.min)
            E.op("dve", "tensor_tensor", out=sck[dk][:, (i % 4) * 128:(i % 4) * 128 + 128],
                 in0=sck[dk][:, (i % 4) * 128:(i % 4) * 128 + 128], in1=CMASK[:], op=ALU.add)
            yield

        def bisect_gen(t):
            i = 4 * I + t
            n = 128 * (i + 1)
            sck, scall = SCB[0 if DBG_NOX1 else t % 2]
            MNt = MN2[t % 2]
            if n > topk:
                E.op("dve", "tensor_reduce", out=MX[:], in_=scall[:, 0:n], axis=AX.X, op=ALU.max)
                E.op("dve", "tensor_tensor", out=W0[:], in0=MX[:], in1=MNt[:], op=ALU.subtract)
                E.op("dve", "tensor_scalar", out=W0[:], in0=W0[:], scalar1=1.0001, scalar2=1e-6, op0=ALU.mult, op1=ALU.add)
                E.op("dve", "tensor_scalar", out=NHK[:], in0=HKC[:], scalar1=W0[:], scalar2=-1.0, op0=ALU.mult, op1=ALU.mult)
                E.op("dve", "scalar_tensor_tensor", out=NM[:], in0=MNt[:], scalar=-1.0, in1=NHK[:, 0:1], op0=ALU.mult,
                     op1=ALU.add)
                cthr = float(2 * topk - n) - 0.5
                split = n >= 1536
                if split:
                    nA = ((int(0.56 * n) + 127) // 128) * 128
                    nB = n - nA
                    MQa = V(MQ.ap[:, 0:nA], "MQa")
                    MQb = V(MQ.ap[:, nA:n], "MQb")
                    E.op("dve", "tensor_scalar", out=MID[:], in0=NM[:], scalar1=-1.0, scalar2=None, op0=ALU.mult)
                yield
                for k in range(NITER):
                    if split:
                        E.op("act", "activation", out=MQa, in_=scall[:, 0:nA], func=AF.Sign, bias=NM[:], scale=1.0,
                             accum_out=SACC[:])
                        E.op("dve", "tensor_scalar", out=MQb, in0=scall[:, nA:n], scalar1=MID[:], scalar2=None,
                             op0=ALU.is_ge, op1=ALU.add, accum_out=CNT[:])
                        yield
                        E.op("dve", "tensor_scalar", out=TMP[:], in0=CNT[:], scalar1=2.0, scalar2=-(float(nB) + cthr),
                             op0=ALU.mult, op1=ALU.add)
                        E.op("act", "activation", out=SG[:], in_=SACC[:], func=AF.Sign, bias=TMP[:], scale=1.0)
                    else:
                        E.op("act", "activation", out=MQ[:, 0:n], in_=scall[:, 0:n], func=AF.Sign, bias=NM[:], scale=1.0,
                             accum_out=SACC[:])
                        yield
                        E.op("act", "activation", out=SG[:], in_=SACC[:], func=AF.Sign, bias=-cthr, scale=1.0)
                    E.op("act", "activation", out=NM[:], in_=SG[:], func=AF.Identity, scale=NHK[:, k + 1:k + 2], bias=NM[:])
                    if split and k + 1 < NITER:
                        E.op("dve", "tensor_scalar", out=MID[:], in0=NM[:], scalar1=-1.0, scalar2=None, op0=ALU.mult)
                E.op("act", "activation", out=LO[:], in_=NM[:], func=AF.Identity, scale=-1.0, bias=NHK[:, NITER:NITER + 1])
                E.op("dve", "tensor_scalar", out=V(MQ.ap[:, 0:n], ["MQ", "MQa", "MQb"]), in0=scall[:, 0:n], scalar1=LO[:],
                     scalar2=None, op0=ALU.is_ge)
            else:
                E.op("dve", "tensor_scalar", out=MQ[:, 0:n], in0=scall[:, 0:n], scalar1=-1.0e29, scalar2=None,
                     op0=ALU.is_ge)
            if t < 3:
                E._emit("pool", "memset", dict(ap=MQ.ap[:, n:128 * nkt], constant=0.0), [], ["MQ"], "pool", 1)
            yield
            for jg in range((nkt + 3) // 4):
                p = psum("mm")
                pb = bfv(p)
                for jj in range(4):
                    j = 4 * jg + jj
                    E.op("pe", "transpose", out=pb[:, jj * 128:(jj + 1) * 128], in_=MQ[:, 128 * j:128 * (j + 1)],
                         identity=IDENTB[:])
                E.op("dve", "tensor_copy", out=MT[:, 4 * jg:4 * jg + 4, t * 128:(t + 1) * 128],
                     in_=pb[:, 0:512].f(lambda a: a.rearrange("p (j q) -> p j q", j=4)))
            yield

        for _ in score_gen(0):
            pass
        for t in range(4):
            bg = bisect_gen(t)
            sg = score_gen(t + 1) if t + 1 < 4 else None
            nb_steps = NITER + 3
            ns_steps = (((128 * (4 * I + t + 2) + 511) // 512) + 1) if sg is not None else 0
            ratio = float(ns_steps) / nb_steps
            if DBG_NOOVERLAP:
                ratio = 0.0
            accr = 0.0
            for _ in bg:
                accr += ratio
                while sg is not None and accr >= 1.0:
                    accr -= 1.0
                    try:
                        next(sg)
                    except StopIteration:
                        sg = None
            if sg is not None:
                for _ in sg:
                    pass
        for t in range(4):
            E.dma("sp", "d_x%d" % t, out=X1[t][:], in_=V(x[tok0 + t * 128:tok0 + (t + 1) * 128, :], "dram_x"))
        for pp in range(4):
            accs = [psum("acc2"), psum("acc2")]
            qk = {}

            def emit_qk(j, pp=pp):
                for hh in range(2):
                    base = 64 * hh
                    p = psum("qk")
                    E.op("pe", "matmul", out=p[:, :], lhsT=KT[base:base + 64, pp, 128 * j:128 * (j + 1)],
                         rhs=AQT[base:base + 64, pp, :], start=True, stop=True)
                    qk[(j, hh)] = p
            for j in range(min(2, nkt)):
                emit_qk(j)
            for j in range(nkt):
                if j + 2 < nkt:
                    emit_qk(j + 2)
                for hh in range(2):
                    h = 2 * pp + hh
                    p = qk.pop((j, hh))
                    e = EP[(2 * j + hh) % 8]
                    E.op("act", "activation", out=e[:], in_=p[:, :], func=AF.Exp)
                    E.op("dve", "tensor_tensor", out=e[:], in0=e[:], in1=MT[:, j, :], op=ALU.mult)
                    r = j - 4 * I
                    if r >= -1:
                        for tq, v in ((r, 0), (r + 1, 1)):
                            if 0 <= tq <= 3:
                                E.op("dve", "tensor_tensor", out=e[:, tq * 128:(tq + 1) * 128],
                                     in0=e[:, tq * 128:(tq + 1) * 128], in1=WT[:, h, v * 128:(v + 1) * 128], op=ALU.mult)
                    E.op("pe", "matmul", out=accs[hh][0:65, :], lhsT=V(VP_t[:, j, h, :], ["VP", "VPones"]), rhs=e[:],
                         start=(j == 0), stop=(j == nkt - 1))
            for hh in range(2):
                base = 64 * hh
                acc = accs[hh]
                E.op("dve", "reciprocal", out=FT[2 * hh][64:65, :], in_=acc[64:65, :])
                pbc = psum("qk")
                E.op("pe", "matmul", out=pbc[0:64, :], lhsT=ONES[64:65, 0:64], rhs=FT[2 * hh][64:65, :], start=True,
                     stop=True)
                E.op("act", "activation", out=FT[2 * hh + 1][0:64, :], in_=pbc[0:64, :], func=AF.Copy)
                E.op("dve", "tensor_tensor", out=MIXc[4 + pp][base:base + 64, :], in0=acc[0:64, :],
                     in1=FT[2 * hh + 1][0:64, :], op=ALU.mult)

        E.barrier()
        E.dma("sp", "d_gfin", out=GFIN[:], in_=V(d_gfin, "dram_gfin"))
        for hf in range(2):
            W = get_w()
            for t in range(4):
                p = psum("mm")
                for c in range(8):
                    E.op("pe", "matmul", out=p[:, :], lhsT=MIXc[c][:, t * 128:(t + 1) * 128], rhs=W[:, c, :],
                         start=(c == 0), stop=(c == 7))
                E.op("dve", "tensor_tensor", out=X1[t][:, hf * 512:(hf + 1) * 512], in0=p[:, :],
                     in1=X1[t][:, hf * 512:(hf + 1) * 512], op=ALU.add)
        for t in range(4):
            E.op("act", "activation", out=JUNK2[:], in_=X1[t][:], func=AF.Square, accum_out=SS2[t][:])
            rstd_from_ss(SS2[t][:], RSTD2[t][:], 1024.0)
            E.op("dve", "tensor_scalar", out=HBE[t % 2][:], in0=X1[t][:], scalar1=RSTD2[t][:], scalar2=None, op0=ALU.mult)
            transposes_to(HT[:, :, t * 128:(t + 1) * 128], HBE[t % 2], 8)

        def ffn_conv(p, Y, U, ci):
            E.op("act", "activation", out=Y[:], in_=p[:, :], func=AF.Identity, scale=FCW[:, 3 * ci + 2:3 * ci + 3],
                 bias=FCB[:, ci:ci + 1])
            E.op("act", "activation", out=U[:, 2:514], in_=p[:, :], func=AF.Copy)
            if I > 0:
                E.op("act", "activation", out=U[:, 0:2], in_=HFh[ci][:], func=AF.Copy)
            else:
                E._emit("act", "memzero", dict(ap=U.ap[:, 0:2]), [], [U.key], "act", 1)
            E.op("act", "activation", out=HFh[ci][:], in_=U[:, 512:514], func=AF.Copy)
            E.op("dve", "scalar_tensor_tensor", out=Y[:], in0=U[:, 1:513], scalar=FCW[:, 3 * ci + 1:3 * ci + 2], in1=Y[:],
                 op0=ALU.mult, op1=ALU.add)
            E.op("dve", "scalar_tensor_tensor", out=Y[:], in0=U[:, 0:512], scalar=FCW[:, 3 * ci:3 * ci + 1], in1=Y[:],
                 op0=ALU.mult, op1=ALU.add)

        for g in range(11):
            W = get_w()
            for sub in range(2):
                c = 2 * g + sub
                pg_ = psum("mm")
                pv_ = psum("mm")
                for kc in range(8):
                    E.op("pe", "matmul", out=pg_[:, :], lhsT=W[:, kc, sub * 128:(sub + 1) * 128], rhs=HT[:, kc, :],
                         start=(kc == 0), stop=(kc == 7))
                for kc in range(8):
                    E.op("pe", "matmul", out=pv_[:, :], lhsT=W[:, kc, 256 + sub * 128:256 + (sub + 1) * 128],
                         rhs=HT[:, kc, :], start=(kc == 0), stop=(kc == 7))
                Yg, Yv = YE[(c % 2) * 2], YE[(c % 2) * 2 + 1]
                ffn_conv(pg_, Yg, UE[(c % 2) * 2], c)
                ffn_conv(pv_, Yv, UE[(c % 2) * 2 + 1], 22 + c)
                E.op("act", "activation", out=Yg[:], in_=Yg[:], func=AF.Silu)
                E.op("pool", "tensor_tensor", out=AT[:, c, :], in0=Yg[:], in1=Yv[:], op=ALU.mult)
        for hf in range(2):
            accs = [psum("acc") for _ in range(4)]
            for kg in range(3):
                W = get_w()
                nk = 8 if kg < 2 else 6
                for t in range(4):
                    for kk in range(nk):
                        c = 8 * kg + kk
                        E.op("pe", "matmul", out=accs[t][:, :], lhsT=AT[:, c, t * 128:(t + 1) * 128], rhs=W[:, kk, :],
                             start=(c == 0), stop=(c == 21), skip_group_check=True)
            for t in range(4):
                E.op("dve", "tensor_tensor", out=X1[t][:, hf * 512:(hf + 1) * 512], in0=accs[t][:, :],
                     in1=X1[t][:, hf * 512:(hf + 1) * 512], op=ALU.add)
        for t in range(4):
            E.op("act", "activation", out=JUNK2[:], in_=X1[t][:], func=AF.Square, accum_out=SS3[t][:])
            rstd_from_ss(SS3[t][:], RSTD3[t][:], 1024.0)
            ob = OUTB[t % 2]
            E.op("dve", "scalar_tensor_tensor", out=ob[:], in0=X1[t][:], scalar=RSTD3[t][:], in1=GFIN[:],
                 op0=ALU.mult, op1=ALU.mult)
            E.dma("pool", "d_out%d" % (t % 2), out=V(y[tok0 + t * 128:tok0 + (t + 1) * 128, :], "dram_y"), in_=ob[:])
        if I + 1 < NB:
            for t in range(4):
                E.dma("sp", "d_x%d" % t, out=X1[t][:],
                      in_=V(x[tok0 + 512 + t * 128:tok0 + 512 + (t + 1) * 128, :], "dram_x"))
        E.barrier()

    with nc.Block() as block:
        @block.sync
        def _(eng):
            E.replay("sp", eng)

        @block.tensor
        def _(eng):
            E.replay("pe", eng)

        @block.scalar
        def _(eng):
            E.replay("act", eng)

        @block.vector
        def _(eng):
            E.replay("dve", eng)

        @block.gpsimd
        def _(eng):
            E.replay("pool", eng)
            E.final_wait(eng, "pool")
    stack.close()
    return nc


def _rel_bucket(d):
    d = np.maximum(d, 0)
    large = 16 + (np.log(np.maximum(d, 1).astype(np.float32) / 16) / math.log(128 / 16) * 16).astype(np.int32)
    large = np.minimum(large, 31)
    return np.where(d < 16, d, large)


def host_consts(inp):
    f32 = np.float32
    c = {}
    c["gmix"] = np.ascontiguousarray(inp["norm_mix"].reshape(8, 128).T).astype(f32)
    c["gffn"] = np.ascontiguousarray(inp["norm_ffn"].reshape(8, 128).T).astype(f32)
    c["gfin"] = np.ascontiguousarray(np.broadcast_to(inp["norm_final"].reshape(1, 1024), (128, 1024))).astype(f32)
    mcw = inp["mlstm_conv_w"].reshape(4, 4, 128)
    c["mcw"] = np.ascontiguousarray(mcw.transpose(2, 1, 0).reshape(128, 16)).astype(f32)
    c["mcb"] = np.ascontiguousarray(inp["mlstm_conv_b"].reshape(4, 128).T).astype(f32)
    gb = np.concatenate([inp["i_bias"].reshape(4), inp["f_bias"].reshape(4)])
    c["gbias"] = np.ascontiguousarray(np.broadcast_to(gb.reshape(1, 8), (128, 8))).astype(f32)
    c["mnorm"] = np.ascontiguousarray(np.broadcast_to(inp["mlstm_norm"].reshape(1, 512), (128, 512))).astype(f32)
    c["gk"] = np.ascontiguousarray(np.broadcast_to(inp["idx_k_norm"].reshape(1, 64), (128, 64))).astype(f32)
    c["relb"] = np.ascontiguousarray(inp["rel_bias"]).astype(f32)
    c["b31"] = np.ascontiguousarray(inp["rel_bias"][31].reshape(8, 1)).astype(f32)
    oh = np.zeros((32, 384), f32)
    for k in range(384):
        d = k - 127
        if d >= 0:
            oh[int(_rel_bucket(np.array([d]))[0]), k] = 1.0
    c["oh"] = oh
    fw = inp["ffn_conv_w"].reshape(3, 44, 128)
    c["fcw"] = np.ascontiguousarray(fw.transpose(2, 1, 0).reshape(128, 132)).astype(f32)
    c["fcb"] = np.ascontiguousarray(inp["ffn_conv_b"].reshape(44, 128).T).astype(f32)
    c["ident"] = np.eye(128, dtype=f32)
    c["jrev"] = np.ascontiguousarray(np.eye(128, dtype=f32)[::-1])
    s = np.arange(128)[:, None]
    l = np.arange(128)[None, :]
    same = (s // 64) == (l // 64)
    tri = (same & (s <= l)).astype(f32)
    blk = same.astype(f32)
    blka = np.broadcast_to((s < 64), (128, 128)).astype(f32)
    blkb = np.broadcast_to((s >= 64), (128, 128)).astype(f32)
    c["tri4"] = np.ascontiguousarray(np.concatenate([tri, blk, blka, blkb], axis=1))
    q = np.arange(128)[:, None]
    kk = np.arange(128)[None, :]
    c["cmask"] = np.where(kk <= q, 0.0, NEG).astype(f32)
    c["hkc"] = np.ascontiguousarray(np.broadcast_to((0.5 ** (np.arange(NITER + 1) + 1)).reshape(1, NITER + 1), (128, NITER + 1))).astype(f32)
    return c


_NC_CACHE = {}


def kernel(**inputs):
    inp = {k: np.asarray(v) for k, v in inputs.items()}
    x = inp["x"]
    B, S, D = x.shape
    NB = S // 512
    topk = min(256, S // 4)
    key = (NB, topk)
    if key not in _NC_CACHE:
        _NC_CACHE[key] = build(NB, topk)
    nc = _NC_CACHE[key]
    c = host_consts(inp)
    shared = dict(c)
    shared["w_in"] = np.ascontiguousarray(inp["w_in"][0])
    shared["w_out"] = np.ascontiguousarray(inp["w_out"][0])
    shared["w_up"] = np.ascontiguousarray(inp["w_up"][0])
    shared["w_down"] = np.ascontiguousarray(inp["w_down"][0])
    in_maps = []
    for b in range(B):
        m = dict(shared)
        m["x"] = np.ascontiguousarray(x[b])
        in_maps.append(m)
    res = run_bass_kernel_spmd(nc, in_maps, core_ids=list(range(B)))
    out = np.stack([np.asarray(r["y"]) for r in res.results], axis=0).astype(np.float32)
    return out
```

```python
import math
import numpy as np
import ml_dtypes
import concourse.bass as bass
import concourse.mybir as mybir
from concourse.bass_utils import run_bass_kernel_spmd

F32 = mybir.dt.float32
BF16 = mybir.dt.bfloat16
AF = mybir.ActivationFunctionType
ALU = mybir.AluOpType
AX = mybir.AxisListType

EPS = 1e-6
import os
DBG_NOOVERLAP = os.environ.get('DBG_NOOVERLAP', '0') == '1'
DBG_NOX1 = os.environ.get('DBG_NOX1', '0') == '1'
NITER = 16
NEG = -1.0e30

WIN_GROUPS = [
    [(0, 512)],
    [(512, 512)],
    [(1024, 512)],
    [(1544, 512)],
    [(2056, 512)],
    [(2568, 512)],
    [(3080, 512)],
    [(3592, 72), (1536, 8)],
]


class V:
    __slots__ = ("ap", "key")

    def __init__(self, ap, key):
        if type(ap).__name__.endswith("Handle"):
            ap = ap[:]
        self.ap = ap
        self.key = key

    def __getitem__(self, idx):
        return V(self.ap[idx], self.key)

    def f(self, fn):
        return V(fn(self.ap), self.key)


class Em:
    ENG = ("pe", "act", "dve", "pool", "sp")

    def __init__(self, nc, stack):
        self.nc = nc
        self.stack = stack
        self.q = {e: [] for e in self.ENG}
        self.cnt = {}
        self.sems = {}
        self.lastw = {}
        self.readers = {}
        self.waited = {e: {} for e in self.ENG}
        for e in ("pe", "act", "dve", "pool"):
            self._sem(e)

    def _sem(self, key):
        if key not in self.sems:
            self.sems[key] = self.stack.enter_context(self.nc.semaphore("s_" + key))
            self.cnt[key] = 0
        return self.sems[key]

    def _emit(self, eng, meth, kw, reads, writes, semkey, inc):
        deps = {}

        def add(d):
            if d is None:
                return
            k, v = d
            if deps.get(k, 0) < v:
                deps[k] = v
        for b in reads:
            add(self.lastw.get(b))
        for b in writes:
            add(self.lastw.get(b))
            for k, v in self.readers.get(b, {}).items():
                add((k, v))
        waits = []
        for k, v in deps.items():
            if eng == "pe" and k == "pe":
                continue
            if self.waited[eng].get(k, 0) >= v:
                continue
            self.waited[eng][k] = v
            waits.append((k, v))
        self._sem(semkey)
        self.cnt[semkey] += inc
        val = self.cnt[semkey]
        self.q[eng].append((meth, kw, waits, semkey, inc))
        for b in reads:
            if b in writes:
                continue
            self.readers.setdefault(b, {})[semkey] = val
        for b in writes:
            self.lastw[b] = (semkey, val)
            self.readers[b] = {}
        return val

    def op(self, eng, meth, **kw):
        reads, writes, real = [], [], {}
        for k, v in kw.items():
            if isinstance(v, V):
                real[k] = v.ap
                keys = v.key if isinstance(v.key, (list, tuple)) else [v.key]
                if k in ("out", "accum_out"):
                    writes.extend(keys)
                else:
                    reads.extend(keys)
            else:
                real[k] = v
        return self._emit(eng, meth, real, reads, writes, eng, 1)

    def dma(self, eng, sem, out, in_, **extra):
        okeys = out.key if isinstance(out.key, (list, tuple)) else [out.key]
        ikeys = in_.key if isinstance(in_.key, (list, tuple)) else [in_.key]
        kw = dict(out=out.ap, in_=in_.ap)
        kw.update(extra)
        return self._emit(eng, "dma_start", kw, list(ikeys), list(okeys), sem, 16)

    def barrier(self):
        cur = dict(self.cnt)
        for e in self.ENG:
            waits = []
            for k, v in cur.items():
                if v == 0 or (e == k):
                    continue
                if self.waited[e].get(k, 0) >= v:
                    continue
                self.waited[e][k] = v
                waits.append((k, v))
            if waits:
                self.q[e].append((None, None, waits, None, 0))

    def replay(self, eng, engobj):
        for meth, kw, waits, semkey, inc in self.q[eng]:
            for k, v in waits:
                engobj.wait_ge(self.sems[k], v)
            if meth is None:
                continue
            ins = getattr(engobj, meth)(**kw)
            ins.then_inc(self.sems[semkey], inc)

    def final_wait(self, engobj, eng):
        for k, v in self.cnt.items():
            if v > 0 and self.waited[eng].get(k, 0) < v:
                engobj.wait_ge(self.sems[k], v)


def build(NB, topk):
    from contextlib import ExitStack
    S = NB * 512
    NT = NB * 4
    nc = bass.Bass("TRN2", target_bir_lowering=False)
    stack = ExitStack()
    E = Em(nc, stack)

    def din(name, shape, dt=F32):
        return nc.dram_tensor(name, list(shape), dt, kind="ExternalInput").ap()

    x = din("x", [S, 1024])
    w_in = din("w_in", [1024, 3664])
    w_out = din("w_out", [1024, 1024])
    w_up = din("w_up", [1024, 5632])
    w_down = din("w_down", [2816, 1024])
    d_gmix = din("gmix", [128, 8])
    d_gffn = din("gffn", [128, 8])
    d_gfin = din("gfin", [128, 1024])
    d_mcw = din("mcw", [128, 16])
    d_mcb = din("mcb", [128, 4])
    d_gbias = din("gbias", [128, 8])
    d_mnorm = din("mnorm", [128, 512])
    d_gk = din("gk", [128, 64])
    d_relb = din("relb", [32, 8])
    d_b31 = din("b31", [8, 1])
    d_oh = din("oh", [32, 384])
    d_fcw = din("fcw", [128, 132])
    d_fcb = din("fcb", [128, 44])
    d_ident = din("ident", [128, 128])
    d_tri4 = din("tri4", [128, 512])
    d_cmask = din("cmask", [128, 128])
    d_hkc = din("hkc", [128, NITER + 1])
    d_jrev = din("jrev", [128, 128])
    y = nc.dram_tensor("y", [S, 1024], F32, kind="ExternalOutput").ap()

    def dscratch(name, shape, dt=BF16):
        return nc.dram_tensor(name, list(shape), dt, kind="Internal").ap()

    sc_in = [dscratch("sc_in%d" % g, [128, 8, 512]) for g in range(8)]
    sc_out = [dscratch("sc_out%d" % g, [128, 8, 512]) for g in range(2)]
    sc_up = [dscratch("sc_up%d" % g, [128, 8, 512]) for g in range(11)]
    sc_dn = [dscratch("sc_dn%d" % g, [128, 8, 512]) for g in range(6)]
    tvd = dscratch("tvd", [8, 384])

    def sb(name, shape, dt):
        return stack.enter_context(nc.sbuf_tensor(name, list(shape), dt))

    KT = V(sb("KT", [128, 4, S], BF16), "KT")
    VP_t = sb("VP", [128, NT, 8, 65], BF16)
    IK = V(sb("IK", [128, S], BF16), "IK")
    WT = V(sb("WT", [128, 8, 256], BF16), "WT")
    WS = [V(sb("WS%d" % i, [128, 8, 512], BF16), "WS%d" % i) for i in range(3)]
    X1_t = sb("X1", [128, 4, 1024], F32)
    X1 = [V(X1_t[:, t, :], "X1_%d" % t) for t in range(4)]
    MIXT = sb("MIXT", [128, 8, 512], BF16)
    GMIX = V(sb("GMIX", [128, 8], F32), "c_gmix")
    GFFN = V(sb("GFFN", [128, 8], F32), "c_gffn")
    MCW = V(sb("MCW", [128, 16], F32), "c_mcw")
    MCB = V(sb("MCB", [128, 4], F32), "c_mcb")
    GBIAS = V(sb("GBIAS", [128, 8], F32), "c_gbias")
    GKB = V(sb("GKB", [128, 64], F32), "c_gk")
    FCW = V(sb("FCW", [128, 132], F32), "c_fcw")
    FCB = V(sb("FCB", [128, 44], F32), "c_fcb")
    IDENT = V(sb("IDENT", [128, 128], F32), "c_ident")
    IDENTB = V(sb("IDENTB", [128, 128], BF16), "c_identb")
    CMASK = V(sb("CMASK", [128, 128], F32), "c_cmask")
    HKC = V(sb("HKC", [128, NITER + 1], F32), "c_hkc")
    DG = V(sb("DG", [128, 8, 128], BF16), "DG")
    RELB = V(sb("RELB", [32, 8], F32), "c_relb")
    B31 = V(sb("B31", [8, 1], F32), "c_b31")
    OH = V(sb("OH", [32, 384], F32), "c_oh")
    ONES = V(sb("ONES", [128, 64], F32), "c_ones")
    CST_t = sb("CST", [128, 2, 129], F32)
    CB_t = sb("CB", [128, 4, 129], BF16)
    HM_t = sb("HM", [128, 4, 3], F32)
    HF_t = sb("HF", [128, 44, 2], F32)
    ST_t = sb("ST", [128, 320], F32)
    ARF = 18432
    AR = sb("ARENA", [128, ARF], F32)

    st_off = [0]

    def st(name, n):
        o = st_off[0]
        st_off[0] += n
        assert st_off[0] <= 320
        return V(ST_t[:, o:o + n], "st_" + name)

    def arv(off_bytes, nbytes, dt, key, pat=None, **kw):
        a = AR[:, off_bytes // 4:(off_bytes + nbytes) // 4]
        if dt == BF16:
            a = a.bitcast(BF16)
        if pat is not None:
            a = a.rearrange(pat, **kw)
        return V(a, key)

    PS = [stack.enter_context(nc.psum_tensor("ps%d" % i, [128, 512], F32)) for i in range(8)]
    psrot = {"mm": [0, 0, 4], "acc": [4, 0, 4], "qk": [0, 0, 6], "acc2": [6, 0, 2]}

    def psum(pool):
        b0, i, n = psrot[pool]
        psrot[pool][1] = (i + 1) % n
        k = b0 + i
        return V(PS[k][:, :], "ps%d" % k)

    def bfv(p):
        return V(p.ap.bitcast(BF16), p.key)

    for dst, src in ((GMIX, d_gmix), (GFFN, d_gffn), (MCW, d_mcw), (MCB, d_mcb), (GBIAS, d_gbias), (GKB, d_gk),
                     (FCW, d_fcw), (FCB, d_fcb), (IDENT, d_ident), (CMASK, d_cmask), (HKC, d_hkc), (RELB, d_relb),
                     (B31, d_b31), (OH, d_oh)):
        E.dma("sp", "d_" + dst.key, out=dst[:], in_=V(src, "dram_" + dst.key))
    E.op("dve", "tensor_copy", out=IDENTB[:], in_=IDENT[:])
    E._emit("dve", "memset", dict(ap=ONES.ap, constant=1.0), [], [ONES.key], "dve", 1)
    E._emit("pool", "memset", dict(ap=CST_t[:, :, :], constant=0.0), [], ["CST0", "CST1"], "pool", 1)
    E._emit("pool", "memset", dict(ap=CB_t[:, :, :], constant=0.0), [], ["CB0", "CB1", "CB2", "CB3"], "pool", 1)
    E._emit("pool", "memset", dict(ap=VP_t[:, :, :, 64:65], constant=1.0), [], ["VPones"], "pool", 1)

    TVS = arv(0, 768, BF16, "tvs")
    NB31 = st("nb31", 1)
    E.op("dve", "tensor_scalar", out=NB31[0:8, :], in0=B31[:], scalar1=-1.0, scalar2=None, op0=ALU.mult)
    pt = psum("mm")
    E.op("pe", "matmul", out=pt[0:8, 0:384], lhsT=RELB[:], rhs=OH[:], start=True, stop=True)
    E.op("act", "activation", out=TVS[0:8, :], in_=pt[0:8, 0:384], func=AF.Exp, bias=NB31[0:8, :], scale=1.0)
    E._emit("dve", "memset", dict(ap=TVS.ap[0:8, 0:127], constant=0.0), ["tvs"], ["tvs"], "dve", 1)
    TVD = V(tvd, "tvd")
    E.dma("sp", "d_tvd", out=TVD[:, :], in_=TVS[0:8, :])
    WTR = arv(1024, 4096, BF16, "WTR", "p (h c) -> p h c", h=8)
    JREVB = arv(5120, 256, BF16, "JREVB")
    JREVF = arv(5632, 512, F32, "JREVF")
    E.dma("sp", "d_jrev", out=JREVF[:], in_=V(d_jrev, "dram_jrev"))
    E.op("dve", "tensor_copy", out=JREVB[:], in_=JREVF[:])
    for h in range(8):
        src = bass.AP(tensor=tvd.tensor, offset=h * 384, ap=[[1, 128], [1, 256]])
        E.dma("sp", "d_wt", out=WTR[:, h, :], in_=V(src, "tvd"))
    for q4 in range(4):
        pw = psum("mm")
        E.op("pe", "matmul", out=pw[:, :], lhsT=JREVB[:],
             rhs=WTR[:, 2 * q4:2 * q4 + 2, :].f(lambda a: a.rearrange("p h c -> p (h c)")), start=True, stop=True)
        E.op("dve", "tensor_copy", out=WT[:, 2 * q4:2 * q4 + 2, :],
             in_=pw[:, :].f(lambda a: a.rearrange("p (h c) -> p h c", h=2)))
    E.barrier()

    STG = [arv(1024 + i * 16384, 16384, F32, "stg%d" % i, "p (a b) -> p a b", a=8) for i in range(2)]
    STB = [arv(1024 + 32768 + i * 8192, 8192, BF16, "stb%d" % i, "p (a b) -> p a b", a=8) for i in range(2)]
    groups = []
    w_in_r = w_in.rearrange("(c p) n -> p c n", p=128)
    w_out_r = w_out.rearrange("(c p) n -> p c n", p=128)
    w_up_r = w_up.rearrange("(c p) n -> p c n", p=128)
    w_dn_r = w_down.rearrange("(c p) n -> p c n", p=128)
    for g, pieces in enumerate(WIN_GROUPS):
        pcs, c0 = [], 0
        for (s0, n) in pieces:
            pcs.append((w_in_r[:, :, s0:s0 + n], c0, n))
            c0 += n
        groups.append((sc_in[g], "sc_in%d" % g, pcs, 8, c0, GMIX))
    for g in range(2):
        groups.append((sc_out[g], "sc_out%d" % g, [(w_out_r[:, :, g * 512:(g + 1) * 512], 0, 512)], 8, 512, None))
    for g in range(11):
        groups.append((sc_up[g], "sc_up%d" % g,
                       [(w_up_r[:, :, 256 * g:256 * g + 256], 0, 256),
                        (w_up_r[:, :, 2816 + 256 * g:2816 + 256 * g + 256], 256, 256)], 8, 512, GFFN))
    for hf in range(2):
        for kg in range(3):
            nk = 8 if kg < 2 else 6
            groups.append((sc_dn[hf * 3 + kg], "sc_dn%d" % (hf * 3 + kg),
                           [(w_dn_r[:, 8 * kg:8 * kg + nk, hf * 512:(hf + 1) * 512], 0, 512)], nk, 512, None))
    cast_engs = ["dve", "act"]
    for gi, (dst, dkey, pcs, kc, ncols, scl) in enumerate(groups):
        sl = gi % 2
        for (src, c0, n) in pcs:
            E.dma("sp", "d_stg%d" % sl, out=STG[sl][:, 0:kc, c0:c0 + n], in_=V(src, "dram_w"))
        eng = cast_engs[gi % 2]
        if scl is None:
            if eng == "act":
                E.op("act", "activation", out=STB[sl][:, 0:kc, 0:ncols], in_=STG[sl][:, 0:kc, 0:ncols], func=AF.Copy)
            else:
                E.op(eng, "tensor_copy", out=STB[sl][:, 0:kc, 0:ncols], in_=STG[sl][:, 0:kc, 0:ncols])
        else:
            for c in range(kc):
                if eng == "act":
                    E.op("act", "activation", out=STB[sl][:, c, 0:ncols], in_=STG[sl][:, c, 0:ncols],
                         func=AF.Copy, scale=scl[:, c:c + 1])
                else:
                    E.op(eng, "tensor_scalar", out=STB[sl][:, c, 0:ncols], in0=STG[sl][:, c, 0:ncols],
                         scalar1=scl[:, c:c + 1], scalar2=None, op0=ALU.mult)
        E.dma("pool", "d_stb%d" % sl, out=V(dst[:, 0:kc, 0:ncols], dkey), in_=STB[sl][:, 0:kc, 0:ncols])
    E.barrier()

    wseq = []
    for I in range(NB):
        for g in range(8):
            wseq.append((sc_in[g], "sc_in%d" % g, 8, 512 if g < 7 else 80))
        for g in range(2):
            wseq.append((sc_out[g], "sc_out%d" % g, 8, 512))
        for g in range(11):
            wseq.append((sc_up[g], "sc_up%d" % g, 8, 512))
        for g in range(6):
            wseq.append((sc_dn[g], "sc_dn%d" % g, 8 if g % 3 < 2 else 6, 512))
    wstate = {"loaded": 0, "next": 0}

    def get_w():
        idx = wstate["next"]
        wstate["next"] += 1
        while wstate["loaded"] <= idx + 2 and wstate["loaded"] < len(wseq):
            li = wstate["loaded"]
            src, skey, kc, ncols = wseq[li]
            slot = li % 3
            E.dma("sp", "d_ws%d" % slot, out=WS[slot][:, 0:kc, 0:ncols], in_=V(src[:, 0:kc, 0:ncols], skey))
            wstate["loaded"] += 1
        return WS[idx % 3]

    HB = arv(0, 2048, BF16, "HB")
    HBA = [HB, arv(38400, 2048, BF16, "HBb")]
    HBE = [HB, arv(67584, 2048, BF16, "HBc")]
    HT = arv(2048, 8192, BF16, "HT", "p (a b) -> p a b", a=8)
    QKT = arv(10240, 4096, BF16, "QKT", "p (a b) -> p a b", a=4)
    YC = [arv(14336 + i * 2048, 2048, F32, "YC%d" % i) for i in range(2)]
    VM_full = AR[:, 18432 // 4:(18432 + 4160) // 4].bitcast(BF16)[:, 0:2064].rearrange("p (t h d) -> p t h d", t=4, h=4)
    VM = V(VM_full, "VM")
    OG = arv(22592, 4096, BF16, "OG", "p (a b) -> p a b", a=4)
    TRI4 = arv(26688, 2048, F32, "TRI4")
    MNORM = arv(28736, 2048, F32, "MNORM")
    IKN = arv(30784, 128, BF16, "IKN")
    KW = arv(30912, 512, BF16, "KW", "p (h d) -> p h d", h=4)
    PTB = [arv(37056 + i * 256, 256, BF16, "PT%d" % i) for i in range(4)]
    HMO = arv(31936, 1024, BF16, "HMO")
    JUNK = arv(32960, 4096, F32, "JUNK")
    AQT = arv(65536, 4096, BF16, "AQT", "p (a b) -> p a b", a=4)
    IQT = arv(69632, 4096, BF16, "IQT", "p (a b) -> p a b", a=4)
    SCK = [V(AR[:, (0 + kc * 2048) // 4:(0 + (kc + 1) * 2048) // 4], "SC%d" % kc) for kc in range(8)]
    SCall = V(AR[:, 0:4096], ["SC%d" % kc for kc in range(8)])
    MQ = arv(16384, 8192, BF16, "MQ")
    MT = arv(24576, 32768, BF16, "MT", "p (j q) -> p j q", j=32)
    EP = [arv(57344 + i * 1024, 1024, BF16, "EP%d" % i) for i in range(8)]
    FT = [arv(i * 2048, 2048, F32, "FT%d" % i) for i in range(4)]
    RB = [arv(61440 + i * 2048, 2048, F32, "RB%d" % i) for i in range(2)]
    RBF = [arv(61440 + i * 1024, 1024, BF16, "RBF%d" % i) for i in range(4)]
    YE = [arv(10240 + i * 2048, 2048, F32, "YE%d" % i) for i in range(4)]
    AT = arv(18432, 22528, BF16, "AT", "p (c q) -> p c q", c=22)
    GFIN = arv(40960, 4096, F32, "GFIN")
    OUTB = [arv(45056 + i * 4096, 4096, F32, "OUTB%d" % i) for i in range(2)]
    JUNK2 = arv(53248, 4096, F32, "JUNK2")
    UE = [arv(57344 + i * 2064, 2056, F32, "UE%d" % i) for i in range(4)]

    SS = [st("ss%d" % t_, 1) for t_ in range(4)]
    RSTD = [st("rstd%d" % t_, 1) for t_ in range(4)]
    SSK = [st("ssk%d" % t_, 1) for t_ in range(4)]
    RK = [st("rk%d" % t_, 1) for t_ in range(4)]
    GATES = [st("gates%d" % t, 8) for t in range(4)]
    WQ = [st("wq%d" % t, 8) for t in range(4)]
    E1 = st("e1", 4)
    NLF = st("nlf", 4)
    GS = st("gs", 16)
    EB8 = st("eb8", 4)
    T1 = st("t1", 4)
    EK = st("ek", 4)
    T2 = st("t2", 4)
    WKs = st("wk", 4)
    EG = st("eg", 8)
    DN = st("dn", 4)
    RR = st("rr", 4)
    MS = st("ms", 4)
    TT = st("tt", 4)
    SCL = st("scl", 4)
    MX = st("mx", 1)
    MN2 = [st("mn0", 1), st("mn1", 1)]
    W0 = st("w0", 1)
    NHK = st("nhk", NITER + 1)
    NM = st("nm", 1)
    SACC = st("sacc", 1)
    SG = st("sg", 1)
    LO = st("lo", 1)
    MID = st("mid", 1)
    CNT = st("cnt", 1)
    TMP = st("tmp", 1)
    SS2 = [st("ss2%d" % t_, 1) for t_ in range(4)]
    RSTD2 = [st("rstd2%d" % t_, 1) for t_ in range(4)]
    SS3 = [st("ss3%d" % t_, 1) for t_ in range(4)]
    RSTD3 = [st("rstd3%d" % t_, 1) for t_ in range(4)]

    CST = [V(CST_t[:, pp, :], "CST%d" % pp) for pp in range(2)]
    CBh = [V(CB_t[:, h, :], "CB%d" % h) for h in range(4)]
    HMh = [V(HM_t[:, c, :], "HM%d" % c) for c in range(4)]
    HFh = [V(HF_t[:, c, :], "HF%d" % c) for c in range(44)]
    MIXc = [V(MIXT[:, c, :], "MIX%d" % c) for c in range(8)]
    MIX03 = V(MIXT[:, 0:4, :], ["MIX0", "MIX1", "MIX2", "MIX3"])

    def rstd_from_ss(ssv, rv, n):
        E.op("dve", "tensor_scalar", out=rv, in0=ssv, scalar1=1.0 / n, scalar2=EPS, op0=ALU.mult, op1=ALU.add)
        E.op("act", "activation", out=rv, in_=rv, func=AF.Sqrt)
        E.op("dve", "reciprocal", out=rv, in_=rv)

    def transposes_to(dst_view, src, nchunk):
        p = psum("mm")
        pb = bfv(p)
        for c in range(nchunk):
            E.op("pe", "transpose", out=pb[:, c * 128:(c + 1) * 128], in_=src[:, c * 128:(c + 1) * 128],
                 identity=IDENTB[:])
        E.op("act", "activation", out=dst_view,
             in_=pb[:, 0:nchunk * 128].f(lambda a: a.rearrange("p (c q) -> p c q", c=nchunk)), func=AF.Copy)

    rr = {"i": 0}

    def alt(*engs):
        rr["i"] += 1
        return engs[rr["i"] % len(engs)]

    for I in range(NB):
        tok0 = I * 512
        E.dma("sp", "d_tri4", out=TRI4[:], in_=V(d_tri4, "dram_tri4"))
        E.dma("sp", "d_mnorm", out=MNORM[:], in_=V(d_mnorm, "dram_mnorm"))
        if I == 0:
            for t in range(4):
                E.dma("sp", "d_x%d" % t, out=X1[t][:], in_=V(x[tok0 + t * 128:tok0 + (t + 1) * 128, :], "dram_x"))
        for t in range(4):
            E.op("act", "activation", out=JUNK[:], in_=X1[t][:], func=AF.Square, accum_out=SS[t][:])
            rstd_from_ss(SS[t][:], RSTD[t][:], 1024.0)
            E.op("dve", "tensor_scalar", out=HBA[t % 2][:], in0=X1[t][:], scalar1=RSTD[t][:], scalar2=None, op0=ALU.mult)
            transposes_to(HT[:, :, t * 128:(t + 1) * 128], HBA[t % 2], 8)

        def fm_group(W, evac):
            for cc in range(4):
                p = psum("mm")
                for c in range(8):
                    E.op("pe", "matmul", out=p[:, :], lhsT=W[:, c, cc * 128:(cc + 1) * 128], rhs=HT[:, c, :],
                         start=(c == 0), stop=(c == 7))
                evac(cc, p)

        def tm_group(W, ncols, evac):
            for t in range(4):
                p = psum("mm")
                for c in range(8):
                    E.op("pe", "matmul", out=p[:, 0:ncols], lhsT=HT[:, c, t * 128:(t + 1) * 128], rhs=W[:, c, 0:ncols],
                         start=(c == 0), stop=(c == 7))
                evac(t, p)

        def ev_g0(cc, p):
            Y = YC[cc % 2]
            E.op("act", "activation", out=Y[:], in_=p[:, :], func=AF.Identity, scale=MCW[:, cc * 4 + 3:cc * 4 + 4],
                 bias=MCB[:, cc:cc + 1])
            for j, sh in ((2, 1), (1, 2), (0, 3)):
                E.op("dve", "scalar_tensor_tensor", out=Y[:, sh:512], in0=p[:, 0:512 - sh],
                     scalar=MCW[:, cc * 4 + j:cc * 4 + j + 1], in1=Y[:, sh:512], op0=ALU.mult, op1=ALU.add)
            if I > 0:
                H = HMh[cc]
                E.op("dve", "scalar_tensor_tensor", out=Y[:, 0:3], in0=H[:, 0:3], scalar=MCW[:, cc * 4:cc * 4 + 1],
                     in1=Y[:, 0:3], op0=ALU.mult, op1=ALU.add)
                E.op("dve", "scalar_tensor_tensor", out=Y[:, 0:2], in0=H[:, 1:3], scalar=MCW[:, cc * 4 + 1:cc * 4 + 2],
                     in1=Y[:, 0:2], op0=ALU.mult, op1=ALU.add)
                E.op("dve", "scalar_tensor_tensor", out=Y[:, 0:1], in0=H[:, 2:3], scalar=MCW[:, cc * 4 + 2:cc * 4 + 3],
                     in1=Y[:, 0:1], op0=ALU.mult, op1=ALU.add)
            E.op("act", "activation", out=HMh[cc][:], in_=p[:, 509:512], func=AF.Copy)
            E.op("act", "activation", out=QKT[:, cc, :], in_=Y[:], func=AF.Silu)
        fm_group(get_w(), ev_g0)

        def ev_g1(t, p):
            E.op("act", "activation", out=VM[:, t, :, 0:128],
                 in_=p[:, :].f(lambda a: a.rearrange("p (h d) -> p h d", h=4)), func=AF.Copy)
        E._emit("pool", "memset", dict(ap=VM.ap[:, :, :, 128:129], constant=1.0), [], ["VM"], "pool", 1)
        tm_group(get_w(), 512, ev_g1)

        def ev_g2(t, p):
            E.op("act", "activation", out=OG[:, t, :], in_=p[:, :], func=AF.Sigmoid)
            E.op("pool", "tensor_tensor", out=OG[:, t, :], in0=OG[:, t, :], in1=MNORM[:], op=ALU.mult)
        tm_group(get_w(), 512, ev_g2)

        def ev_g3(cc, p):
            E.op("act", "activation", out=AQT[:, cc, :], in_=p[:, :], func=AF.Copy, scale=0.125)
        fm_group(get_w(), ev_g3)

        def ev_g4(cc, p):
            E.op("dve", "tensor_copy", out=KT[:, cc, tok0:tok0 + 512], in_=p[:, :])
        fm_group(get_w(), ev_g4)

        def ev_g5(t, p):
            E.op("act", "activation", out=V(VP_t[:, I * 4 + t, :, 0:64], "VP"),
                 in_=p[:, :].f(lambda a: a.rearrange("p (h d) -> p h d", h=8)), func=AF.Copy)
        tm_group(get_w(), 512, ev_g5)

        def ev_g6(cc, p):
            E.op("dve", "tensor_copy", out=IQT[:, cc, :], in_=p[:, :])
        fm_group(get_w(), ev_g6)

        def ev_g7(t, p):
            E.op("act", "activation", out=JUNK[:, 0:64], in_=p[:, 0:64], func=AF.Square, accum_out=SSK[t][:])
            rstd_from_ss(SSK[t][:], RK[t][:], 64.0)
            E.op("dve", "scalar_tensor_tensor", out=IKN[:], in0=p[:, 0:64], scalar=RK[t][:], in1=GKB[:],
                 op0=ALU.mult, op1=ALU.mult)
            E.op("dve", "tensor_scalar", out=WQ[t][:], in0=p[:, 64:72], scalar1=float(8 ** -0.5 * 64 ** -0.5),
                 scalar2=None, op0=ALU.mult)
            E.op("dve", "tensor_tensor", out=GATES[t][:], in0=p[:, 72:80], in1=GBIAS[:], op=ALU.add)
            p2 = psum("mm")
            pb = bfv(p2)
            E.op("pe", "transpose", out=pb[0:64, 0:128], in_=IKN[:], identity=IDENTB[:])
            c0 = tok0 + t * 128
            E.op("dve", "tensor_copy", out=IK[0:64, c0:c0 + 128], in_=pb[0:64, 0:128])
            E.op("dve", "tensor_copy", out=IK[64:128, c0:c0 + 128], in_=pb[0:64, 0:128])
        tm_group(get_w(), 80, ev_g7)

        for t in range(4):
            c0 = t * 128
            pk = psum("mm")
            pkb = bfv(pk)
            for pp in range(2):
                E.op("pe", "transpose", out=pkb[:, pp * 128:(pp + 1) * 128], in_=QKT[:, 2 + pp, c0:c0 + 128],
                     identity=IDENTB[:])
            G = GATES[t]
            E.op("act", "activation", out=E1[:], in_=G[:, 4:8], func=AF.Exp, scale=-1.0)
            E.op("dve", "tensor_scalar", out=E1[:], in0=E1[:], scalar1=1.0, scalar2=None, op0=ALU.add)
            E.op("act", "activation", out=NLF[:], in_=E1[:], func=AF.Ln)
            pg = psum("mm")
            for k in range(4):
                E.op("pe", "matmul", out=pg[:, 4 * k:4 * k + 4], lhsT=TRI4[:, 128 * k:128 * (k + 1)], rhs=NLF[:],
                     start=True, stop=True)
            E.op("dve", "tensor_copy", out=GS[:], in_=pg[:, 0:16])
            E.op("act", "activation", out=EB8[:], in_=GS[:, 0:4], func=AF.Exp, scale=-1.0, bias=-math.log(8.0))
            E.op("dve", "tensor_tensor", out=T1[:], in0=G[:, 0:4], in1=GS[:, 0:4], op=ALU.add)
            E.op("act", "activation", out=EK[:], in_=T1[:], func=AF.Exp)
            E.op("dve", "tensor_tensor", out=T2[:], in0=T1[:], in1=GS[:, 4:8], op=ALU.subtract)
            E.op("act", "activation", out=WKs[:], in_=T2[:], func=AF.Exp)
            E.op("act", "activation", out=EG[:], in_=GS[:, 8:16], func=AF.Exp, scale=-1.0)
            E.op("dve", "tensor_tensor", out=KW[:],
                 in0=pkb[:, 0:256].f(lambda a: a.rearrange("p (h d) -> p h d", h=4)),
                 in1=WKs[:, 0:4].f(lambda a: a.unsqueeze(2).to_broadcast([128, 4, 64])), op=ALU.mult)
            npss = [psum("acc") for _ in range(4)]
            for h in range(4):
                pp, hh = h // 2, h % 2
                base, o = 64 * hh, 0
                pr = psum("mm")
                E.op("pe", "matmul", out=pr[:, 0:128], lhsT=QKT[base:base + 64, 2 + pp, c0:c0 + 128],
                     rhs=QKT[base:base + 64, pp, c0:c0 + 128], start=True, stop=True)
                PT = PTB[h]
                E.op("dve", "scalar_tensor_tensor", out=PT[:], in0=pr[:, 0:128], scalar=EK[:, h:h + 1],
                     in1=TRI4[:, 0:128], op0=ALU.mult, op1=ALU.mult)
                E.op("pe", "matmul", out=npss[h][:, o:o + 129], lhsT=PT[:], rhs=VM[:, t, h, :], start=True, stop=False,
                     skip_group_check=True)
            for ch in range(2):
                r0 = 64 * ch
                pkvs = []
                for h in range(4):
                    pp, hh = h // 2, h % 2
                    base, o = 64 * hh, 0
                    E.op("pe", "matmul", out=npss[h][r0:r0 + 64, o:o + 129], lhsT=QKT[:, pp, c0 + r0:c0 + r0 + 64],
                         rhs=CBh[h][:], start=False, stop=(ch == 1), skip_group_check=True)
                    pkv = psum("mm")
                    E.op("pe", "matmul", out=pkv[base:base + 64, 0:129], lhsT=KW[r0:r0 + 64, h, :],
                         rhs=VM[r0:r0 + 64, t, h, :], start=True, stop=True)
                    pkvs.append(pkv)
                for h in range(4):
                    pp, hh = h // 2, h % 2
                    base = 64 * hh
                    E.op("dve", "scalar_tensor_tensor", out=CST[pp][base:base + 64, :], in0=CST[pp][base:base + 64, :],
                         scalar=EG[base:base + 64, 4 * ch + h:4 * ch + h + 1], in1=pkvs[h][base:base + 64, 0:129],
                         op0=ALU.mult, op1=ALU.add)
                for h in range(4):
                    pp, hh = h // 2, h % 2
                    base = 64 * hh
                    E.op("act", "activation", out=CBh[h][base:base + 64, :], in_=CST[pp][base:base + 64, :],
                         func=AF.Copy)
            for h in range(4):
                E.op("dve", "tensor_tensor", out=DN[:, h:h + 1], in0=npss[h][:, 128:129], in1=EB8[:, h:h + 1], op=ALU.mult)
            E.op("act", "activation", out=DN[:], in_=DN[:], func=AF.Abs)
            E.op("dve", "tensor_scalar", out=DN[:], in0=DN[:], scalar1=1.0, scalar2=None, op0=ALU.max)
            E.op("dve", "reciprocal", out=DN[:], in_=DN[:])
            E.op("dve", "tensor_tensor", out=RR[:], in0=DN[:], in1=EB8[:, 0:4], op=ALU.mult)
            for h in range(4):
                E.op("act", "activation", out=JUNK[:, h * 128:(h + 1) * 128], in_=npss[h][:, 0:128], func=AF.Square,
                     accum_out=MS[:, h:h + 1])
            E.op("dve", "tensor_tensor", out=TT[:], in0=RR[:], in1=RR[:], op=ALU.mult)
            E.op("dve", "tensor_tensor", out=TT[:], in0=TT[:], in1=MS[:], op=ALU.mult)
            E.op("dve", "tensor_scalar", out=TT[:], in0=TT[:], scalar1=1.0 / 128.0, scalar2=EPS, op0=ALU.mult,
                 op1=ALU.add)
            E.op("act", "activation", out=TT[:], in_=TT[:], func=AF.Sqrt)
            E.op("dve", "reciprocal", out=TT[:], in_=TT[:])
            E.op("dve", "tensor_tensor", out=SCL[:], in0=TT[:], in1=RR[:], op=ALU.mult)
            for h in range(4):
                E.op("dve", "scalar_tensor_tensor", out=HMO[:, h * 128:(h + 1) * 128],
                     in0=npss[h][:, 0:128], scalar=SCL[:, h:h + 1], in1=OG[:, t, h * 128:(h + 1) * 128],
                     op0=ALU.mult, op1=ALU.mult)
            transposes_to(MIX03[:, :, c0:c0 + 128], HMO, 4)

        E.barrier()
        nkt = 4 * I + 4
        X1flat = X1_t[:].rearrange("p a b -> p (a b)")
        SCB = [
            ([SCK[kc] for kc in range(8)], SCall),
            ([V(X1_t[:, kc // 2, (kc % 2) * 512:(kc % 2) * 512 + 512], "X1_%d" % (kc // 2)) for kc in range(8)],
             V(X1flat, ["X1_0", "X1_1", "X1_2", "X1_3"])),
        ]

        def score_gen(t):
            i = 4 * I + t
            n = 128 * (i + 1)
            nch = (n + 511) // 512
            sck, scall = SCB[0 if DBG_NOX1 else t % 2]
            for h in range(8):
                E.op("dve", "tensor_scalar", out=DG[:, h, :], in0=IDENT[:], scalar1=WQ[t][:, h:h + 1], scalar2=None,
                     op0=ALU.mult)
            dk = i // 4
            for kc in range(nch):
                w = min(512, n - 512 * kc)
                pacc = psum("acc")
                pis = {}

                def emit_isc(h, kc=kc, w=w):
                    base, pp = 64 * (h % 2), h // 2
                    p = psum("mm")
                    E.op("pe", "matmul", out=p[:, 0:w], lhsT=IQT[base:base + 64, pp, t * 128:(t + 1) * 128],
                         rhs=IK[base:base + 64, 512 * kc:512 * kc + w], start=True, stop=True)
                    pis[h] = p
                for h in range(3):
                    emit_isc(h)
                for h in range(8):
                    if h + 3 < 8:
                        emit_isc(h + 3)
                    p = pis.pop(h)
                    R = RBF[h % 4]
                    if h % 2 == 0:
                        E.op("act", "activation", out=R[:, 0:w], in_=p[:, 0:w], func=AF.Relu)
                    else:
                        E.op("dve", "tensor_scalar", out=R[:, 0:w], in0=p[:, 0:w], scalar1=0.0, scalar2=None, op0=ALU.max)
                    E.op("pe", "matmul", out=pacc[:, 0:w], lhsT=DG[:, h, :], rhs=R[:, 0:w], start=(h == 0),
                         stop=(h == 7), skip_group_check=True)
                    if h == 7:
                        E.op("dve", "tensor_copy", out=sck[kc][:, 0:w], in_=pacc[:, 0:w])
                    yield (h == 7)
            if n > topk:
                E.op("dve", "tensor_reduce", out=MN2[t % 2][:], in_=scall[:, 0:n], axis=AX.X, op=ALU.min)
            E.op("dve", "tensor_tensor", out=sck[dk][:, (i % 4) * 128:(i % 4) * 128 + 128],
                 in0=sck[dk][:, (i % 4) * 128:(i % 4) * 128 + 128], in1=CMASK[:], op=ALU.add)
            yield True

        def bisect_gen(t):
            i = 4 * I + t
            n = 128 * (i + 1)
            sck, scall = SCB[0 if DBG_NOX1 else t % 2]
            MNt = MN2[t % 2]
            if n > topk:
                E.op("dve", "tensor_reduce", out=MX[:], in_=scall[:, 0:n], axis=AX.X, op=ALU.max)
                E.op("dve", "tensor_tensor", out=W0[:], in0=MX[:], in1=MNt[:], op=ALU.subtract)
                E.op("dve", "tensor_scalar", out=W0[:], in0=W0[:], scalar1=1.0001, scalar2=1e-6, op0=ALU.mult, op1=ALU.add)
                E.op("dve", "tensor_scalar", out=NHK[:], in0=HKC[:], scalar1=W0[:], scalar2=-1.0, op0=ALU.mult, op1=ALU.mult)
                E.op("dve", "scalar_tensor_tensor", out=NM[:], in0=MNt[:], scalar=-1.0, in1=NHK[:, 0:1], op0=ALU.mult,
                     op1=ALU.add)
                cthr = float(2 * topk - n) - 0.5
                split = n >= 1536
                if split:
                    nA = ((int(0.56 * n) + 127) // 128) * 128
                    nB = n - nA
                    MQa = V(MQ.ap[:, 0:nA], "MQa")
                    MQb = V(MQ.ap[:, nA:n], "MQb")
                    E.op("dve", "tensor_scalar", out=MID[:], in0=NM[:], scalar1=-1.0, scalar2=None, op0=ALU.mult)
                yield
                for k in range(NITER):
                    if split:
                        E.op("act", "activation", out=MQa, in_=scall[:, 0:nA], func=AF.Sign, bias=NM[:], scale=1.0,
                             accum_out=SACC[:])
                        E.op("dve", "tensor_scalar", out=MQb, in0=scall[:, nA:n], scalar1=MID[:], scalar2=None,
                             op0=ALU.is_ge, op1=ALU.add, accum_out=CNT[:])
                        yield
                        E.op("dve", "tensor_scalar", out=TMP[:], in0=CNT[:], scalar1=2.0, scalar2=-(float(nB) + cthr),
                             op0=ALU.mult, op1=ALU.add)
                        E.op("act", "activation", out=SG[:], in_=SACC[:], func=AF.Sign, bias=TMP[:], scale=1.0)
                    else:
                        E.op("act", "activation", out=MQ[:, 0:n], in_=scall[:, 0:n], func=AF.Sign, bias=NM[:], scale=1.0,
                             accum_out=SACC[:])
                        yield
                        E.op("act", "activation", out=SG[:], in_=SACC[:], func=AF.Sign, bias=-cthr, scale=1.0)
                    E.op("act", "activation", out=NM[:], in_=SG[:], func=AF.Identity, scale=NHK[:, k + 1:k + 2], bias=NM[:])
                    if split and k + 1 < NITER:
                        E.op("dve", "tensor_scalar", out=MID[:], in0=NM[:], scalar1=-1.0, scalar2=None, op0=ALU.mult)
                E.op("act", "activation", out=LO[:], in_=NM[:], func=AF.Identity, scale=-1.0, bias=NHK[:, NITER:NITER + 1])
                E.op("dve", "tensor_scalar", out=V(MQ.ap[:, 0:n], ["MQ", "MQa", "MQb"]), in0=scall[:, 0:n], scalar1=LO[:],
                     scalar2=None, op0=ALU.is_ge)
            else:
                E.op("dve", "tensor_scalar", out=MQ[:, 0:n], in0=scall[:, 0:n], scalar1=-1.0e29, scalar2=None,
                     op0=ALU.is_ge)
            if t < 3:
                E._emit("pool", "memset", dict(ap=MQ.ap[:, n:128 * nkt], constant=0.0), [], ["MQ"], "pool", 1)
            yield "pre_transpose"
            for jg in range((nkt + 3) // 4):
                p = psum("mm")
                pb = bfv(p)
                for jj in range(4):
                    j = 4 * jg + jj
                    E.op("pe", "transpose", out=pb[:, jj * 128:(jj + 1) * 128], in_=MQ[:, 128 * j:128 * (j + 1)],
                         identity=IDENTB[:])
                E.op("dve", "tensor_copy", out=MT[:, 4 * jg:4 * jg + 4, t * 128:(t + 1) * 128],
                     in_=pb[:, 0:512].f(lambda a: a.rearrange("p (j q) -> p j q", j=4)))
            yield

        for _ in score_gen(0):
            pass
        for t in range(4):
            bg = bisect_gen(t)
            sg = score_gen(t + 1) if t + 1 < 4 else None
            nb_steps = NITER + 2
            ns_steps = (8 * ((128 * (4 * I + t + 2) + 511) // 512) + 1) if sg is not None else 0
            ratio = float(ns_steps) / nb_steps
            if DBG_NOOVERLAP:
                ratio = 0.0
            accr = 0.0
            state = {"sg": sg, "bnd": True}

            def adv():
                try:
                    state["bnd"] = bool(next(state["sg"]))
                except StopIteration:
                    state["sg"] = None
                    state["bnd"] = True
            for tag in bg:
                if tag == "pre_transpose":
                    while state["sg"] is not None and not state["bnd"]:
                        adv()
                    continue
                accr += ratio
                while state["sg"] is not None and accr >= 1.0:
                    accr -= 1.0
                    adv()
            while state["sg"] is not None:
                adv()
        for t in range(4):
            E.dma("sp", "d_x%d" % t, out=X1[t][:], in_=V(x[tok0 + t * 128:tok0 + (t + 1) * 128, :], "dram_x"))
        for pp in range(4):
            accs = [psum("acc2"), psum("acc2")]
            qk = {}

            def emit_qk(j, pp=pp):
                for hh in range(2):
                    base = 64 * hh
                    p = psum("qk")
                    E.op("pe", "matmul", out=p[:, :], lhsT=KT[base:base + 64, pp, 128 * j:128 * (j + 1)],
                         rhs=AQT[base:base + 64, pp, :], start=True, stop=True)
                    qk[(j, hh)] = p
            for j in range(min(2, nkt)):
                emit_qk(j)
            for j in range(nkt):
                if j + 2 < nkt:
                    emit_qk(j + 2)
                for hh in range(2):
                    h = 2 * pp + hh
                    p = qk.pop((j, hh))
                    e = EP[(2 * j + hh) % 8]
                    E.op("act", "activation", out=e[:], in_=p[:, :], func=AF.Exp)
                    E.op("dve", "tensor_tensor", out=e[:], in0=e[:], in1=MT[:, j, :], op=ALU.mult)
                    r = j - 4 * I
                    if r >= -1:
                        for tq, v in ((r, 0), (r + 1, 1)):
                            if 0 <= tq <= 3:
                                E.op("dve", "tensor_tensor", out=e[:, tq * 128:(tq + 1) * 128],
                                     in0=e[:, tq * 128:(tq + 1) * 128], in1=WT[:, h, v * 128:(v + 1) * 128], op=ALU.mult)
                    E.op("pe", "matmul", out=accs[hh][0:65, :], lhsT=V(VP_t[:, j, h, :], ["VP", "VPones"]), rhs=e[:],
                         start=(j == 0), stop=(j == nkt - 1))
            for hh in range(2):
                base = 64 * hh
                acc = accs[hh]
                E.op("dve", "reciprocal", out=FT[2 * hh][64:65, :], in_=acc[64:65, :])
                pbc = psum("qk")
                E.op("pe", "matmul", out=pbc[0:64, :], lhsT=ONES[64:65, 0:64], rhs=FT[2 * hh][64:65, :], start=True,
                     stop=True)
                E.op("act", "activation", out=FT[2 * hh + 1][0:64, :], in_=pbc[0:64, :], func=AF.Copy)
                E.op("dve", "tensor_tensor", out=MIXc[4 + pp][base:base + 64, :], in0=acc[0:64, :],
                     in1=FT[2 * hh + 1][0:64, :], op=ALU.mult)

        E.barrier()
        E.dma("sp", "d_gfin", out=GFIN[:], in_=V(d_gfin, "dram_gfin"))
        for hf in range(2):
            W = get_w()
            for t in range(4):
                p = psum("mm")
                for c in range(8):
                    E.op("pe", "matmul", out=p[:, :], lhsT=MIXc[c][:, t * 128:(t + 1) * 128], rhs=W[:, c, :],
                         start=(c == 0), stop=(c == 7))
                E.op("dve", "tensor_tensor", out=X1[t][:, hf * 512:(hf + 1) * 512], in0=p[:, :],
                     in1=X1[t][:, hf * 512:(hf + 1) * 512], op=ALU.add)
        for t in range(4):
            E.op("act", "activation", out=JUNK2[:], in_=X1[t][:], func=AF.Square, accum_out=SS2[t][:])
            rstd_from_ss(SS2[t][:], RSTD2[t][:], 1024.0)
            E.op("dve", "tensor_scalar", out=HBE[t % 2][:], in0=X1[t][:], scalar1=RSTD2[t][:], scalar2=None, op0=ALU.mult)
            transposes_to(HT[:, :, t * 128:(t + 1) * 128], HBE[t % 2], 8)

        def ffn_conv(p, Y, U, ci):
            E.op("act", "activation", out=Y[:], in_=p[:, :], func=AF.Identity, scale=FCW[:, 3 * ci + 2:3 * ci + 3],
                 bias=FCB[:, ci:ci + 1])
            E.op("act", "activation", out=U[:, 2:514], in_=p[:, :], func=AF.Copy)
            if I > 0:
                E.op("act", "activation", out=U[:, 0:2], in_=HFh[ci][:], func=AF.Copy)
            else:
                E._emit("act", "memzero", dict(ap=U.ap[:, 0:2]), [], [U.key], "act", 1)
            E.op("act", "activation", out=HFh[ci][:], in_=U[:, 512:514], func=AF.Copy)
            E.op("dve", "scalar_tensor_tensor", out=Y[:], in0=U[:, 1:513], scalar=FCW[:, 3 * ci + 1:3 * ci + 2], in1=Y[:],
                 op0=ALU.mult, op1=ALU.add)
            E.op("dve", "scalar_tensor_tensor", out=Y[:], in0=U[:, 0:512], scalar=FCW[:, 3 * ci:3 * ci + 1], in1=Y[:],
                 op0=ALU.mult, op1=ALU.add)

        for g in range(11):
            W = get_w()
            for sub in range(2):
                c = 2 * g + sub
                pg_ = psum("mm")
                pv_ = psum("mm")
                for kc in range(8):
                    E.op("pe", "matmul", out=pg_[:, :], lhsT=W[:, kc, sub * 128:(sub + 1) * 128], rhs=HT[:, kc, :],
                         start=(kc == 0), stop=(kc == 7))
                for kc in range(8):
                    E.op("pe", "matmul", out=pv_[:, :], lhsT=W[:, kc, 256 + sub * 128:256 + (sub + 1) * 128],
                         rhs=HT[:, kc, :], start=(kc == 0), stop=(kc == 7))
                Yg, Yv = YE[(c % 2) * 2], YE[(c % 2) * 2 + 1]
                ffn_conv(pg_, Yg, UE[(c % 2) * 2], c)
                ffn_conv(pv_, Yv, UE[(c % 2) * 2 + 1], 22 + c)
                E.op("act", "activation", out=Yg[:], in_=Yg[:], func=AF.Silu)
                E.op("pool", "tensor_tensor", out=AT[:, c, :], in0=Yg[:], in1=Yv[:], op=ALU.mult)
        for hf in range(2):
            accs = [psum("acc") for _ in range(4)]
            for kg in range(3):
                W = get_w()
                nk = 8 if kg < 2 else 6
                for t in range(4):
                    for kk in range(nk):
                        c = 8 * kg + kk
                        E.op("pe", "matmul", out=accs[t][:, :], lhsT=AT[:, c, t * 128:(t + 1) * 128], rhs=W[:, kk, :],
                             start=(c == 0), stop=(c == 21), skip_group_check=True)
            for t in range(4):
                E.op("dve", "tensor_tensor", out=X1[t][:, hf * 512:(hf + 1) * 512], in0=accs[t][:, :],
                     in1=X1[t][:, hf * 512:(hf + 1) * 512], op=ALU.add)
        for t in range(4):
            E.op("act", "activation", out=JUNK2[:], in_=X1[t][:], func=AF.Square, accum_out=SS3[t][:])
            rstd_from_ss(SS3[t][:], RSTD3[t][:], 1024.0)
            ob = OUTB[t % 2]
            E.op("dve", "scalar_tensor_tensor", out=ob[:], in0=X1[t][:], scalar=RSTD3[t][:], in1=GFIN[:],
                 op0=ALU.mult, op1=ALU.mult)
            E.dma("pool", "d_out%d" % (t % 2), out=V(y[tok0 + t * 128:tok0 + (t + 1) * 128, :], "dram_y"), in_=ob[:])
        if I + 1 < NB:
            for t in range(4):
                E.dma("sp", "d_x%d" % t, out=X1[t][:],
                      in_=V(x[tok0 + 512 + t * 128:tok0 + 512 + (t + 1) * 128, :], "dram_x"))
        E.barrier()

    with nc.Block() as block:
        @block.sync
        def _(eng):
            E.replay("sp", eng)

        @block.tensor
        def _(eng):
            E.replay("pe", eng)

        @block.scalar
        def _(eng):
            E.replay("act", eng)

        @block.vector
        def _(eng):
            E.replay("dve", eng)

        @block.gpsimd
        def _(eng):
            E.replay("pool", eng)
            E.final_wait(eng, "pool")
    stack.close()
    return nc


def _rel_bucket(d):
    d = np.maximum(d, 0)
    large = 16 + (np.log(np.maximum(d, 1).astype(np.float32) / 16) / math.log(128 / 16) * 16).astype(np.int32)
    large = np.minimum(large, 31)
    return np.where(d < 16, d, large)


def host_consts(inp):
    f32 = np.float32
    c = {}
    c["gmix"] = np.ascontiguousarray(inp["norm_mix"].reshape(8, 128).T).astype(f32)
    c["gffn"] = np.ascontiguousarray(inp["norm_ffn"].reshape(8, 128).T).astype(f32)
    c["gfin"] = np.ascontiguousarray(np.broadcast_to(inp["norm_final"].reshape(1, 1024), (128, 1024))).astype(f32)
    mcw = inp["mlstm_conv_w"].reshape(4, 4, 128)
    c["mcw"] = np.ascontiguousarray(mcw.transpose(2, 1, 0).reshape(128, 16)).astype(f32)
    c["mcb"] = np.ascontiguousarray(inp["mlstm_conv_b"].reshape(4, 128).T).astype(f32)
    gb = np.concatenate([inp["i_bias"].reshape(4), inp["f_bias"].reshape(4)])
    c["gbias"] = np.ascontiguousarray(np.broadcast_to(gb.reshape(1, 8), (128, 8))).astype(f32)
    c["mnorm"] = np.ascontiguousarray(np.broadcast_to(inp["mlstm_norm"].reshape(1, 512), (128, 512))).astype(f32)
    c["gk"] = np.ascontiguousarray(np.broadcast_to(inp["idx_k_norm"].reshape(1, 64), (128, 64))).astype(f32)
    c["relb"] = np.ascontiguousarray(inp["rel_bias"]).astype(f32)
    c["b31"] = np.ascontiguousarray(inp["rel_bias"][31].reshape(8, 1)).astype(f32)
    oh = np.zeros((32, 384), f32)
    for k in range(384):
        d = k - 127
        if d >= 0:
            oh[int(_rel_bucket(np.array([d]))[0]), k] = 1.0
    c["oh"] = oh
    fw = inp["ffn_conv_w"].reshape(3, 44, 128)
    c["fcw"] = np.ascontiguousarray(fw.transpose(2, 1, 0).reshape(128, 132)).astype(f32)
    c["fcb"] = np.ascontiguousarray(inp["ffn_conv_b"].reshape(44, 128).T).astype(f32)
    c["ident"] = np.eye(128, dtype=f32)
    c["jrev"] = np.ascontiguousarray(np.eye(128, dtype=f32)[::-1])
    s = np.arange(128)[:, None]
    l = np.arange(128)[None, :]
    same = (s // 64) == (l // 64)
    tri = (same & (s <= l)).astype(f32)
    blk = same.astype(f32)
    blka = np.broadcast_to((s < 64), (128, 128)).astype(f32)
    blkb = np.broadcast_to((s >= 64), (128, 128)).astype(f32)
    c["tri4"] = np.ascontiguousarray(np.concatenate([tri, blk, blka, blkb], axis=1))
    q = np.arange(128)[:, None]
    kk = np.arange(128)[None, :]
    c["cmask"] = np.where(kk <= q, 0.0, NEG).astype(f32)
    c["hkc"] = np.ascontiguousarray(np.broadcast_to((0.5 ** (np.arange(NITER + 1) + 1)).reshape(1, NITER + 1), (128, NITER + 1))).astype(f32)
    return c


_NC_CACHE = {}


def kernel(**inputs):
    inp = {k: np.asarray(v) for k, v in inputs.items()}
    x = inp["x"]
    B, S, D = x.shape
    NB = S // 512
    topk = min(256, S // 4)
    key = (NB, topk)
    if key not in _NC_CACHE:
        _NC_CACHE[key] = build(NB, topk)
    nc = _NC_CACHE[key]
    c = host_consts(inp)
    shared = dict(c)
    shared["w_in"] = np.ascontiguousarray(inp["w_in"][0])
    shared["w_out"] = np.ascontiguousarray(inp["w_out"][0])
    shared["w_up"] = np.ascontiguousarray(inp["w_up"][0])
    shared["w_down"] = np.ascontiguousarray(inp["w_down"][0])
    in_maps = []
    for b in range(B):
        m = dict(shared)
        m["x"] = np.ascontiguousarray(x[b])
        in_maps.append(m)
    res = run_bass_kernel_spmd(nc, in_maps, core_ids=list(range(B)))
    out = np.stack([np.asarray(r["y"]) for r in res.results], axis=0).astype(np.float32)
    return out
```

```python
import math
import numpy as np
import ml_dtypes
import concourse.bass as bass
import concourse.mybir as mybir
from concourse.bass_utils import run_bass_kernel_spmd

F32 = mybir.dt.float32
BF16 = mybir.dt.bfloat16
AF = mybir.ActivationFunctionType
ALU = mybir.AluOpType
AX = mybir.AxisListType

EPS = 1e-6
import os
DBG_NOOVERLAP = os.environ.get('DBG_NOOVERLAP', '0') == '1'
DBG_NOX1 = os.environ.get('DBG_NOX1', '0') == '1'
NITER = 16
NEG = -1.0e30

WIN_GROUPS = [
    [(0, 512)],
    [(512, 512)],
    [(1024, 512)],
    [(1544, 512)],
    [(2056, 512)],
    [(2568, 512)],
    [(3080, 512)],
    [(3592, 72), (1536, 8)],
]


class V:
    __slots__ = ("ap", "key")

    def __init__(self, ap, key):
        if type(ap).__name__.endswith("Handle"):
            ap = ap[:]
        self.ap = ap
        self.key = key

    def __getitem__(self, idx):
        return V(self.ap[idx], self.key)

    def f(self, fn):
        return V(fn(self.ap), self.key)


class Em:
    ENG = ("pe", "act", "dve", "pool", "sp")

    def __init__(self, nc, stack):
        self.nc = nc
        self.stack = stack
        self.q = {e: [] for e in self.ENG}
        self.cnt = {}
        self.sems = {}
        self.lastw = {}
        self.readers = {}
        self.waited = {e: {} for e in self.ENG}
        for e in ("pe", "act", "dve", "pool"):
            self._sem(e)

    def _sem(self, key):
        if key not in self.sems:
            self.sems[key] = self.stack.enter_context(self.nc.semaphore("s_" + key))
            self.cnt[key] = 0
        return self.sems[key]

    def _emit(self, eng, meth, kw, reads, writes, semkey, inc):
        deps = {}

        def add(d):
            if d is None:
                return
            k, v = d
            if deps.get(k, 0) < v:
                deps[k] = v
        for b in reads:
            add(self.lastw.get(b))
        for b in writes:
            add(self.lastw.get(b))
            for k, v in self.readers.get(b, {}).items():
                add((k, v))
        waits = []
        for k, v in deps.items():
            if eng == "pe" and k == "pe":
                continue
            if self.waited[eng].get(k, 0) >= v:
                continue
            self.waited[eng][k] = v
            waits.append((k, v))
        self._sem(semkey)
        self.cnt[semkey] += inc
        val = self.cnt[semkey]
        self.q[eng].append((meth, kw, waits, semkey, inc))
        for b in reads:
            if b in writes:
                continue
            self.readers.setdefault(b, {})[semkey] = val
        for b in writes:
            self.lastw[b] = (semkey, val)
            self.readers[b] = {}
        return val

    def op(self, eng, meth, **kw):
        reads, writes, real = [], [], {}
        for k, v in kw.items():
            if isinstance(v, V):
                real[k] = v.ap
                keys = v.key if isinstance(v.key, (list, tuple)) else [v.key]
                if k in ("out", "accum_out"):
                    writes.extend(keys)
                else:
                    reads.extend(keys)
            else:
                real[k] = v
        return self._emit(eng, meth, real, reads, writes, eng, 1)

    def dma(self, eng, sem, out, in_, **extra):
        okeys = out.key if isinstance(out.key, (list, tuple)) else [out.key]
        ikeys = in_.key if isinstance(in_.key, (list, tuple)) else [in_.key]
        kw = dict(out=out.ap, in_=in_.ap)
        kw.update(extra)
        return self._emit(eng, "dma_start", kw, list(ikeys), list(okeys), sem, 16)

    def barrier(self):
        cur = dict(self.cnt)
        for e in self.ENG:
            waits = []
            for k, v in cur.items():
                if v == 0 or (e == k):
                    continue
                if self.waited[e].get(k, 0) >= v:
                    continue
                self.waited[e][k] = v
                waits.append((k, v))
            if waits:
                self.q[e].append((None, None, waits, None, 0))

    def replay(self, eng, engobj):
        for meth, kw, waits, semkey, inc in self.q[eng]:
            for k, v in waits:
                engobj.wait_ge(self.sems[k], v)
            if meth is None:
                continue
            ins = getattr(engobj, meth)(**kw)
            ins.then_inc(self.sems[semkey], inc)

    def final_wait(self, engobj, eng):
        for k, v in self.cnt.items():
            if v > 0 and self.waited[eng].get(k, 0) < v:
                engobj.wait_ge(self.sems[k], v)


def build(NB, topk):
    from contextlib import ExitStack
    S = NB * 512
    NT = NB * 4
    nc = bass.Bass("TRN2", target_bir_lowering=False)
    stack = ExitStack()
    E = Em(nc, stack)

    def din(name, shape, dt=F32):
        return nc.dram_tensor(name, list(shape), dt, kind="ExternalInput").ap()

    x = din("x", [S, 1024])
    w_in = din("w_in", [1024, 3664])
    w_out = din("w_out", [1024, 1024])
    w_up = din("w_up", [1024, 5632])
    w_down = din("w_down", [2816, 1024])
    d_gmix = din("gmix", [128, 8])
    d_gffn = din("gffn", [128, 8])
    d_gfin = din("gfin", [128, 1024])
    d_mcw = din("mcw", [128, 16])
    d_mcb = din("mcb", [128, 4])
    d_gbias = din("gbias", [128, 8])
    d_mnorm = din("mnorm", [128, 512])
    d_gk = din("gk", [128, 64])
    d_relb = din("relb", [32, 8])
    d_b31 = din("b31", [8, 1])
    d_oh = din("oh", [32, 384])
    d_fcw = din("fcw", [128, 132])
    d_fcb = din("fcb", [128, 44])
    d_ident = din("ident", [128, 128])
    d_tri4 = din("tri4", [128, 512])
    d_cmask = din("cmask", [128, 128])
    d_hkc = din("hkc", [128, NITER + 1])
    d_jrev = din("jrev", [128, 128])
    y = nc.dram_tensor("y", [S, 1024], F32, kind="ExternalOutput").ap()

    def dscratch(name, shape, dt=BF16):
        return nc.dram_tensor(name, list(shape), dt, kind="Internal").ap()

    sc_in = [dscratch("sc_in%d" % g, [128, 8, 512]) for g in range(8)]
    sc_out = [dscratch("sc_out%d" % g, [128, 8, 512]) for g in range(2)]
    sc_up = [dscratch("sc_up%d" % g, [128, 8, 512]) for g in range(11)]
    sc_dn = [dscratch("sc_dn%d" % g, [128, 8, 512]) for g in range(6)]
    tvd = dscratch("tvd", [8, 384])

    def sb(name, shape, dt):
        return stack.enter_context(nc.sbuf_tensor(name, list(shape), dt))

    KT = V(sb("KT", [128, 4, S], BF16), "KT")
    VP_t = sb("VP", [128, NT, 8, 65], BF16)
    IK = V(sb("IK", [128, S], BF16), "IK")
    WT = V(sb("WT", [128, 8, 256], BF16), "WT")
    WS = [V(sb("WS%d" % i, [128, 8, 512], BF16), "WS%d" % i) for i in range(3)]
    X1_t = sb("X1", [128, 4, 1024], F32)
    X1 = [V(X1_t[:, t, :], "X1_%d" % t) for t in range(4)]
    MIXT = sb("MIXT", [128, 8, 512], BF16)
    GMIX = V(sb("GMIX", [128, 8], F32), "c_gmix")
    GFFN = V(sb("GFFN", [128, 8], F32), "c_gffn")
    MCW = V(sb("MCW", [128, 16], F32), "c_mcw")
    MCB = V(sb("MCB", [128, 4], F32), "c_mcb")
    GBIAS = V(sb("GBIAS", [128, 8], F32), "c_gbias")
    GKB = V(sb("GKB", [128, 64], F32), "c_gk")
    FCW = V(sb("FCW", [128, 132], F32), "c_fcw")
    FCB = V(sb("FCB", [128, 44], F32), "c_fcb")
    IDENT = V(sb("IDENT", [128, 128], F32), "c_ident")
    IDENTB = V(sb("IDENTB", [128, 128], BF16), "c_identb")
    CMASK = V(sb("CMASK", [128, 128], F32), "c_cmask")
    HKC = V(sb("HKC", [128, NITER + 1], F32), "c_hkc")
    DG = V(sb("DG", [128, 8, 128], BF16), "DG")
    RELB = V(sb("RELB", [32, 8], F32), "c_relb")
    B31 = V(sb("B31", [8, 1], F32), "c_b31")
    OH = V(sb("OH", [32, 384], F32), "c_oh")
    ONES = V(sb("ONES", [128, 64], F32), "c_ones")
    CST_t = sb("CST", [128, 2, 129], F32)
    CB_t = sb("CB", [128, 4, 129], BF16)
    HM_t = sb("HM", [128, 4, 3], F32)
    HF_t = sb("HF", [128, 44, 2], F32)
    ST_t = sb("ST", [128, 320], F32)
    ARF = 18432
    AR = sb("ARENA", [128, ARF], F32)

    st_off = [0]

    def st(name, n):
        o = st_off[0]
        st_off[0] += n
        assert st_off[0] <= 320
        return V(ST_t[:, o:o + n], "st_" + name)

    def arv(off_bytes, nbytes, dt, key, pat=None, **kw):
        a = AR[:, off_bytes // 4:(off_bytes + nbytes) // 4]
        if dt == BF16:
            a = a.bitcast(BF16)
        if pat is not None:
            a = a.rearrange(pat, **kw)
        return V(a, key)

    PS = [stack.enter_context(nc.psum_tensor("ps%d" % i, [128, 512], F32)) for i in range(8)]
    psrot = {"mm": [0, 0, 4], "acc": [4, 0, 4], "qk": [0, 0, 6], "acc2": [6, 0, 2]}

    def psum(pool):
        b0, i, n = psrot[pool]
        psrot[pool][1] = (i + 1) % n
        k = b0 + i
        return V(PS[k][:, :], "ps%d" % k)

    def bfv(p):
        return V(p.ap.bitcast(BF16), p.key)

    for dst, src in ((GMIX, d_gmix), (GFFN, d_gffn), (MCW, d_mcw), (MCB, d_mcb), (GBIAS, d_gbias), (GKB, d_gk),
                     (FCW, d_fcw), (FCB, d_fcb), (IDENT, d_ident), (CMASK, d_cmask), (HKC, d_hkc), (RELB, d_relb),
                     (B31, d_b31), (OH, d_oh)):
        E.dma("sp", "d_" + dst.key, out=dst[:], in_=V(src, "dram_" + dst.key))
    E.op("dve", "tensor_copy", out=IDENTB[:], in_=IDENT[:])
    E._emit("dve", "memset", dict(ap=ONES.ap, constant=1.0), [], [ONES.key], "dve", 1)
    E._emit("pool", "memset", dict(ap=CST_t[:, :, :], constant=0.0), [], ["CST0", "CST1"], "pool", 1)
    E._emit("pool", "memset", dict(ap=CB_t[:, :, :], constant=0.0), [], ["CB0", "CB1", "CB2", "CB3"], "pool", 1)
    E._emit("pool", "memset", dict(ap=VP_t[:, :, :, 64:65], constant=1.0), [], ["VPones"], "pool", 1)

    TVS = arv(0, 768, BF16, "tvs")
    NB31 = st("nb31", 1)
    E.op("dve", "tensor_scalar", out=NB31[0:8, :], in0=B31[:], scalar1=-1.0, scalar2=None, op0=ALU.mult)
    pt = psum("mm")
    E.op("pe", "matmul", out=pt[0:8, 0:384], lhsT=RELB[:], rhs=OH[:], start=True, stop=True)
    E.op("act", "activation", out=TVS[0:8, :], in_=pt[0:8, 0:384], func=AF.Exp, bias=NB31[0:8, :], scale=1.0)
    E._emit("dve", "memset", dict(ap=TVS.ap[0:8, 0:127], constant=0.0), ["tvs"], ["tvs"], "dve", 1)
    TVD = V(tvd, "tvd")
    E.dma("sp", "d_tvd", out=TVD[:, :], in_=TVS[0:8, :])
    WTR = arv(1024, 4096, BF16, "WTR", "p (h c) -> p h c", h=8)
    JREVB = arv(5120, 256, BF16, "JREVB")
    JREVF = arv(5632, 512, F32, "JREVF")
    E.dma("sp", "d_jrev", out=JREVF[:], in_=V(d_jrev, "dram_jrev"))
    E.op("dve", "tensor_copy", out=JREVB[:], in_=JREVF[:])
    for h in range(8):
        src = bass.AP(tensor=tvd.tensor, offset=h * 384, ap=[[1, 128], [1, 256]])
        E.dma("sp", "d_wt", out=WTR[:, h, :], in_=V(src, "tvd"))
    for q4 in range(4):
        pw = psum("mm")
        E.op("pe", "matmul", out=pw[:, :], lhsT=JREVB[:],
             rhs=WTR[:, 2 * q4:2 * q4 + 2, :].f(lambda a: a.rearrange("p h c -> p (h c)")), start=True, stop=True)
        E.op("dve", "tensor_copy", out=WT[:, 2 * q4:2 * q4 + 2, :],
             in_=pw[:, :].f(lambda a: a.rearrange("p (h c) -> p h c", h=2)))
    E.barrier()

    STG = [arv(1024 + i * 16384, 16384, F32, "stg%d" % i, "p (a b) -> p a b", a=8) for i in range(2)]
    STB = [arv(1024 + 32768 + i * 8192, 8192, BF16, "stb%d" % i, "p (a b) -> p a b", a=8) for i in range(2)]
    groups = []
    w_in_r = w_in.rearrange("(c p) n -> p c n", p=128)
    w_out_r = w_out.rearrange("(c p) n -> p c n", p=128)
    w_up_r = w_up.rearrange("(c p) n -> p c n", p=128)
    w_dn_r = w_down.rearrange("(c p) n -> p c n", p=128)
    for g, pieces in enumerate(WIN_GROUPS):
        pcs, c0 = [], 0
        for (s0, n) in pieces:
            pcs.append((w_in_r[:, :, s0:s0 + n], c0, n))
            c0 += n
        groups.append((sc_in[g], "sc_in%d" % g, pcs, 8, c0, GMIX))
    for g in range(2):
        groups.append((sc_out[g], "sc_out%d" % g, [(w_out_r[:, :, g * 512:(g + 1) * 512], 0, 512)], 8, 512, None))
    for g in range(11):
        groups.append((sc_up[g], "sc_up%d" % g,
                       [(w_up_r[:, :, 256 * g:256 * g + 256], 0, 256),
                        (w_up_r[:, :, 2816 + 256 * g:2816 + 256 * g + 256], 256, 256)], 8, 512, GFFN))
    for hf in range(2):
        for kg in range(3):
            nk = 8 if kg < 2 else 6
            groups.append((sc_dn[hf * 3 + kg], "sc_dn%d" % (hf * 3 + kg),
                           [(w_dn_r[:, 8 * kg:8 * kg + nk, hf * 512:(hf + 1) * 512], 0, 512)], nk, 512, None))
    cast_engs = ["dve", "act"]
    for gi, (dst, dkey, pcs, kc, ncols, scl) in enumerate(groups):
        sl = gi % 2
        for (src, c0, n) in pcs:
            E.dma("sp", "d_stg%d" % sl, out=STG[sl][:, 0:kc, c0:c0 + n], in_=V(src, "dram_w"))
        eng = cast_engs[gi % 2]
        if scl is None:
            if eng == "act":
                E.op("act", "activation", out=STB[sl][:, 0:kc, 0:ncols], in_=STG[sl][:, 0:kc, 0:ncols], func=AF.Copy)
            else:
                E.op(eng, "tensor_copy", out=STB[sl][:, 0:kc, 0:ncols], in_=STG[sl][:, 0:kc, 0:ncols])
        else:
            for c in range(kc):
                if eng == "act":
                    E.op("act", "activation", out=STB[sl][:, c, 0:ncols], in_=STG[sl][:, c, 0:ncols],
                         func=AF.Copy, scale=scl[:, c:c + 1])
                else:
                    E.op(eng, "tensor_scalar", out=STB[sl][:, c, 0:ncols], in0=STG[sl][:, c, 0:ncols],
                         scalar1=scl[:, c:c + 1], scalar2=None, op0=ALU.mult)
        E.dma("pool", "d_stb%d" % sl, out=V(dst[:, 0:kc, 0:ncols], dkey), in_=STB[sl][:, 0:kc, 0:ncols])
    E.barrier()

    wseq = []
    for I in range(NB):
        for g in range(8):
            wseq.append((sc_in[g], "sc_in%d" % g, 8, 512 if g < 7 else 80))
        for g in range(2):
            wseq.append((sc_out[g], "sc_out%d" % g, 8, 512))
        for g in range(11):
            wseq.append((sc_up[g], "sc_up%d" % g, 8, 512))
        for g in range(6):
            wseq.append((sc_dn[g], "sc_dn%d" % g, 8 if g % 3 < 2 else 6, 512))
    wstate = {"loaded": 0, "next": 0}

    def get_w():
        idx = wstate["next"]
        wstate["next"] += 1
        while wstate["loaded"] <= idx + 2 and wstate["loaded"] < len(wseq):
            li = wstate["loaded"]
            src, skey, kc, ncols = wseq[li]
            slot = li % 3
            E.dma("sp", "d_ws%d" % slot, out=WS[slot][:, 0:kc, 0:ncols], in_=V(src[:, 0:kc, 0:ncols], skey))
            wstate["loaded"] += 1
        return WS[idx % 3]

    HB = arv(0, 2048, BF16, "HB")
    HBA = [HB, arv(38400, 2048, BF16, "HBb")]
    HBE = [HB, arv(67584, 2048, BF16, "HBc")]
    HT = arv(2048, 8192, BF16, "HT", "p (a b) -> p a b", a=8)
    QKT = arv(10240, 4096, BF16, "QKT", "p (a b) -> p a b", a=4)
    YC = [arv(14336 + i * 2048, 2048, F32, "YC%d" % i) for i in range(2)]
    VM_full = AR[:, 18432 // 4:(18432 + 4160) // 4].bitcast(BF16)[:, 0:2064].rearrange("p (t h d) -> p t h d", t=4, h=4)
    VM = V(VM_full, "VM")
    OG = arv(22592, 4096, BF16, "OG", "p (a b) -> p a b", a=4)
    TRI4 = arv(26688, 2048, F32, "TRI4")
    MNORM = arv(28736, 2048, F32, "MNORM")
    IKN = arv(30784, 128, BF16, "IKN")
    KW = arv(30912, 512, BF16, "KW", "p (h d) -> p h d", h=4)
    PTB = [arv(37056 + i * 256, 256, BF16, "PT%d" % i) for i in range(4)]
    HMO = arv(31936, 1024, BF16, "HMO")
    JUNK = arv(32960, 4096, F32, "JUNK")
    AQT = arv(65536, 4096, BF16, "AQT", "p (a b) -> p a b", a=4)
    IQT = arv(69632, 4096, BF16, "IQT", "p (a b) -> p a b", a=4)
    SCK = [V(AR[:, (0 + kc * 2048) // 4:(0 + (kc + 1) * 2048) // 4], "SC%d" % kc) for kc in range(8)]
    SCall = V(AR[:, 0:4096], ["SC%d" % kc for kc in range(8)])
    MQ = arv(16384, 8192, BF16, "MQ")
    MT = arv(24576, 32768, BF16, "MT", "p (j q) -> p j q", j=32)
    EP = [arv(57344 + i * 1024, 1024, BF16, "EP%d" % i) for i in range(8)]
    FT = [arv(i * 2048, 2048, F32, "FT%d" % i) for i in range(4)]
    RB = [arv(61440 + i * 2048, 2048, F32, "RB%d" % i) for i in range(2)]
    RBF = [arv(61440 + i * 1024, 1024, BF16, "RBF%d" % i) for i in range(4)]
    YE = [arv(10240 + i * 2048, 2048, F32, "YE%d" % i) for i in range(4)]
    AT = arv(18432, 22528, BF16, "AT", "p (c q) -> p c q", c=22)
    GFIN = arv(40960, 4096, F32, "GFIN")
    OUTB = [arv(45056 + i * 4096, 4096, F32, "OUTB%d" % i) for i in range(2)]
    JUNK2 = arv(53248, 4096, F32, "JUNK2")
    UE = [arv(57344 + i * 2064, 2056, F32, "UE%d" % i) for i in range(4)]

    SS = [st("ss%d" % t_, 1) for t_ in range(4)]
    RSTD = [st("rstd%d" % t_, 1) for t_ in range(4)]
    SSK = [st("ssk%d" % t_, 1) for t_ in range(4)]
    RK = [st("rk%d" % t_, 1) for t_ in range(4)]
    GATES = [st("gates%d" % t, 8) for t in range(4)]
    WQ = [st("wq%d" % t, 8) for t in range(4)]
    E1 = st("e1", 4)
    NLF = st("nlf", 4)
    GS = st("gs", 16)
    EB8 = st("eb8", 4)
    T1 = st("t1", 4)
    EK = st("ek", 4)
    T2 = st("t2", 4)
    WKs = st("wk", 4)
    EG = st("eg", 8)
    DN = st("dn", 4)
    RR = st("rr", 4)
    MS = st("ms", 4)
    TT = st("tt", 4)
    SCL = st("scl", 4)
    MX = st("mx", 1)
    MN2 = [st("mn0", 1), st("mn1", 1)]
    W0 = st("w0", 1)
    NHK = st("nhk", NITER + 1)
    NM = st("nm", 1)
    SACC = st("sacc", 1)
    SG = st("sg", 1)
    LO = st("lo", 1)
    MID = st("mid", 1)
    CNT = st("cnt", 1)
    TMP = st("tmp", 1)
    SS2 = [st("ss2%d" % t_, 1) for t_ in range(4)]
    RSTD2 = [st("rstd2%d" % t_, 1) for t_ in range(4)]
    SS3 = [st("ss3%d" % t_, 1) for t_ in range(4)]
    RSTD3 = [st("rstd3%d" % t_, 1) for t_ in range(4)]

    CST = [V(CST_t[:, pp, :], "CST%d" % pp) for pp in range(2)]
    CBh = [V(CB_t[:, h, :], "CB%d" % h) for h in range(4)]
    HMh = [V(HM_t[:, c, :], "HM%d" % c) for c in range(4)]
    HFh = [V(HF_t[:, c, :], "HF%d" % c) for c in range(44)]
    MIXc = [V(MIXT[:, c, :], "MIX%d" % c) for c in range(8)]
    MIX03 = V(MIXT[:, 0:4, :], ["MIX0", "MIX1", "MIX2", "MIX3"])

    def rstd_batch(srcs, sss, rvs, junk, n):
        for t_ in range(len(srcs)):
            E.op("act", "activation", out=junk, in_=srcs[t_], func=AF.Square, accum_out=sss[t_])
        for t_ in range(len(srcs)):
            E.op("dve", "tensor_scalar", out=rvs[t_], in0=sss[t_], scalar1=1.0 / n, scalar2=EPS, op0=ALU.mult, op1=ALU.add)
        for t_ in range(len(srcs)):
            E.op("act", "activation", out=rvs[t_], in_=rvs[t_], func=AF.Sqrt)
        for t_ in range(len(srcs)):
            E.op("dve", "reciprocal", out=rvs[t_], in_=rvs[t_])

    def rstd_from_ss(ssv, rv, n):
        E.op("dve", "tensor_scalar", out=rv, in0=ssv, scalar1=1.0 / n, scalar2=EPS, op0=ALU.mult, op1=ALU.add)
        E.op("act", "activation", out=rv, in_=rv, func=AF.Sqrt)
        E.op("dve", "reciprocal", out=rv, in_=rv)

    def transposes_to(dst_view, src, nchunk):
        p = psum("mm")
        pb = bfv(p)
        for c in range(nchunk):
            E.op("pe", "transpose", out=pb[:, c * 128:(c + 1) * 128], in_=src[:, c * 128:(c + 1) * 128],
                 identity=IDENTB[:])
        E.op("act", "activation", out=dst_view,
             in_=pb[:, 0:nchunk * 128].f(lambda a: a.rearrange("p (c q) -> p c q", c=nchunk)), func=AF.Copy)

    rr = {"i": 0}

    def alt(*engs):
        rr["i"] += 1
        return engs[rr["i"] % len(engs)]

    for I in range(NB):
        tok0 = I * 512
        E.dma("sp", "d_tri4", out=TRI4[:], in_=V(d_tri4, "dram_tri4"))
        E.dma("sp", "d_mnorm", out=MNORM[:], in_=V(d_mnorm, "dram_mnorm"))
        if I == 0:
            for t in range(4):
                E.dma("sp", "d_x%d" % t, out=X1[t][:], in_=V(x[tok0 + t * 128:tok0 + (t + 1) * 128, :], "dram_x"))
        rstd_batch([X1[t][:] for t in range(4)], [SS[t][:] for t in range(4)], [RSTD[t][:] for t in range(4)],
                   JUNK[:], 1024.0)
        for t in range(4):
            E.op("dve", "tensor_scalar", out=HBA[t % 2][:], in0=X1[t][:], scalar1=RSTD[t][:], scalar2=None, op0=ALU.mult)
            transposes_to(HT[:, :, t * 128:(t + 1) * 128], HBA[t % 2], 8)

        def fm_group(W, evac):
            for cc in range(4):
                p = psum("mm")
                for c in range(8):
                    E.op("pe", "matmul", out=p[:, :], lhsT=W[:, c, cc * 128:(cc + 1) * 128], rhs=HT[:, c, :],
                         start=(c == 0), stop=(c == 7))
                evac(cc, p)

        def tm_group(W, ncols, evac):
            for t in range(4):
                p = psum("mm")
                for c in range(8):
                    E.op("pe", "matmul", out=p[:, 0:ncols], lhsT=HT[:, c, t * 128:(t + 1) * 128], rhs=W[:, c, 0:ncols],
                         start=(c == 0), stop=(c == 7))
                evac(t, p)

        def ev_g0(cc, p):
            Y = YC[cc % 2]
            E.op("act", "activation", out=Y[:], in_=p[:, :], func=AF.Identity, scale=MCW[:, cc * 4 + 3:cc * 4 + 4],
                 bias=MCB[:, cc:cc + 1])
            for j, sh in ((2, 1), (1, 2), (0, 3)):
                E.op("dve", "scalar_tensor_tensor", out=Y[:, sh:512], in0=p[:, 0:512 - sh],
                     scalar=MCW[:, cc * 4 + j:cc * 4 + j + 1], in1=Y[:, sh:512], op0=ALU.mult, op1=ALU.add)
            if I > 0:
                H = HMh[cc]
                E.op("dve", "scalar_tensor_tensor", out=Y[:, 0:3], in0=H[:, 0:3], scalar=MCW[:, cc * 4:cc * 4 + 1],
                     in1=Y[:, 0:3], op0=ALU.mult, op1=ALU.add)
                E.op("dve", "scalar_tensor_tensor", out=Y[:, 0:2], in0=H[:, 1:3], scalar=MCW[:, cc * 4 + 1:cc * 4 + 2],
                     in1=Y[:, 0:2], op0=ALU.mult, op1=ALU.add)
                E.op("dve", "scalar_tensor_tensor", out=Y[:, 0:1], in0=H[:, 2:3], scalar=MCW[:, cc * 4 + 2:cc * 4 + 3],
                     in1=Y[:, 0:1], op0=ALU.mult, op1=ALU.add)
            E.op("act", "activation", out=HMh[cc][:], in_=p[:, 509:512], func=AF.Copy)
            E.op("act", "activation", out=QKT[:, cc, :], in_=Y[:], func=AF.Silu)
        fm_group(get_w(), ev_g0)

        def ev_g1(t, p):
            E.op("act", "activation", out=VM[:, t, :, 0:128],
                 in_=p[:, :].f(lambda a: a.rearrange("p (h d) -> p h d", h=4)), func=AF.Copy)
        E._emit("pool", "memset", dict(ap=VM.ap[:, :, :, 128:129], constant=1.0), [], ["VM"], "pool", 1)
        tm_group(get_w(), 512, ev_g1)

        def ev_g2(t, p):
            E.op("act", "activation", out=OG[:, t, :], in_=p[:, :], func=AF.Sigmoid)
            E.op("pool", "tensor_tensor", out=OG[:, t, :], in0=OG[:, t, :], in1=MNORM[:], op=ALU.mult)
        tm_group(get_w(), 512, ev_g2)

        def ev_g3(cc, p):
            E.op("act", "activation", out=AQT[:, cc, :], in_=p[:, :], func=AF.Copy, scale=0.125)
        fm_group(get_w(), ev_g3)

        def ev_g4(cc, p):
            E.op("dve", "tensor_copy", out=KT[:, cc, tok0:tok0 + 512], in_=p[:, :])
        fm_group(get_w(), ev_g4)

        def ev_g5(t, p):
            E.op("act", "activation", out=V(VP_t[:, I * 4 + t, :, 0:64], "VP"),
                 in_=p[:, :].f(lambda a: a.rearrange("p (h d) -> p h d", h=8)), func=AF.Copy)
        tm_group(get_w(), 512, ev_g5)

        def ev_g6(cc, p):
            E.op("dve", "tensor_copy", out=IQT[:, cc, :], in_=p[:, :])
        fm_group(get_w(), ev_g6)

        def ev_g7(t, p):
            E.op("act", "activation", out=JUNK[:, 0:64], in_=p[:, 0:64], func=AF.Square, accum_out=SSK[t][:])
            rstd_from_ss(SSK[t][:], RK[t][:], 64.0)
            E.op("dve", "scalar_tensor_tensor", out=IKN[:], in0=p[:, 0:64], scalar=RK[t][:], in1=GKB[:],
                 op0=ALU.mult, op1=ALU.mult)
            E.op("dve", "tensor_scalar", out=WQ[t][:], in0=p[:, 64:72], scalar1=float(8 ** -0.5 * 64 ** -0.5),
                 scalar2=None, op0=ALU.mult)
            E.op("dve", "tensor_tensor", out=GATES[t][:], in0=p[:, 72:80], in1=GBIAS[:], op=ALU.add)
            p2 = psum("mm")
            pb = bfv(p2)
            E.op("pe", "transpose", out=pb[0:64, 0:128], in_=IKN[:], identity=IDENTB[:])
            c0 = tok0 + t * 128
            E.op("dve", "tensor_copy", out=IK[0:64, c0:c0 + 128], in_=pb[0:64, 0:128])
            E.op("dve", "tensor_copy", out=IK[64:128, c0:c0 + 128], in_=pb[0:64, 0:128])
        tm_group(get_w(), 80, ev_g7)

        for t in range(4):
            c0 = t * 128
            pk = psum("mm")
            pkb = bfv(pk)
            for pp in range(2):
                E.op("pe", "transpose", out=pkb[:, pp * 128:(pp + 1) * 128], in_=QKT[:, 2 + pp, c0:c0 + 128],
                     identity=IDENTB[:])
            G = GATES[t]
            E.op("act", "activation", out=E1[:], in_=G[:, 4:8], func=AF.Exp, scale=-1.0)
            E.op("dve", "tensor_scalar", out=E1[:], in0=E1[:], scalar1=1.0, scalar2=None, op0=ALU.add)
            E.op("act", "activation", out=NLF[:], in_=E1[:], func=AF.Ln)
            pg = psum("mm")
            for k in range(4):
                E.op("pe", "matmul", out=pg[:, 4 * k:4 * k + 4], lhsT=TRI4[:, 128 * k:128 * (k + 1)], rhs=NLF[:],
                     start=True, stop=True)
            E.op("dve", "tensor_copy", out=GS[:], in_=pg[:, 0:16])
            E.op("act", "activation", out=EB8[:], in_=GS[:, 0:4], func=AF.Exp, scale=-1.0, bias=-math.log(8.0))
            E.op("dve", "tensor_tensor", out=T1[:], in0=G[:, 0:4], in1=GS[:, 0:4], op=ALU.add)
            E.op("act", "activation", out=EK[:], in_=T1[:], func=AF.Exp)
            E.op("dve", "tensor_tensor", out=T2[:], in0=T1[:], in1=GS[:, 4:8], op=ALU.subtract)
            E.op("act", "activation", out=WKs[:], in_=T2[:], func=AF.Exp)
            E.op("act", "activation", out=EG[:], in_=GS[:, 8:16], func=AF.Exp, scale=-1.0)
            E.op("dve", "tensor_tensor", out=KW[:],
                 in0=pkb[:, 0:256].f(lambda a: a.rearrange("p (h d) -> p h d", h=4)),
                 in1=WKs[:, 0:4].f(lambda a: a.unsqueeze(2).to_broadcast([128, 4, 64])), op=ALU.mult)
            npss = [psum("acc") for _ in range(4)]
            for h in range(4):
                pp, hh = h // 2, h % 2
                base, o = 64 * hh, 0
                pr = psum("mm")
                E.op("pe", "matmul", out=pr[:, 0:128], lhsT=QKT[base:base + 64, 2 + pp, c0:c0 + 128],
                     rhs=QKT[base:base + 64, pp, c0:c0 + 128], start=True, stop=True)
                PT = PTB[h]
                E.op("dve", "scalar_tensor_tensor", out=PT[:], in0=pr[:, 0:128], scalar=EK[:, h:h + 1],
                     in1=TRI4[:, 0:128], op0=ALU.mult, op1=ALU.mult)
                E.op("pe", "matmul", out=npss[h][:, o:o + 129], lhsT=PT[:], rhs=VM[:, t, h, :], start=True, stop=False,
                     skip_group_check=True)
            for ch in range(2):
                r0 = 64 * ch
                pkvs = []
                for h in range(4):
                    pp, hh = h // 2, h % 2
                    base, o = 64 * hh, 0
                    E.op("pe", "matmul", out=npss[h][r0:r0 + 64, o:o + 129], lhsT=QKT[:, pp, c0 + r0:c0 + r0 + 64],
                         rhs=CBh[h][:], start=False, stop=(ch == 1), skip_group_check=True)
                    pkv = psum("mm")
                    E.op("pe", "matmul", out=pkv[base:base + 64, 0:129], lhsT=KW[r0:r0 + 64, h, :],
                         rhs=VM[r0:r0 + 64, t, h, :], start=True, stop=True)
                    pkvs.append(pkv)
                for h in range(4):
                    pp, hh = h // 2, h % 2
                    base = 64 * hh
                    E.op("dve", "scalar_tensor_tensor", out=CST[pp][base:base + 64, :], in0=CST[pp][base:base + 64, :],
                         scalar=EG[base:base + 64, 4 * ch + h:4 * ch + h + 1], in1=pkvs[h][base:base + 64, 0:129],
                         op0=ALU.mult, op1=ALU.add)
                for h in range(4):
                    pp, hh = h // 2, h % 2
                    base = 64 * hh
                    E.op("act", "activation", out=CBh[h][base:base + 64, :], in_=CST[pp][base:base + 64, :],
                         func=AF.Copy)
            for h in range(4):
                E.op("dve", "tensor_tensor", out=DN[:, h:h + 1], in0=npss[h][:, 128:129], in1=EB8[:, h:h + 1], op=ALU.mult)
            E.op("act", "activation", out=DN[:], in_=DN[:], func=AF.Abs)
            E.op("dve", "tensor_scalar", out=DN[:], in0=DN[:], scalar1=1.0, scalar2=None, op0=ALU.max)
            E.op("dve", "reciprocal", out=DN[:], in_=DN[:])
            E.op("dve", "tensor_tensor", out=RR[:], in0=DN[:], in1=EB8[:, 0:4], op=ALU.mult)
            for h in range(4):
                E.op("act", "activation", out=JUNK[:, h * 128:(h + 1) * 128], in_=npss[h][:, 0:128], func=AF.Square,
                     accum_out=MS[:, h:h + 1])
            E.op("dve", "tensor_tensor", out=TT[:], in0=RR[:], in1=RR[:], op=ALU.mult)
            E.op("dve", "tensor_tensor", out=TT[:], in0=TT[:], in1=MS[:], op=ALU.mult)
            E.op("dve", "tensor_scalar", out=TT[:], in0=TT[:], scalar1=1.0 / 128.0, scalar2=EPS, op0=ALU.mult,
                 op1=ALU.add)
            E.op("act", "activation", out=TT[:], in_=TT[:], func=AF.Sqrt)
            E.op("dve", "reciprocal", out=TT[:], in_=TT[:])
            E.op("dve", "tensor_tensor", out=SCL[:], in0=TT[:], in1=RR[:], op=ALU.mult)
            for h in range(4):
                E.op("dve", "scalar_tensor_tensor", out=HMO[:, h * 128:(h + 1) * 128],
                     in0=npss[h][:, 0:128], scalar=SCL[:, h:h + 1], in1=OG[:, t, h * 128:(h + 1) * 128],
                     op0=ALU.mult, op1=ALU.mult)
            transposes_to(MIX03[:, :, c0:c0 + 128], HMO, 4)

        E.barrier()
        nkt = 4 * I + 4
        X1flat = X1_t[:].rearrange("p a b -> p (a b)")
        SCB = [
            ([SCK[kc] for kc in range(8)], SCall),
            ([V(X1_t[:, kc // 2, (kc % 2) * 512:(kc % 2) * 512 + 512], "X1_%d" % (kc // 2)) for kc in range(8)],
             V(X1flat, ["X1_0", "X1_1", "X1_2", "X1_3"])),
        ]

        def score_gen(t):
            i = 4 * I + t
            n = 128 * (i + 1)
            nch = (n + 511) // 512
            sck, scall = SCB[0 if DBG_NOX1 else t % 2]
            for h in range(8):
                E.op("dve", "tensor_scalar", out=DG[:, h, :], in0=IDENT[:], scalar1=WQ[t][:, h:h + 1], scalar2=None,
                     op0=ALU.mult)
            dk = i // 4
            for kc in range(nch):
                w = min(512, n - 512 * kc)
                pacc = psum("acc")
                pis = {}

                def emit_isc(h, kc=kc, w=w):
                    base, pp = 64 * (h % 2), h // 2
                    p = psum("mm")
                    E.op("pe", "matmul", out=p[:, 0:w], lhsT=IQT[base:base + 64, pp, t * 128:(t + 1) * 128],
                         rhs=IK[base:base + 64, 512 * kc:512 * kc + w], start=True, stop=True)
                    pis[h] = p
                for h in range(3):
                    emit_isc(h)
                for h in range(8):
                    if h + 3 < 8:
                        emit_isc(h + 3)
                    p = pis.pop(h)
                    R = RBF[h % 4]
                    if h % 2 == 0:
                        E.op("act", "activation", out=R[:, 0:w], in_=p[:, 0:w], func=AF.Relu)
                    else:
                        E.op("dve", "tensor_scalar", out=R[:, 0:w], in0=p[:, 0:w], scalar1=0.0, scalar2=None, op0=ALU.max)
                    E.op("pe", "matmul", out=pacc[:, 0:w], lhsT=DG[:, h, :], rhs=R[:, 0:w], start=(h == 0),
                         stop=(h == 7), skip_group_check=True)
                    if h == 7:
                        E.op("dve", "tensor_copy", out=sck[kc][:, 0:w], in_=pacc[:, 0:w])
                    yield (h == 7)
            if n > topk:
                E.op("dve", "tensor_reduce", out=MN2[t % 2][:], in_=scall[:, 0:n], axis=AX.X, op=ALU.min)
            E.op("dve", "tensor_tensor", out=sck[dk][:, (i % 4) * 128:(i % 4) * 128 + 128],
                 in0=sck[dk][:, (i % 4) * 128:(i % 4) * 128 + 128], in1=CMASK[:], op=ALU.add)
            yield True

        def bisect_gen(t):
            i = 4 * I + t
            n = 128 * (i + 1)
            sck, scall = SCB[0 if DBG_NOX1 else t % 2]
            MNt = MN2[t % 2]
            if n > topk:
                E.op("dve", "tensor_reduce", out=MX[:], in_=scall[:, 0:n], axis=AX.X, op=ALU.max)
                E.op("dve", "tensor_tensor", out=W0[:], in0=MX[:], in1=MNt[:], op=ALU.subtract)
                E.op("dve", "tensor_scalar", out=W0[:], in0=W0[:], scalar1=1.0001, scalar2=1e-6, op0=ALU.mult, op1=ALU.add)
                E.op("dve", "tensor_scalar", out=NHK[:], in0=HKC[:], scalar1=W0[:], scalar2=-1.0, op0=ALU.mult, op1=ALU.mult)
                E.op("dve", "scalar_tensor_tensor", out=NM[:], in0=MNt[:], scalar=-1.0, in1=NHK[:, 0:1], op0=ALU.mult,
                     op1=ALU.add)
                cthr = float(2 * topk - n) - 0.5
                split = n >= 1536
                if split:
                    nA = ((int(0.56 * n) + 127) // 128) * 128
                    nB = n - nA
                    MQa = V(MQ.ap[:, 0:nA], "MQa")
                    MQb = V(MQ.ap[:, nA:n], "MQb")
                    E.op("dve", "tensor_scalar", out=MID[:], in0=NM[:], scalar1=-1.0, scalar2=None, op0=ALU.mult)
                yield
                for k in range(NITER):
                    if split:
                        E.op("act", "activation", out=MQa, in_=scall[:, 0:nA], func=AF.Sign, bias=NM[:], scale=1.0,
                             accum_out=SACC[:])
                        E.op("dve", "tensor_scalar", out=MQb, in0=scall[:, nA:n], scalar1=MID[:], scalar2=None,
                             op0=ALU.is_ge, op1=ALU.add, accum_out=CNT[:])
                        yield
                        E.op("dve", "tensor_scalar", out=TMP[:], in0=CNT[:], scalar1=2.0, scalar2=-(float(nB) + cthr),
                             op0=ALU.mult, op1=ALU.add)
                        E.op("act", "activation", out=SG[:], in_=SACC[:], func=AF.Sign, bias=TMP[:], scale=1.0)
                    else:
                        E.op("act", "activation", out=MQ[:, 0:n], in_=scall[:, 0:n], func=AF.Sign, bias=NM[:], scale=1.0,
                             accum_out=SACC[:])
                        yield
                        E.op("act", "activation", out=SG[:], in_=SACC[:], func=AF.Sign, bias=-cthr, scale=1.0)
                    E.op("act", "activation", out=NM[:], in_=SG[:], func=AF.Identity, scale=NHK[:, k + 1:k + 2], bias=NM[:])
                    if split and k + 1 < NITER:
                        E.op("dve", "tensor_scalar", out=MID[:], in0=NM[:], scalar1=-1.0, scalar2=None, op0=ALU.mult)
                E.op("act", "activation", out=LO[:], in_=NM[:], func=AF.Identity, scale=-1.0, bias=NHK[:, NITER:NITER + 1])
                E.op("dve", "tensor_scalar", out=V(MQ.ap[:, 0:n], ["MQ", "MQa", "MQb"]), in0=scall[:, 0:n], scalar1=LO[:],
                     scalar2=None, op0=ALU.is_ge)
            else:
                E.op("dve", "tensor_scalar", out=MQ[:, 0:n], in0=scall[:, 0:n], scalar1=-1.0e29, scalar2=None,
                     op0=ALU.is_ge)
            if t < 3:
                E._emit("pool", "memset", dict(ap=MQ.ap[:, n:128 * nkt], constant=0.0), [], ["MQ"], "pool", 1)
            yield "pre_transpose"
            for jg in range((nkt + 3) // 4):
                p = psum("mm")
                pb = bfv(p)
                for jj in range(4):
                    j = 4 * jg + jj
                    E.op("pe", "transpose", out=pb[:, jj * 128:(jj + 1) * 128], in_=MQ[:, 128 * j:128 * (j + 1)],
                         identity=IDENTB[:])
                E.op("dve", "tensor_copy", out=MT[:, 4 * jg:4 * jg + 4, t * 128:(t + 1) * 128],
                     in_=pb[:, 0:512].f(lambda a: a.rearrange("p (j q) -> p j q", j=4)))
            yield

        for _ in score_gen(0):
            pass
        for t in range(4):
            bg = bisect_gen(t)
            sg = score_gen(t + 1) if t + 1 < 4 else None
            nb_steps = NITER + 2
            ns_steps = (8 * ((128 * (4 * I + t + 2) + 511) // 512) + 1) if sg is not None else 0
            ratio = float(ns_steps) / nb_steps
            if DBG_NOOVERLAP:
                ratio = 0.0
            accr = 0.0
            state = {"sg": sg, "bnd": True}

            def adv():
                try:
                    state["bnd"] = bool(next(state["sg"]))
                except StopIteration:
                    state["sg"] = None
                    state["bnd"] = True
            for tag in bg:
                if tag == "pre_transpose":
                    while state["sg"] is not None and not state["bnd"]:
                        adv()
                    continue
                accr += ratio
                while state["sg"] is not None and accr >= 1.0:
                    accr -= 1.0
                    adv()
            while state["sg"] is not None:
                adv()
        for t in range(4):
            E.dma("sp", "d_x%d" % t, out=X1[t][:], in_=V(x[tok0 + t * 128:tok0 + (t + 1) * 128, :], "dram_x"))
        for pp in range(4):
            accs = [psum("acc2"), psum("acc2")]
            qk = {}

            def emit_qk(j, pp=pp):
                for hh in range(2):
                    base = 64 * hh
                    p = psum("qk")
                    E.op("pe", "matmul", out=p[:, :], lhsT=KT[base:base + 64, pp, 128 * j:128 * (j + 1)],
                         rhs=AQT[base:base + 64, pp, :], start=True, stop=True)
                    qk[(j, hh)] = p
            for j in range(min(2, nkt)):
                emit_qk(j)
            for j in range(nkt):
                if j + 2 < nkt:
                    emit_qk(j + 2)
                for hh in range(2):
                    h = 2 * pp + hh
                    p = qk.pop((j, hh))
                    e = EP[(2 * j + hh) % 8]
                    E.op("act", "activation", out=e[:], in_=p[:, :], func=AF.Exp)
                    E.op("dve", "tensor_tensor", out=e[:], in0=e[:], in1=MT[:, j, :], op=ALU.mult)
                    r = j - 4 * I
                    if r >= -1:
                        for tq, v in ((r, 0), (r + 1, 1)):
                            if 0 <= tq <= 3:
                                E.op("dve", "tensor_tensor", out=e[:, tq * 128:(tq + 1) * 128],
                                     in0=e[:, tq * 128:(tq + 1) * 128], in1=WT[:, h, v * 128:(v + 1) * 128], op=ALU.mult)
                    E.op("pe", "matmul", out=accs[hh][0:65, :], lhsT=V(VP_t[:, j, h, :], ["VP", "VPones"]), rhs=e[:],
                         start=(j == 0), stop=(j == nkt - 1))
            for hh in range(2):
                E.op("act", "activation", out=FT[2 * hh][64:65, :], in_=accs[hh][64:65, :], func=AF.Ln)
            for hh in range(2):
                E.op("act", "activation", out=FT[2 * hh][64:65, :], in_=FT[2 * hh][64:65, :], func=AF.Exp, scale=-1.0)
            pbcs = []
            for hh in range(2):
                pbc = psum("qk")
                E.op("pe", "matmul", out=pbc[0:64, :], lhsT=ONES[64:65, 0:64], rhs=FT[2 * hh][64:65, :], start=True,
                     stop=True)
                pbcs.append(pbc)
            for hh in range(2):
                E.op("act", "activation", out=FT[2 * hh + 1][0:64, :], in_=pbcs[hh][0:64, :], func=AF.Copy)
            for hh in range(2):
                base = 64 * hh
                E.op("dve", "tensor_tensor", out=MIXc[4 + pp][base:base + 64, :], in0=accs[hh][0:64, :],
                     in1=FT[2 * hh + 1][0:64, :], op=ALU.mult)

        E.barrier()
        E.dma("sp", "d_gfin", out=GFIN[:], in_=V(d_gfin, "dram_gfin"))
        for hf in range(2):
            W = get_w()
            for t in range(4):
                p = psum("mm")
                for c in range(8):
                    E.op("pe", "matmul", out=p[:, :], lhsT=MIXc[c][:, t * 128:(t + 1) * 128], rhs=W[:, c, :],
                         start=(c == 0), stop=(c == 7))
                E.op("dve", "tensor_tensor", out=X1[t][:, hf * 512:(hf + 1) * 512], in0=p[:, :],
                     in1=X1[t][:, hf * 512:(hf + 1) * 512], op=ALU.add)
        rstd_batch([X1[t][:] for t in range(4)], [SS2[t][:] for t in range(4)], [RSTD2[t][:] for t in range(4)],
                   JUNK2[:], 1024.0)
        for t in range(4):
            E.op("dve", "tensor_scalar", out=HBE[t % 2][:], in0=X1[t][:], scalar1=RSTD2[t][:], scalar2=None, op0=ALU.mult)
            transposes_to(HT[:, :, t * 128:(t + 1) * 128], HBE[t % 2], 8)

        def ffn_conv(p, Y, U, ci):
            E.op("act", "activation", out=Y[:], in_=p[:, :], func=AF.Identity, scale=FCW[:, 3 * ci + 2:3 * ci + 3],
                 bias=FCB[:, ci:ci + 1])
            E.op("act", "activation", out=U[:, 2:514], in_=p[:, :], func=AF.Copy)
            if I > 0:
                E.op("act", "activation", out=U[:, 0:2], in_=HFh[ci][:], func=AF.Copy)
            else:
                E._emit("act", "memzero", dict(ap=U.ap[:, 0:2]), [], [U.key], "act", 1)
            E.op("act", "activation", out=HFh[ci][:], in_=U[:, 512:514], func=AF.Copy)
            E.op("dve", "scalar_tensor_tensor", out=Y[:], in0=U[:, 1:513], scalar=FCW[:, 3 * ci + 1:3 * ci + 2], in1=Y[:],
                 op0=ALU.mult, op1=ALU.add)
            E.op("dve", "scalar_tensor_tensor", out=Y[:], in0=U[:, 0:512], scalar=FCW[:, 3 * ci:3 * ci + 1], in1=Y[:],
                 op0=ALU.mult, op1=ALU.add)

        for g in range(11):
            W = get_w()
            for sub in range(2):
                c = 2 * g + sub
                pg_ = psum("mm")
                pv_ = psum("mm")
                for kc in range(8):
                    E.op("pe", "matmul", out=pg_[:, :], lhsT=W[:, kc, sub * 128:(sub + 1) * 128], rhs=HT[:, kc, :],
                         start=(kc == 0), stop=(kc == 7))
                for kc in range(8):
                    E.op("pe", "matmul", out=pv_[:, :], lhsT=W[:, kc, 256 + sub * 128:256 + (sub + 1) * 128],
                         rhs=HT[:, kc, :], start=(kc == 0), stop=(kc == 7))
                Yg, Yv = YE[(c % 2) * 2], YE[(c % 2) * 2 + 1]
                ffn_conv(pg_, Yg, UE[(c % 2) * 2], c)
                ffn_conv(pv_, Yv, UE[(c % 2) * 2 + 1], 22 + c)
                E.op("act", "activation", out=Yg[:], in_=Yg[:], func=AF.Silu)
                E.op("pool", "tensor_tensor", out=AT[:, c, :], in0=Yg[:], in1=Yv[:], op=ALU.mult)
        for hf in range(2):
            accs = [psum("acc") for _ in range(4)]
            for kg in range(3):
                W = get_w()
                nk = 8 if kg < 2 else 6
                for t in range(4):
                    for kk in range(nk):
                        c = 8 * kg + kk
                        E.op("pe", "matmul", out=accs[t][:, :], lhsT=AT[:, c, t * 128:(t + 1) * 128], rhs=W[:, kk, :],
                             start=(c == 0), stop=(c == 21), skip_group_check=True)
            for t in range(4):
                E.op("dve", "tensor_tensor", out=X1[t][:, hf * 512:(hf + 1) * 512], in0=accs[t][:, :],
                     in1=X1[t][:, hf * 512:(hf + 1) * 512], op=ALU.add)
        rstd_batch([X1[t][:] for t in range(4)], [SS3[t][:] for t in range(4)], [RSTD3[t][:] for t in range(4)],
                   JUNK2[:], 1024.0)
        for t in range(4):
            ob = OUTB[t % 2]
            E.op("dve", "scalar_tensor_tensor", out=ob[:], in0=X1[t][:], scalar=RSTD3[t][:], in1=GFIN[:],
                 op0=ALU.mult, op1=ALU.mult)
            E.dma("pool", "d_out%d" % (t % 2), out=V(y[tok0 + t * 128:tok0 + (t + 1) * 128, :], "dram_y"), in_=ob[:])
        if I + 1 < NB:
            for t in range(4):
                E.dma("sp", "d_x%d" % t, out=X1[t][:],
                      in_=V(x[tok0 + 512 + t * 128:tok0 + 512 + (t + 1) * 128, :], "dram_x"))
        E.barrier()

    with nc.Block() as block:
        @block.sync
        def _(eng):
            E.replay("sp", eng)

        @block.tensor
        def _(eng):
            E.replay("pe", eng)

        @block.scalar
        def _(eng):
            E.replay("act", eng)

        @block.vector
        def _(eng):
            E.replay("dve", eng)

        @block.gpsimd
        def _(eng):
            E.replay("pool", eng)
            E.final_wait(eng, "pool")
    stack.close()
    return nc


def _rel_bucket(d):
    d = np.maximum(d, 0)
    large = 16 + (np.log(np.maximum(d, 1).astype(np.float32) / 16) / math.log(128 / 16) * 16).astype(np.int32)
    large = np.minimum(large, 31)
    return np.where(d < 16, d, large)


def host_consts(inp):
    f32 = np.float32
    c = {}
    c["gmix"] = np.ascontiguousarray(inp["norm_mix"].reshape(8, 128).T).astype(f32)
    c["gffn"] = np.ascontiguousarray(inp["norm_ffn"].reshape(8, 128).T).astype(f32)
    c["gfin"] = np.ascontiguousarray(np.broadcast_to(inp["norm_final"].reshape(1, 1024), (128, 1024))).astype(f32)
    mcw = inp["mlstm_conv_w"].reshape(4, 4, 128)
    c["mcw"] = np.ascontiguousarray(mcw.transpose(2, 1, 0).reshape(128, 16)).astype(f32)
    c["mcb"] = np.ascontiguousarray(inp["mlstm_conv_b"].reshape(4, 128).T).astype(f32)
    gb = np.concatenate([inp["i_bias"].reshape(4), inp["f_bias"].reshape(4)])
    c["gbias"] = np.ascontiguousarray(np.broadcast_to(gb.reshape(1, 8), (128, 8))).astype(f32)
    c["mnorm"] = np.ascontiguousarray(np.broadcast_to(inp["mlstm_norm"].reshape(1, 512), (128, 512))).astype(f32)
    c["gk"] = np.ascontiguousarray(np.broadcast_to(inp["idx_k_norm"].reshape(1, 64), (128, 64))).astype(f32)
    c["relb"] = np.ascontiguousarray(inp["rel_bias"]).astype(f32)
    c["b31"] = np.ascontiguousarray(inp["rel_bias"][31].reshape(8, 1)).astype(f32)
    oh = np.zeros((32, 384), f32)
    for k in range(384):
        d = k - 127
        if d >= 0:
            oh[int(_rel_bucket(np.array([d]))[0]), k] = 1.0
    c["oh"] = oh
    fw = inp["ffn_conv_w"].reshape(3, 44, 128)
    c["fcw"] = np.ascontiguousarray(fw.transpose(2, 1, 0).reshape(128, 132)).astype(f32)
    c["fcb"] = np.ascontiguousarray(inp["ffn_conv_b"].reshape(44, 128).T).astype(f32)
    c["ident"] = np.eye(128, dtype=f32)
    c["jrev"] = np.ascontiguousarray(np.eye(128, dtype=f32)[::-1])
    s = np.arange(128)[:, None]
    l = np.arange(128)[None, :]
    same = (s // 64) == (l // 64)
    tri = (same & (s <= l)).astype(f32)
    blk = same.astype(f32)
    blka = np.broadcast_to((s < 64), (128, 128)).astype(f32)
    blkb = np.broadcast_to((s >= 64), (128, 128)).astype(f32)
    c["tri4"] = np.ascontiguousarray(np.concatenate([tri, blk, blka, blkb], axis=1))
    q = np.arange(128)[:, None]
    kk = np.arange(128)[None, :]
    c["cmask"] = np.where(kk <= q, 0.0, NEG).astype(f32)
    c["hkc"] = np.ascontiguousarray(np.broadcast_to((0.5 ** (np.arange(NITER + 1) + 1)).reshape(1, NITER + 1), (128, NITER + 1))).astype(f32)
    return c


_NC_CACHE = {}


def kernel(**inputs):
    inp = {k: np.asarray(v) for k, v in inputs.items()}
    x = inp["x"]
    B, S, D = x.shape
    NB = S // 512
    topk = min(256, S // 4)
    key = (NB, topk)
    if key not in _NC_CACHE:
        _NC_CACHE[key] = build(NB, topk)
    nc = _NC_CACHE[key]
    c = host_consts(inp)
    shared = dict(c)
    shared["w_in"] = np.ascontiguousarray(inp["w_in"][0])
    shared["w_out"] = np.ascontiguousarray(inp["w_out"][0])
    shared["w_up"] = np.ascontiguousarray(inp["w_up"][0])
    shared["w_down"] = np.ascontiguousarray(inp["w_down"][0])
    in_maps = []
    for b in range(B):
        m = dict(shared)
        m["x"] = np.ascontiguousarray(x[b])
        in_maps.append(m)
    res = run_bass_kernel_spmd(nc, in_maps, core_ids=list(range(B)))
    out = np.stack([np.asarray(r["y"]) for r in res.results], axis=0).astype(np.float32)
    return out
```

```python
import math
import numpy as np
import ml_dtypes
import concourse.bass as bass
import concourse.mybir as mybir
from concourse.bass_utils import run_bass_kernel_spmd

F32 = mybir.dt.float32
BF16 = mybir.dt.bfloat16
AF = mybir.ActivationFunctionType
ALU = mybir.AluOpType
AX = mybir.AxisListType

EPS = 1e-6
import os
DBG_NOOVERLAP = os.environ.get('DBG_NOOVERLAP', '0') == '1'
DBG_NOX1 = os.environ.get('DBG_NOX1', '0') == '1'
NITER = 16
NEG = -1.0e30

WIN_GROUPS = [
    [(0, 512)],
    [(512, 512)],
    [(1024, 512)],
    [(1544, 512)],
    [(2056, 512)],
    [(2568, 512)],
    [(3080, 512)],
    [(3592, 72), (1536, 8)],
]


class V:
    __slots__ = ("ap", "key")

    def __init__(self, ap, key):
        if type(ap).__name__.endswith("Handle"):
            ap = ap[:]
        self.ap = ap
        self.key = key

    def __getitem__(self, idx):
        return V(self.ap[idx], self.key)

    def f(self, fn):
        return V(fn(self.ap), self.key)


class Em:
    ENG = ("pe", "act", "dve", "pool", "sp")

    def __init__(self, nc, stack):
        self.nc = nc
        self.stack = stack
        self.q = {e: [] for e in self.ENG}
        self.cnt = {}
        self.sems = {}
        self.lastw = {}
        self.readers = {}
        self.waited = {e: {} for e in self.ENG}
        for e in ("pe", "act", "dve", "pool"):
            self._sem(e)

    def _sem(self, key):
        if key not in self.sems:
            self.sems[key] = self.stack.enter_context(self.nc.semaphore("s_" + key))
            self.cnt[key] = 0
        return self.sems[key]

    def _emit(self, eng, meth, kw, reads, writes, semkey, inc):
        deps = {}

        def add(d):
            if d is None:
                return
            k, v = d
            if deps.get(k, 0) < v:
                deps[k] = v
        for b in reads:
            add(self.lastw.get(b))
        for b in writes:
            add(self.lastw.get(b))
            for k, v in self.readers.get(b, {}).items():
                add((k, v))
        waits = []
        for k, v in deps.items():
            if eng == "pe" and k == "pe":
                continue
            if self.waited[eng].get(k, 0) >= v:
                continue
            self.waited[eng][k] = v
            waits.append((k, v))
        self._sem(semkey)
        self.cnt[semkey] += inc
        val = self.cnt[semkey]
        self.q[eng].append((meth, kw, waits, semkey, inc))
        for b in reads:
            if b in writes:
                continue
            self.readers.setdefault(b, {})[semkey] = val
        for b in writes:
            self.lastw[b] = (semkey, val)
            self.readers[b] = {}
        return val

    def op(self, eng, meth, **kw):
        reads, writes, real = [], [], {}
        for k, v in kw.items():
            if isinstance(v, V):
                real[k] = v.ap
                keys = v.key if isinstance(v.key, (list, tuple)) else [v.key]
                if k in ("out", "accum_out"):
                    writes.extend(keys)
                else:
                    reads.extend(keys)
            else:
                real[k] = v
        return self._emit(eng, meth, real, reads, writes, eng, 1)

    def dma(self, eng, sem, out, in_, **extra):
        okeys = out.key if isinstance(out.key, (list, tuple)) else [out.key]
        ikeys = in_.key if isinstance(in_.key, (list, tuple)) else [in_.key]
        kw = dict(out=out.ap, in_=in_.ap)
        kw.update(extra)
        return self._emit(eng, "dma_start", kw, list(ikeys), list(okeys), sem, 16)

    def barrier(self):
        cur = dict(self.cnt)
        for e in self.ENG:
            waits = []
            for k, v in cur.items():
                if v == 0 or (e == k):
                    continue
                if self.waited[e].get(k, 0) >= v:
                    continue
                self.waited[e][k] = v
                waits.append((k, v))
            if waits:
                self.q[e].append((None, None, waits, None, 0))

    def replay(self, eng, engobj):
        for meth, kw, waits, semkey, inc in self.q[eng]:
            for k, v in waits:
                engobj.wait_ge(self.sems[k], v)
            if meth is None:
                continue
            ins = getattr(engobj, meth)(**kw)
            ins.then_inc(self.sems[semkey], inc)

    def final_wait(self, engobj, eng):
        for k, v in self.cnt.items():
            if v > 0 and self.waited[eng].get(k, 0) < v:
                engobj.wait_ge(self.sems[k], v)


def build(NB, topk):
    from contextlib import ExitStack
    S = NB * 512
    NT = NB * 4
    nc = bass.Bass("TRN2", target_bir_lowering=False)
    stack = ExitStack()
    E = Em(nc, stack)

    def din(name, shape, dt=F32):
        return nc.dram_tensor(name, list(shape), dt, kind="ExternalInput").ap()

    x = din("x", [S, 1024])
    w_in = din("w_in", [1024, 3664])
    w_out = din("w_out", [1024, 1024])
    w_up = din("w_up", [1024, 5632])
    w_down = din("w_down", [2816, 1024])
    d_gmix = din("gmix", [128, 8])
    d_gffn = din("gffn", [128, 8])
    d_gfin = din("gfin", [128, 1024])
    d_mcw = din("mcw", [128, 16])
    d_mcb = din("mcb", [128, 4])
    d_gbias = din("gbias", [128, 8])
    d_mnorm = din("mnorm", [128, 512])
    d_gk = din("gk", [128, 64])
    d_relb = din("relb", [32, 8])
    d_b31 = din("b31", [8, 1])
    d_oh = din("oh", [32, 384])
    d_fcw = din("fcw", [128, 132])
    d_fcb = din("fcb", [128, 44])
    d_ident = din("ident", [128, 128])
    d_tri4 = din("tri4", [128, 512])
    d_cmask = din("cmask", [128, 128])
    d_hkc = din("hkc", [128, NITER + 1])
    d_jrev = din("jrev", [128, 128])
    y = nc.dram_tensor("y", [S, 1024], F32, kind="ExternalOutput").ap()

    def dscratch(name, shape, dt=BF16):
        return nc.dram_tensor(name, list(shape), dt, kind="Internal").ap()

    sc_in = [dscratch("sc_in%d" % g, [128, 8, 512]) for g in range(8)]
    sc_out = [dscratch("sc_out%d" % g, [128, 8, 512]) for g in range(2)]
    sc_up = [dscratch("sc_up%d" % g, [128, 8, 512]) for g in range(11)]
    sc_dn = [dscratch("sc_dn%d" % g, [128, 8, 512]) for g in range(6)]
    tvd = dscratch("tvd", [8, 384])

    def sb(name, shape, dt):
        return stack.enter_context(nc.sbuf_tensor(name, list(shape), dt))

    KT = V(sb("KT", [128, 4, S], BF16), "KT")
    VP_t = sb("VP", [128, NT, 8, 65], BF16)
    IK = V(sb("IK", [128, S], BF16), "IK")
    WT = V(sb("WT", [128, 8, 256], BF16), "WT")
    WS = [V(sb("WS%d" % i, [128, 8, 512], BF16), "WS%d" % i) for i in range(3)]
    X1_t = sb("X1", [128, 4, 1024], F32)
    X1 = [V(X1_t[:, t, :], "X1_%d" % t) for t in range(4)]
    MIXT = sb("MIXT", [128, 8, 512], BF16)
    GMIX = V(sb("GMIX", [128, 8], F32), "c_gmix")
    GFFN = V(sb("GFFN", [128, 8], F32), "c_gffn")
    MCW = V(sb("MCW", [128, 16], F32), "c_mcw")
    MCB = V(sb("MCB", [128, 4], F32), "c_mcb")
    GBIAS = V(sb("GBIAS", [128, 8], F32), "c_gbias")
    GKB = V(sb("GKB", [128, 64], F32), "c_gk")
    FCW = V(sb("FCW", [128, 132], F32), "c_fcw")
    FCB = V(sb("FCB", [128, 44], F32), "c_fcb")
    IDENT = V(sb("IDENT", [128, 128], F32), "c_ident")
    IDENTB = V(sb("IDENTB", [128, 128], BF16), "c_identb")
    CMASK = V(sb("CMASK", [128, 128], F32), "c_cmask")
    HKC = V(sb("HKC", [128, NITER + 1], F32), "c_hkc")
    DG = V(sb("DG", [128, 8, 128], BF16), "DG")
    RELB = V(sb("RELB", [32, 8], F32), "c_relb")
    B31 = V(sb("B31", [8, 1], F32), "c_b31")
    OH = V(sb("OH", [32, 384], F32), "c_oh")
    ONES = V(sb("ONES", [128, 64], F32), "c_ones")
    CST_t = sb("CST", [128, 2, 129], F32)
    CB_t = sb("CB", [128, 4, 129], BF16)
    HM_t = sb("HM", [128, 4, 3], F32)
    HF_t = sb("HF", [128, 44, 2], F32)
    ST_t = sb("ST", [128, 320], F32)
    ARF = 18432
    AR = sb("ARENA", [128, ARF], F32)

    st_off = [0]

    def st(name, n):
        o = st_off[0]
        st_off[0] += n
        assert st_off[0] <= 320
        return V(ST_t[:, o:o + n], "st_" + name)

    def arv(off_bytes, nbytes, dt, key, pat=None, **kw):
        a = AR[:, off_bytes // 4:(off_bytes + nbytes) // 4]
        if dt == BF16:
            a = a.bitcast(BF16)
        if pat is not None:
            a = a.rearrange(pat, **kw)
        return V(a, key)

    PS = [stack.enter_context(nc.psum_tensor("ps%d" % i, [128, 512], F32)) for i in range(8)]
    psrot = {"mm": [0, 0, 4], "acc": [4, 0, 4], "qk": [0, 0, 6], "acc2": [6, 0, 2]}

    def psum(pool):
        b0, i, n = psrot[pool]
        psrot[pool][1] = (i + 1) % n
        k = b0 + i
        return V(PS[k][:, :], "ps%d" % k)

    def bfv(p):
        return V(p.ap.bitcast(BF16), p.key)

    for dst, src in ((GMIX, d_gmix), (GFFN, d_gffn), (MCW, d_mcw), (MCB, d_mcb), (GBIAS, d_gbias), (GKB, d_gk),
                     (FCW, d_fcw), (FCB, d_fcb), (IDENT, d_ident), (CMASK, d_cmask), (HKC, d_hkc), (RELB, d_relb),
                     (B31, d_b31), (OH, d_oh)):
        E.dma("sp", "d_" + dst.key, out=dst[:], in_=V(src, "dram_" + dst.key))
    E.op("dve", "tensor_copy", out=IDENTB[:], in_=IDENT[:])
    E._emit("dve", "memset", dict(ap=ONES.ap, constant=1.0), [], [ONES.key], "dve", 1)
    E._emit("pool", "memset", dict(ap=CST_t[:, :, :], constant=0.0), [], ["CST0", "CST1"], "pool", 1)
    E._emit("pool", "memset", dict(ap=CB_t[:, :, :], constant=0.0), [], ["CB0", "CB1", "CB2", "CB3"], "pool", 1)
    E._emit("pool", "memset", dict(ap=VP_t[:, :, :, 64:65], constant=1.0), [], ["VPones"], "pool", 1)

    TVS = arv(0, 768, BF16, "tvs")
    NB31 = st("nb31", 1)
    E.op("dve", "tensor_scalar", out=NB31[0:8, :], in0=B31[:], scalar1=-1.0, scalar2=None, op0=ALU.mult)
    pt = psum("mm")
    E.op("pe", "matmul", out=pt[0:8, 0:384], lhsT=RELB[:], rhs=OH[:], start=True, stop=True)
    E.op("act", "activation", out=TVS[0:8, :], in_=pt[0:8, 0:384], func=AF.Exp, bias=NB31[0:8, :], scale=1.0)
    E._emit("dve", "memset", dict(ap=TVS.ap[0:8, 0:127], constant=0.0), ["tvs"], ["tvs"], "dve", 1)
    TVD = V(tvd, "tvd")
    E.dma("sp", "d_tvd", out=TVD[:, :], in_=TVS[0:8, :])
    WTR = arv(1024, 4096, BF16, "WTR", "p (h c) -> p h c", h=8)
    JREVB = arv(5120, 256, BF16, "JREVB")
    JREVF = arv(5632, 512, F32, "JREVF")
    E.dma("sp", "d_jrev", out=JREVF[:], in_=V(d_jrev, "dram_jrev"))
    E.op("dve", "tensor_copy", out=JREVB[:], in_=JREVF[:])
    for h in range(8):
        src = bass.AP(tensor=tvd.tensor, offset=h * 384, ap=[[1, 128], [1, 256]])
        E.dma("sp", "d_wt", out=WTR[:, h, :], in_=V(src, "tvd"))
    for q4 in range(4):
        pw = psum("mm")
        E.op("pe", "matmul", out=pw[:, :], lhsT=JREVB[:],
             rhs=WTR[:, 2 * q4:2 * q4 + 2, :].f(lambda a: a.rearrange("p h c -> p (h c)")), start=True, stop=True)
        E.op("dve", "tensor_copy", out=WT[:, 2 * q4:2 * q4 + 2, :],
             in_=pw[:, :].f(lambda a: a.rearrange("p (h c) -> p h c", h=2)))
    E.barrier()

    STG = [arv(1024 + i * 16384, 16384, F32, "stg%d" % i, "p (a b) -> p a b", a=8) for i in range(3)]
    STB = [arv(1024 + 49152 + i * 8192, 8192, BF16, "stb%d" % i, "p (a b) -> p a b", a=8) for i in range(2)]
    groups = []
    w_in_r = w_in.rearrange("(c p) n -> p c n", p=128)
    w_out_r = w_out.rearrange("(c p) n -> p c n", p=128)
    w_up_r = w_up.rearrange("(c p) n -> p c n", p=128)
    w_dn_r = w_down.rearrange("(c p) n -> p c n", p=128)
    for g, pieces in enumerate(WIN_GROUPS):
        pcs, c0 = [], 0
        for (s0, n) in pieces:
            pcs.append((w_in_r[:, :, s0:s0 + n], c0, n))
            c0 += n
        groups.append((sc_in[g], "sc_in%d" % g, pcs, 8, c0, GMIX))
    for g in range(2):
        groups.append((sc_out[g], "sc_out%d" % g, [(w_out_r[:, :, g * 512:(g + 1) * 512], 0, 512)], 8, 512, None))
    for g in range(11):
        groups.append((sc_up[g], "sc_up%d" % g,
                       [(w_up_r[:, :, 256 * g:256 * g + 256], 0, 256),
                        (w_up_r[:, :, 2816 + 256 * g:2816 + 256 * g + 256], 256, 256)], 8, 512, GFFN))
    for hf in range(2):
        for kg in range(3):
            nk = 8 if kg < 2 else 6
            groups.append((sc_dn[hf * 3 + kg], "sc_dn%d" % (hf * 3 + kg),
                           [(w_dn_r[:, 8 * kg:8 * kg + nk, hf * 512:(hf + 1) * 512], 0, 512)], nk, 512, None))
    cast_engs = ["dve", "act"]
    for gi, (dst, dkey, pcs, kc, ncols, scl) in enumerate(groups):
        sl = gi % 3
        sb2 = gi % 2
        for (src, c0, n) in pcs:
            E.dma("sp", "d_stg%d" % sl, out=STG[sl][:, 0:kc, c0:c0 + n], in_=V(src, "dram_w"))
        eng = cast_engs[gi % 2]
        if scl is None:
            if eng == "act":
                E.op("act", "activation", out=STB[sb2][:, 0:kc, 0:ncols], in_=STG[sl][:, 0:kc, 0:ncols], func=AF.Copy)
            else:
                E.op(eng, "tensor_copy", out=STB[sb2][:, 0:kc, 0:ncols], in_=STG[sl][:, 0:kc, 0:ncols])
        else:
            for c in range(kc):
                if eng == "act":
                    E.op("act", "activation", out=STB[sb2][:, c, 0:ncols], in_=STG[sl][:, c, 0:ncols],
                         func=AF.Copy, scale=scl[:, c:c + 1])
                else:
                    E.op(eng, "tensor_scalar", out=STB[sb2][:, c, 0:ncols], in0=STG[sl][:, c, 0:ncols],
                         scalar1=scl[:, c:c + 1], scalar2=None, op0=ALU.mult)
        E.dma("pool", "d_stb%d" % sb2, out=V(dst[:, 0:kc, 0:ncols], dkey), in_=STB[sb2][:, 0:kc, 0:ncols])
    E.barrier()

    wseq = []
    for I in range(NB):
        for g in range(8):
            wseq.append((sc_in[g], "sc_in%d" % g, 8, 512 if g < 7 else 80))
        for g in range(2):
            wseq.append((sc_out[g], "sc_out%d" % g, 8, 512))
        for g in range(11):
            wseq.append((sc_up[g], "sc_up%d" % g, 8, 512))
        for g in range(6):
            wseq.append((sc_dn[g], "sc_dn%d" % g, 8 if g % 3 < 2 else 6, 512))
    wstate = {"loaded": 0, "next": 0}

    def get_w():
        idx = wstate["next"]
        wstate["next"] += 1
        while wstate["loaded"] <= idx + 2 and wstate["loaded"] < len(wseq):
            li = wstate["loaded"]
            src, skey, kc, ncols = wseq[li]
            slot = li % 3
            E.dma("sp", "d_ws%d" % slot, out=WS[slot][:, 0:kc, 0:ncols], in_=V(src[:, 0:kc, 0:ncols], skey))
            wstate["loaded"] += 1
        return WS[idx % 3]

    HB = arv(0, 2048, BF16, "HB")
    HBA = [HB, arv(38400, 2048, BF16, "HBb")]
    HBE = [HB, arv(67584, 2048, BF16, "HBc")]
    HT = arv(2048, 8192, BF16, "HT", "p (a b) -> p a b", a=8)
    QKT = arv(10240, 4096, BF16, "QKT", "p (a b) -> p a b", a=4)
    YC = [arv(14336 + i * 2048, 2048, F32, "YC%d" % i) for i in range(2)]
    VM_full = AR[:, 18432 // 4:(18432 + 4160) // 4].bitcast(BF16)[:, 0:2064].rearrange("p (t h d) -> p t h d", t=4, h=4)
    VM = V(VM_full, "VM")
    OG = arv(22592, 4096, BF16, "OG", "p (a b) -> p a b", a=4)
    TRI4 = arv(26688, 2048, F32, "TRI4")
    MNORM = arv(28736, 2048, F32, "MNORM")
    IKN = arv(30784, 128, BF16, "IKN")
    KW = arv(30912, 512, BF16, "KW", "p (h d) -> p h d", h=4)
    PTB = [arv(37056 + i * 256, 256, BF16, "PT%d" % i) for i in range(4)]
    HMO = arv(31936, 1024, BF16, "HMO")
    JUNK = arv(32960, 4096, F32, "JUNK")
    AQT = arv(65536, 4096, BF16, "AQT", "p (a b) -> p a b", a=4)
    IQT = arv(69632, 4096, BF16, "IQT", "p (a b) -> p a b", a=4)
    SCK = [V(AR[:, (0 + kc * 2048) // 4:(0 + (kc + 1) * 2048) // 4], "SC%d" % kc) for kc in range(8)]
    SCall = V(AR[:, 0:4096], ["SC%d" % kc for kc in range(8)])
    MQ = arv(16384, 8192, BF16, "MQ")
    MT = arv(24576, 32768, BF16, "MT", "p (j q) -> p j q", j=32)
    EP = [arv(57344 + i * 1024, 1024, BF16, "EP%d" % i) for i in range(8)]
    FT = [arv(i * 2048, 2048, F32, "FT%d" % i) for i in range(4)]
    RB = [arv(61440 + i * 2048, 2048, F32, "RB%d" % i) for i in range(2)]
    RBF = [arv(61440 + i * 1024, 1024, BF16, "RBF%d" % i) for i in range(4)]
    YE = [arv(10240 + i * 2048, 2048, F32, "YE%d" % i) for i in range(4)]
    AT = arv(18432, 22528, BF16, "AT", "p (c q) -> p c q", c=22)
    GFIN = arv(40960, 4096, F32, "GFIN")
    OUTB = [arv(45056 + i * 4096, 4096, F32, "OUTB%d" % i) for i in range(2)]
    JUNK2 = arv(53248, 4096, F32, "JUNK2")
    UE = [arv(57344 + i * 2064, 2056, F32, "UE%d" % i) for i in range(4)]

    SS = [st("ss%d" % t_, 1) for t_ in range(4)]
    RSTD = [st("rstd%d" % t_, 1) for t_ in range(4)]
    SSK = [st("ssk%d" % t_, 1) for t_ in range(4)]
    RK = [st("rk%d" % t_, 1) for t_ in range(4)]
    GATES = [st("gates%d" % t, 8) for t in range(4)]
    WQ = [st("wq%d" % t, 8) for t in range(4)]
    E1 = st("e1", 4)
    NLF = st("nlf", 4)
    GS = st("gs", 16)
    EB8 = st("eb8", 4)
    T1 = st("t1", 4)
    EK = st("ek", 4)
    T2 = st("t2", 4)
    WKs = st("wk", 4)
    EG = st("eg", 8)
    DN = st("dn", 4)
    RR = st("rr", 4)
    MS = st("ms", 4)
    TT = st("tt", 4)
    SCL = st("scl", 4)
    MX = st("mx", 1)
    MN2 = [st("mn0", 1), st("mn1", 1)]
    W0 = st("w0", 1)
    NHK = st("nhk", NITER + 1)
    NM = st("nm", 1)
    SACC = st("sacc", 1)
    SG = st("sg", 1)
    LO = st("lo", 1)
    MID = st("mid", 1)
    CNT = st("cnt", 1)
    TMP = st("tmp", 1)
    SS2 = [st("ss2%d" % t_, 1) for t_ in range(4)]
    RSTD2 = [st("rstd2%d" % t_, 1) for t_ in range(4)]
    SS3 = [st("ss3%d" % t_, 1) for t_ in range(4)]
    RSTD3 = [st("rstd3%d" % t_, 1) for t_ in range(4)]

    CST = [V(CST_t[:, pp, :], "CST%d" % pp) for pp in range(2)]
    CBh = [V(CB_t[:, h, :], "CB%d" % h) for h in range(4)]
    HMh = [V(HM_t[:, c, :], "HM%d" % c) for c in range(4)]
    HFh = [V(HF_t[:, c, :], "HF%d" % c) for c in range(44)]
    MIXc = [V(MIXT[:, c, :], "MIX%d" % c) for c in range(8)]
    MIX03 = V(MIXT[:, 0:4, :], ["MIX0", "MIX1", "MIX2", "MIX3"])

    def rstd_batch(srcs, sss, rvs, junk, n):
        for t_ in range(len(srcs)):
            E.op("act", "activation", out=junk, in_=srcs[t_], func=AF.Square, accum_out=sss[t_])
        for t_ in range(len(srcs)):
            E.op("dve", "tensor_scalar", out=rvs[t_], in0=sss[t_], scalar1=1.0 / n, scalar2=EPS, op0=ALU.mult, op1=ALU.add)
        for t_ in range(len(srcs)):
            E.op("act", "activation", out=rvs[t_], in_=rvs[t_], func=AF.Sqrt)
        for t_ in range(len(srcs)):
            E.op("dve", "reciprocal", out=rvs[t_], in_=rvs[t_])

    def rstd_from_ss(ssv, rv, n):
        E.op("dve", "tensor_scalar", out=rv, in0=ssv, scalar1=1.0 / n, scalar2=EPS, op0=ALU.mult, op1=ALU.add)
        E.op("act", "activation", out=rv, in_=rv, func=AF.Sqrt)
        E.op("dve", "reciprocal", out=rv, in_=rv)

    def transposes_to(dst_view, src, nchunk):
        p = psum("mm")
        pb = bfv(p)
        for c in range(nchunk):
            E.op("pe", "transpose", out=pb[:, c * 128:(c + 1) * 128], in_=src[:, c * 128:(c + 1) * 128],
                 identity=IDENTB[:])
        E.op("act", "activation", out=dst_view,
             in_=pb[:, 0:nchunk * 128].f(lambda a: a.rearrange("p (c q) -> p c q", c=nchunk)), func=AF.Copy)

    rr = {"i": 0}

    def alt(*engs):
        rr["i"] += 1
        return engs[rr["i"] % len(engs)]

    for I in range(NB):
        tok0 = I * 512
        E.dma("sp", "d_tri4", out=TRI4[:], in_=V(d_tri4, "dram_tri4"))
        E.dma("sp", "d_mnorm", out=MNORM[:], in_=V(d_mnorm, "dram_mnorm"))
        if I == 0:
            for t in range(4):
                E.dma("sp", "d_x%d" % t, out=X1[t][:], in_=V(x[tok0 + t * 128:tok0 + (t + 1) * 128, :], "dram_x"))
        rstd_batch([X1[t][:] for t in range(4)], [SS[t][:] for t in range(4)], [RSTD[t][:] for t in range(4)],
                   JUNK[:], 1024.0)
        for t in range(4):
            E.op("dve", "tensor_scalar", out=HBA[t % 2][:], in0=X1[t][:], scalar1=RSTD[t][:], scalar2=None, op0=ALU.mult)
            transposes_to(HT[:, :, t * 128:(t + 1) * 128], HBA[t % 2], 8)

        def fm_group(W, evac):
            for cc in range(4):
                p = psum("mm")
                for c in range(8):
                    E.op("pe", "matmul", out=p[:, :], lhsT=W[:, c, cc * 128:(cc + 1) * 128], rhs=HT[:, c, :],
                         start=(c == 0), stop=(c == 7))
                evac(cc, p)

        def tm_group(W, ncols, evac):
            for t in range(4):
                p = psum("mm")
                for c in range(8):
                    E.op("pe", "matmul", out=p[:, 0:ncols], lhsT=HT[:, c, t * 128:(t + 1) * 128], rhs=W[:, c, 0:ncols],
                         start=(c == 0), stop=(c == 7))
                evac(t, p)

        def ev_g0(cc, p):
            Y = YC[cc % 2]
            E.op("act", "activation", out=Y[:], in_=p[:, :], func=AF.Identity, scale=MCW[:, cc * 4 + 3:cc * 4 + 4],
                 bias=MCB[:, cc:cc + 1])
            for j, sh in ((2, 1), (1, 2), (0, 3)):
                E.op("dve", "scalar_tensor_tensor", out=Y[:, sh:512], in0=p[:, 0:512 - sh],
                     scalar=MCW[:, cc * 4 + j:cc * 4 + j + 1], in1=Y[:, sh:512], op0=ALU.mult, op1=ALU.add)
            if I > 0:
                H = HMh[cc]
                E.op("dve", "scalar_tensor_tensor", out=Y[:, 0:3], in0=H[:, 0:3], scalar=MCW[:, cc * 4:cc * 4 + 1],
                     in1=Y[:, 0:3], op0=ALU.mult, op1=ALU.add)
                E.op("dve", "scalar_tensor_tensor", out=Y[:, 0:2], in0=H[:, 1:3], scalar=MCW[:, cc * 4 + 1:cc * 4 + 2],
                     in1=Y[:, 0:2], op0=ALU.mult, op1=ALU.add)
                E.op("dve", "scalar_tensor_tensor", out=Y[:, 0:1], in0=H[:, 2:3], scalar=MCW[:, cc * 4 + 2:cc * 4 + 3],
                     in1=Y[:, 0:1], op0=ALU.mult, op1=ALU.add)
            E.op("act", "activation", out=HMh[cc][:], in_=p[:, 509:512], func=AF.Copy)
            E.op("act", "activation", out=QKT[:, cc, :], in_=Y[:], func=AF.Silu)
        fm_group(get_w(), ev_g0)

        def ev_g1(t, p):
            E.op("act", "activation", out=VM[:, t, :, 0:128],
                 in_=p[:, :].f(lambda a: a.rearrange("p (h d) -> p h d", h=4)), func=AF.Copy)
        E._emit("pool", "memset", dict(ap=VM.ap[:, :, :, 128:129], constant=1.0), [], ["VM"], "pool", 1)
        tm_group(get_w(), 512, ev_g1)

        def ev_g2(t, p):
            E.op("act", "activation", out=OG[:, t, :], in_=p[:, :], func=AF.Sigmoid)
            E.op("pool", "tensor_tensor", out=OG[:, t, :], in0=OG[:, t, :], in1=MNORM[:], op=ALU.mult)
        tm_group(get_w(), 512, ev_g2)

        def ev_g3(cc, p):
            E.op("act", "activation", out=AQT[:, cc, :], in_=p[:, :], func=AF.Copy, scale=0.125)
        fm_group(get_w(), ev_g3)

        def ev_g4(cc, p):
            E.op("dve", "tensor_copy", out=KT[:, cc, tok0:tok0 + 512], in_=p[:, :])
        fm_group(get_w(), ev_g4)

        def ev_g5(t, p):
            E.op("act", "activation", out=V(VP_t[:, I * 4 + t, :, 0:64], "VP"),
                 in_=p[:, :].f(lambda a: a.rearrange("p (h d) -> p h d", h=8)), func=AF.Copy)
        tm_group(get_w(), 512, ev_g5)

        def ev_g6(cc, p):
            E.op("dve", "tensor_copy", out=IQT[:, cc, :], in_=p[:, :])
        fm_group(get_w(), ev_g6)

        def ev_g7(t, p):
            E.op("act", "activation", out=JUNK[:, 0:64], in_=p[:, 0:64], func=AF.Square, accum_out=SSK[t][:])
            rstd_from_ss(SSK[t][:], RK[t][:], 64.0)
            E.op("dve", "scalar_tensor_tensor", out=IKN[:], in0=p[:, 0:64], scalar=RK[t][:], in1=GKB[:],
                 op0=ALU.mult, op1=ALU.mult)
            E.op("dve", "tensor_scalar", out=WQ[t][:], in0=p[:, 64:72], scalar1=float(8 ** -0.5 * 64 ** -0.5),
                 scalar2=None, op0=ALU.mult)
            E.op("dve", "tensor_tensor", out=GATES[t][:], in0=p[:, 72:80], in1=GBIAS[:], op=ALU.add)
            p2 = psum("mm")
            pb = bfv(p2)
            E.op("pe", "transpose", out=pb[0:64, 0:128], in_=IKN[:], identity=IDENTB[:])
            c0 = tok0 + t * 128
            E.op("dve", "tensor_copy", out=IK[0:64, c0:c0 + 128], in_=pb[0:64, 0:128])
            E.op("dve", "tensor_copy", out=IK[64:128, c0:c0 + 128], in_=pb[0:64, 0:128])
        tm_group(get_w(), 80, ev_g7)

        for t in range(4):
            c0 = t * 128
            pk = psum("mm")
            pkb = bfv(pk)
            for pp in range(2):
                E.op("pe", "transpose", out=pkb[:, pp * 128:(pp + 1) * 128], in_=QKT[:, 2 + pp, c0:c0 + 128],
                     identity=IDENTB[:])
            G = GATES[t]
            E.op("act", "activation", out=E1[:], in_=G[:, 4:8], func=AF.Exp, scale=-1.0)
            E.op("dve", "tensor_scalar", out=E1[:], in0=E1[:], scalar1=1.0, scalar2=None, op0=ALU.add)
            E.op("act", "activation", out=NLF[:], in_=E1[:], func=AF.Ln)
            pg = psum("mm")
            for k in range(4):
                E.op("pe", "matmul", out=pg[:, 4 * k:4 * k + 4], lhsT=TRI4[:, 128 * k:128 * (k + 1)], rhs=NLF[:],
                     start=True, stop=True)
            E.op("dve", "tensor_copy", out=GS[:], in_=pg[:, 0:16])
            E.op("act", "activation", out=EB8[:], in_=GS[:, 0:4], func=AF.Exp, scale=-1.0, bias=-math.log(8.0))
            E.op("dve", "tensor_tensor", out=T1[:], in0=G[:, 0:4], in1=GS[:, 0:4], op=ALU.add)
            E.op("act", "activation", out=EK[:], in_=T1[:], func=AF.Exp)
            E.op("dve", "tensor_tensor", out=T2[:], in0=T1[:], in1=GS[:, 4:8], op=ALU.subtract)
            E.op("act", "activation", out=WKs[:], in_=T2[:], func=AF.Exp)
            E.op("act", "activation", out=EG[:], in_=GS[:, 8:16], func=AF.Exp, scale=-1.0)
            E.op("dve", "tensor_tensor", out=KW[:],
                 in0=pkb[:, 0:256].f(lambda a: a.rearrange("p (h d) -> p h d", h=4)),
                 in1=WKs[:, 0:4].f(lambda a: a.unsqueeze(2).to_broadcast([128, 4, 64])), op=ALU.mult)
            npss = [psum("acc") for _ in range(4)]
            for h in range(4):
                pp, hh = h // 2, h % 2
                base, o = 64 * hh, 0
                pr = psum("mm")
                E.op("pe", "matmul", out=pr[:, 0:128], lhsT=QKT[base:base + 64, 2 + pp, c0:c0 + 128],
                     rhs=QKT[base:base + 64, pp, c0:c0 + 128], start=True, stop=True)
                PT = PTB[h]
                E.op("dve", "scalar_tensor_tensor", out=PT[:], in0=pr[:, 0:128], scalar=EK[:, h:h + 1],
                     in1=TRI4[:, 0:128], op0=ALU.mult, op1=ALU.mult)
                E.op("pe", "matmul", out=npss[h][:, o:o + 129], lhsT=PT[:], rhs=VM[:, t, h, :], start=True, stop=False,
                     skip_group_check=True)
            for ch in range(2):
                r0 = 64 * ch
                pkvs = []
                for h in range(4):
                    pp, hh = h // 2, h % 2
                    base, o = 64 * hh, 0
                    E.op("pe", "matmul", out=npss[h][r0:r0 + 64, o:o + 129], lhsT=QKT[:, pp, c0 + r0:c0 + r0 + 64],
                         rhs=CBh[h][:], start=False, stop=(ch == 1), skip_group_check=True)
                    pkv = psum("mm")
                    E.op("pe", "matmul", out=pkv[base:base + 64, 0:129], lhsT=KW[r0:r0 + 64, h, :],
                         rhs=VM[r0:r0 + 64, t, h, :], start=True, stop=True)
                    pkvs.append(pkv)
                for h in range(4):
                    pp, hh = h // 2, h % 2
                    base = 64 * hh
                    E.op("dve", "scalar_tensor_tensor", out=CST[pp][base:base + 64, :], in0=CST[pp][base:base + 64, :],
                         scalar=EG[base:base + 64, 4 * ch + h:4 * ch + h + 1], in1=pkvs[h][base:base + 64, 0:129],
                         op0=ALU.mult, op1=ALU.add)
                for h in range(4):
                    pp, hh = h // 2, h % 2
                    base = 64 * hh
                    E.op("act", "activation", out=CBh[h][base:base + 64, :], in_=CST[pp][base:base + 64, :],
                         func=AF.Copy)
            for h in range(4):
                E.op("dve", "tensor_tensor", out=DN[:, h:h + 1], in0=npss[h][:, 128:129], in1=EB8[:, h:h + 1], op=ALU.mult)
            E.op("act", "activation", out=DN[:], in_=DN[:], func=AF.Abs)
            E.op("dve", "tensor_scalar", out=DN[:], in0=DN[:], scalar1=1.0, scalar2=None, op0=ALU.max)
            E.op("dve", "reciprocal", out=DN[:], in_=DN[:])
            E.op("dve", "tensor_tensor", out=RR[:], in0=DN[:], in1=EB8[:, 0:4], op=ALU.mult)
            for h in range(4):
                E.op("act", "activation", out=JUNK[:, h * 128:(h + 1) * 128], in_=npss[h][:, 0:128], func=AF.Square,
                     accum_out=MS[:, h:h + 1])
            E.op("dve", "tensor_tensor", out=TT[:], in0=RR[:], in1=RR[:], op=ALU.mult)
            E.op("dve", "tensor_tensor", out=TT[:], in0=TT[:], in1=MS[:], op=ALU.mult)
            E.op("dve", "tensor_scalar", out=TT[:], in0=TT[:], scalar1=1.0 / 128.0, scalar2=EPS, op0=ALU.mult,
                 op1=ALU.add)
            E.op("act", "activation", out=TT[:], in_=TT[:], func=AF.Sqrt)
            E.op("dve", "reciprocal", out=TT[:], in_=TT[:])
            E.op("dve", "tensor_tensor", out=SCL[:], in0=TT[:], in1=RR[:], op=ALU.mult)
            for h in range(4):
                E.op("dve", "scalar_tensor_tensor", out=HMO[:, h * 128:(h + 1) * 128],
                     in0=npss[h][:, 0:128], scalar=SCL[:, h:h + 1], in1=OG[:, t, h * 128:(h + 1) * 128],
                     op0=ALU.mult, op1=ALU.mult)
            transposes_to(MIX03[:, :, c0:c0 + 128], HMO, 4)

        E.barrier()
        nkt = 4 * I + 4
        X1flat = X1_t[:].rearrange("p a b -> p (a b)")
        SCB = [
            ([SCK[kc] for kc in range(8)], SCall),
            ([V(X1_t[:, kc // 2, (kc % 2) * 512:(kc % 2) * 512 + 512], "X1_%d" % (kc // 2)) for kc in range(8)],
             V(X1flat, ["X1_0", "X1_1", "X1_2", "X1_3"])),
        ]

        def score_gen(t):
            i = 4 * I + t
            n = 128 * (i + 1)
            nch = (n + 511) // 512
            sck, scall = SCB[0 if DBG_NOX1 else t % 2]
            for h in range(8):
                E.op("dve", "tensor_scalar", out=DG[:, h, :], in0=IDENT[:], scalar1=WQ[t][:, h:h + 1], scalar2=None,
                     op0=ALU.mult)
            dk = i // 4
            for kc in range(nch):
                w = min(512, n - 512 * kc)
                pacc = psum("acc")
                pis = {}

                def emit_isc(h, kc=kc, w=w):
                    base, pp = 64 * (h % 2), h // 2
                    p = psum("mm")
                    E.op("pe", "matmul", out=p[:, 0:w], lhsT=IQT[base:base + 64, pp, t * 128:(t + 1) * 128],
                         rhs=IK[base:base + 64, 512 * kc:512 * kc + w], start=True, stop=True)
                    pis[h] = p
                for h in range(3):
                    emit_isc(h)
                for h in range(8):
                    if h + 3 < 8:
                        emit_isc(h + 3)
                    p = pis.pop(h)
                    R = RBF[h % 4]
                    if h % 2 == 0:
                        E.op("act", "activation", out=R[:, 0:w], in_=p[:, 0:w], func=AF.Relu)
                    else:
                        E.op("dve", "tensor_scalar", out=R[:, 0:w], in0=p[:, 0:w], scalar1=0.0, scalar2=None, op0=ALU.max)
                    E.op("pe", "matmul", out=pacc[:, 0:w], lhsT=DG[:, h, :], rhs=R[:, 0:w], start=(h == 0),
                         stop=(h == 7), skip_group_check=True)
                    if h == 7:
                        E.op("dve", "tensor_copy", out=sck[kc][:, 0:w], in_=pacc[:, 0:w])
                    yield (h == 7)
            if n > topk:
                E.op("dve", "tensor_reduce", out=MN2[t % 2][:], in_=scall[:, 0:n], axis=AX.X, op=ALU.min)
            E.op("dve", "tensor_tensor", out=sck[dk][:, (i % 4) * 128:(i % 4) * 128 + 128],
                 in0=sck[dk][:, (i % 4) * 128:(i % 4) * 128 + 128], in1=CMASK[:], op=ALU.add)
            yield True

        def bisect_gen(t):
            i = 4 * I + t
            n = 128 * (i + 1)
            sck, scall = SCB[0 if DBG_NOX1 else t % 2]
            MNt = MN2[t % 2]
            if n > topk:
                E.op("dve", "tensor_reduce", out=MX[:], in_=scall[:, 0:n], axis=AX.X, op=ALU.max)
                E.op("dve", "tensor_tensor", out=W0[:], in0=MX[:], in1=MNt[:], op=ALU.subtract)
                E.op("dve", "tensor_scalar", out=W0[:], in0=W0[:], scalar1=1.0001, scalar2=1e-6, op0=ALU.mult, op1=ALU.add)
                E.op("dve", "tensor_scalar", out=NHK[:], in0=HKC[:], scalar1=W0[:], scalar2=-1.0, op0=ALU.mult, op1=ALU.mult)
                E.op("dve", "scalar_tensor_tensor", out=NM[:], in0=MNt[:], scalar=-1.0, in1=NHK[:, 0:1], op0=ALU.mult,
                     op1=ALU.add)
                cthr = float(2 * topk - n) - 0.5
                split = n >= 1536
                if split:
                    nA = ((int(0.56 * n) + 127) // 128) * 128
                    nB = n - nA
                    MQa = V(MQ.ap[:, 0:nA], "MQa")
                    MQb = V(MQ.ap[:, nA:n], "MQb")
                    E.op("dve", "tensor_scalar", out=MID[:], in0=NM[:], scalar1=-1.0, scalar2=None, op0=ALU.mult)
                yield
                for k in range(NITER):
                    if split:
                        E.op("act", "activation", out=MQa, in_=scall[:, 0:nA], func=AF.Sign, bias=NM[:], scale=1.0,
                             accum_out=SACC[:])
                        E.op("dve", "tensor_scalar", out=MQb, in0=scall[:, nA:n], scalar1=MID[:], scalar2=None,
                             op0=ALU.is_ge, op1=ALU.add, accum_out=CNT[:])
                        yield
                        E.op("dve", "tensor_scalar", out=TMP[:], in0=CNT[:], scalar1=2.0, scalar2=-(float(nB) + cthr),
                             op0=ALU.mult, op1=ALU.add)
                        E.op("act", "activation", out=SG[:], in_=SACC[:], func=AF.Sign, bias=TMP[:], scale=1.0)
                    else:
                        E.op("act", "activation", out=MQ[:, 0:n], in_=scall[:, 0:n], func=AF.Sign, bias=NM[:], scale=1.0,
                             accum_out=SACC[:])
                        yield
                        E.op("act", "activation", out=SG[:], in_=SACC[:], func=AF.Sign, bias=-cthr, scale=1.0)
                    E.op("act", "activation", out=NM[:], in_=SG[:], func=AF.Identity, scale=NHK[:, k + 1:k + 2], bias=NM[:])
                    if split and k + 1 < NITER:
                        E.op("dve", "tensor_scalar", out=MID[:], in0=NM[:], scalar1=-1.0, scalar2=None, op0=ALU.mult)
                E.op("act", "activation", out=LO[:], in_=NM[:], func=AF.Identity, scale=-1.0, bias=NHK[:, NITER:NITER + 1])
                E.op("dve", "tensor_scalar", out=V(MQ.ap[:, 0:n], ["MQ", "MQa", "MQb"]), in0=scall[:, 0:n], scalar1=LO[:],
                     scalar2=None, op0=ALU.is_ge)
            else:
                E.op("dve", "tensor_scalar", out=MQ[:, 0:n], in0=scall[:, 0:n], scalar1=-1.0e29, scalar2=None,
                     op0=ALU.is_ge)
            if t < 3:
                E._emit("pool", "memset", dict(ap=MQ.ap[:, n:128 * nkt], constant=0.0), [], ["MQ"], "pool", 1)
            yield "pre_transpose"
            for jg in range((nkt + 3) // 4):
                p = psum("mm")
                pb = bfv(p)
                for jj in range(4):
                    j = 4 * jg + jj
                    E.op("pe", "transpose", out=pb[:, jj * 128:(jj + 1) * 128], in_=MQ[:, 128 * j:128 * (j + 1)],
                         identity=IDENTB[:])
                E.op("dve", "tensor_copy", out=MT[:, 4 * jg:4 * jg + 4, t * 128:(t + 1) * 128],
                     in_=pb[:, 0:512].f(lambda a: a.rearrange("p (j q) -> p j q", j=4)))
            yield

        for _ in score_gen(0):
            pass
        for t in range(4):
            bg = bisect_gen(t)
            sg = score_gen(t + 1) if t + 1 < 4 else None
            nb_steps = NITER + 2
            ns_steps = (8 * ((128 * (4 * I + t + 2) + 511) // 512) + 1) if sg is not None else 0
            ratio = float(ns_steps) / nb_steps
            if DBG_NOOVERLAP:
                ratio = 0.0
            accr = 0.0
            state = {"sg": sg, "bnd": True}

            def adv():
                try:
                    state["bnd"] = bool(next(state["sg"]))
                except StopIteration:
                    state["sg"] = None
                    state["bnd"] = True
            for tag in bg:
                if tag == "pre_transpose":
                    while state["sg"] is not None and not state["bnd"]:
                        adv()
                    continue
                accr += ratio
                while state["sg"] is not None and accr >= 1.0:
                    accr -= 1.0
                    adv()
            while state["sg"] is not None:
                adv()
        for t in range(4):
            E.dma("sp", "d_x%d" % t, out=X1[t][:], in_=V(x[tok0 + t * 128:tok0 + (t + 1) * 128, :], "dram_x"))
        for pp in range(4):
            accs = [psum("acc2"), psum("acc2")]
            qk = {}

            def emit_qk(j, pp=pp):
                for hh in range(2):
                    base = 64 * hh
                    p = psum("qk")
                    E.op("pe", "matmul", out=p[:, :], lhsT=KT[base:base + 64, pp, 128 * j:128 * (j + 1)],
                         rhs=AQT[base:base + 64, pp, :], start=True, stop=True)
                    qk[(j, hh)] = p
            for j in range(min(2, nkt)):
                emit_qk(j)
            for j in range(nkt):
                if j + 2 < nkt:
                    emit_qk(j + 2)
                for hh in range(2):
                    h = 2 * pp + hh
                    p = qk.pop((j, hh))
                    e = EP[(2 * j + hh) % 8]
                    E.op("act", "activation", out=e[:], in_=p[:, :], func=AF.Exp)
                    E.op("dve", "tensor_tensor", out=e[:], in0=e[:], in1=MT[:, j, :], op=ALU.mult)
                    r = j - 4 * I
                    if r >= -1:
                        for tq, v in ((r, 0), (r + 1, 1)):
                            if 0 <= tq <= 3:
                                E.op("dve", "tensor_tensor", out=e[:, tq * 128:(tq + 1) * 128],
                                     in0=e[:, tq * 128:(tq + 1) * 128], in1=WT[:, h, v * 128:(v + 1) * 128], op=ALU.mult)
                    E.op("pe", "matmul", out=accs[hh][0:65, :], lhsT=V(VP_t[:, j, h, :], ["VP", "VPones"]), rhs=e[:],
                         start=(j == 0), stop=(j == nkt - 1))
            for hh in range(2):
                E.op("act", "activation", out=FT[2 * hh][64:65, :], in_=accs[hh][64:65, :], func=AF.Ln)
            for hh in range(2):
                E.op("act", "activation", out=FT[2 * hh][64:65, :], in_=FT[2 * hh][64:65, :], func=AF.Exp, scale=-1.0)
            pbcs = []
            for hh in range(2):
                pbc = psum("qk")
                E.op("pe", "matmul", out=pbc[0:64, :], lhsT=ONES[64:65, 0:64], rhs=FT[2 * hh][64:65, :], start=True,
                     stop=True)
                pbcs.append(pbc)
            for hh in range(2):
                E.op("act", "activation", out=FT[2 * hh + 1][0:64, :], in_=pbcs[hh][0:64, :], func=AF.Copy)
            for hh in range(2):
                base = 64 * hh
                E.op("dve", "tensor_tensor", out=MIXc[4 + pp][base:base + 64, :], in0=accs[hh][0:64, :],
                     in1=FT[2 * hh + 1][0:64, :], op=ALU.mult)

        E.barrier()
        E.dma("sp", "d_gfin", out=GFIN[:], in_=V(d_gfin, "dram_gfin"))
        for hf in range(2):
            W = get_w()
            for t in range(4):
                p = psum("mm")
                for c in range(8):
                    E.op("pe", "matmul", out=p[:, :], lhsT=MIXc[c][:, t * 128:(t + 1) * 128], rhs=W[:, c, :],
                         start=(c == 0), stop=(c == 7))
                E.op("dve", "tensor_tensor", out=X1[t][:, hf * 512:(hf + 1) * 512], in0=p[:, :],
                     in1=X1[t][:, hf * 512:(hf + 1) * 512], op=ALU.add)
        rstd_batch([X1[t][:] for t in range(4)], [SS2[t][:] for t in range(4)], [RSTD2[t][:] for t in range(4)],
                   JUNK2[:], 1024.0)
        for t in range(4):
            E.op("dve", "tensor_scalar", out=HBE[t % 2][:], in0=X1[t][:], scalar1=RSTD2[t][:], scalar2=None, op0=ALU.mult)
            transposes_to(HT[:, :, t * 128:(t + 1) * 128], HBE[t % 2], 8)

        def ffn_conv(p, Y, U, ci):
            E.op("act", "activation", out=Y[:], in_=p[:, :], func=AF.Identity, scale=FCW[:, 3 * ci + 2:3 * ci + 3],
                 bias=FCB[:, ci:ci + 1])
            E.op("act", "activation", out=U[:, 2:514], in_=p[:, :], func=AF.Copy)
            if I > 0:
                E.op("act", "activation", out=U[:, 0:2], in_=HFh[ci][:], func=AF.Copy)
            else:
                E._emit("act", "memzero", dict(ap=U.ap[:, 0:2]), [], [U.key], "act", 1)
            E.op("act", "activation", out=HFh[ci][:], in_=U[:, 512:514], func=AF.Copy)
            E.op("dve", "scalar_tensor_tensor", out=Y[:], in0=U[:, 1:513], scalar=FCW[:, 3 * ci + 1:3 * ci + 2], in1=Y[:],
                 op0=ALU.mult, op1=ALU.add)
            E.op("dve", "scalar_tensor_tensor", out=Y[:], in0=U[:, 0:512], scalar=FCW[:, 3 * ci:3 * ci + 1], in1=Y[:],
                 op0=ALU.mult, op1=ALU.add)

        for g in range(11):
            W = get_w()
            for sub in range(2):
                c = 2 * g + sub
                pg_ = psum("mm")
                pv_ = psum("mm")
                for kc in range(8):
                    E.op("pe", "matmul", out=pg_[:, :], lhsT=W[:, kc, sub * 128:(sub + 1) * 128], rhs=HT[:, kc, :],
                         start=(kc == 0), stop=(kc == 7))
                for kc in range(8):
                    E.op("pe", "matmul", out=pv_[:, :], lhsT=W[:, kc, 256 + sub * 128:256 + (sub + 1) * 128],
                         rhs=HT[:, kc, :], start=(kc == 0), stop=(kc == 7))
                Yg, Yv = YE[(c % 2) * 2], YE[(c % 2) * 2 + 1]
                ffn_conv(pg_, Yg, UE[(c % 2) * 2], c)
                ffn_conv(pv_, Yv, UE[(c % 2) * 2 + 1], 22 + c)
                E.op("act", "activation", out=Yg[:], in_=Yg[:], func=AF.Silu)
                E.op("pool", "tensor_tensor", out=V(AT.ap[:, c, :], "AT%d" % c), in0=Yg[:], in1=Yv[:], op=ALU.mult)
        for hf in range(2):
            accs = [psum("acc") for _ in range(4)]
            for kg in range(3):
                W = get_w()
                nk = 8 if kg < 2 else 6
                for t in range(4):
                    for kk in range(nk):
                        c = 8 * kg + kk
                        E.op("pe", "matmul", out=accs[t][:, :], lhsT=V(AT.ap[:, c, t * 128:(t + 1) * 128], "AT%d" % c), rhs=W[:, kk, :],
                             start=(c == 0), stop=(c == 21), skip_group_check=True)
            for t in range(4):
                E.op("dve", "tensor_tensor", out=X1[t][:, hf * 512:(hf + 1) * 512], in0=accs[t][:, :],
                     in1=X1[t][:, hf * 512:(hf + 1) * 512], op=ALU.add)
        rstd_batch([X1[t][:] for t in range(4)], [SS3[t][:] for t in range(4)], [RSTD3[t][:] for t in range(4)],
                   JUNK2[:], 1024.0)
        for t in range(4):
            ob = OUTB[t % 2]
            E.op("dve", "scalar_tensor_tensor", out=ob[:], in0=X1[t][:], scalar=RSTD3[t][:], in1=GFIN[:],
                 op0=ALU.mult, op1=ALU.mult)
            E.dma("pool", "d_out%d" % (t % 2), out=V(y[tok0 + t * 128:tok0 + (t + 1) * 128, :], "dram_y"), in_=ob[:])
        if I + 1 < NB:
            for t in range(4):
                E.dma("sp", "d_x%d" % t, out=X1[t][:],
                      in_=V(x[tok0 + 512 + t * 128:tok0 + 512 + (t + 1) * 128, :], "dram_x"))
        E.barrier()

    with nc.Block() as block:
        @block.sync
        def _(eng):
            E.replay("sp", eng)

        @block.tensor
        def _(eng):
            E.replay("pe", eng)

        @block.scalar
        def _(eng):
            E.replay("act", eng)

        @block.vector
        def _(eng):
            E.replay("dve", eng)

        @block.gpsimd
        def _(eng):
            E.replay("pool", eng)
            E.final_wait(eng, "pool")
    stack.close()
    return nc


def _rel_bucket(d):
    d = np.maximum(d, 0)
    large = 16 + (np.log(np.maximum(d, 1).astype(np.float32) / 16) / math.log(128 / 16) * 16).astype(np.int32)
    large = np.minimum(large, 31)
    return np.where(d < 16, d, large)


def host_consts(inp):
    f32 = np.float32
    c = {}
    c["gmix"] = np.ascontiguousarray(inp["norm_mix"].reshape(8, 128).T).astype(f32)
    c["gffn"] = np.ascontiguousarray(inp["norm_ffn"].reshape(8, 128).T).astype(f32)
    c["gfin"] = np.ascontiguousarray(np.broadcast_to(inp["norm_final"].reshape(1, 1024), (128, 1024))).astype(f32)
    mcw = inp["mlstm_conv_w"].reshape(4, 4, 128)
    c["mcw"] = np.ascontiguousarray(mcw.transpose(2, 1, 0).reshape(128, 16)).astype(f32)
    c["mcb"] = np.ascontiguousarray(inp["mlstm_conv_b"].reshape(4, 128).T).astype(f32)
    gb = np.concatenate([inp["i_bias"].reshape(4), inp["f_bias"].reshape(4)])
    c["gbias"] = np.ascontiguousarray(np.broadcast_to(gb.reshape(1, 8), (128, 8))).astype(f32)
    c["mnorm"] = np.ascontiguousarray(np.broadcast_to(inp["mlstm_norm"].reshape(1, 512), (128, 512))).astype(f32)
    c["gk"] = np.ascontiguousarray(np.broadcast_to(inp["idx_k_norm"].reshape(1, 64), (128, 64))).astype(f32)
    c["relb"] = np.ascontiguousarray(inp["rel_bias"]).astype(f32)
    c["b31"] = np.ascontiguousarray(inp["rel_bias"][31].reshape(8, 1)).astype(f32)
    oh = np.zeros((32, 384), f32)
    for k in range(384):
        d = k - 127
        if d >= 0:
            oh[int(_rel_bucket(np.array([d]))[0]), k] = 1.0
    c["oh"] = oh
    fw = inp["ffn_conv_w"].reshape(3, 44, 128)
    c["fcw"] = np.ascontiguousarray(fw.transpose(2, 1, 0).reshape(128, 132)).astype(f32)
    c["fcb"] = np.ascontiguousarray(inp["ffn_conv_b"].reshape(44, 128).T).astype(f32)
    c["ident"] = np.eye(128, dtype=f32)
    c["jrev"] = np.ascontiguousarray(np.eye(128, dtype=f32)[::-1])
    s = np.arange(128)[:, None]
    l = np.arange(128)[None, :]
    same = (s // 64) == (l // 64)
    tri = (same & (s <= l)).astype(f32)
    blk = same.astype(f32)
    blka = np.broadcast_to((s < 64), (128, 128)).astype(f32)
    blkb = np.broadcast_to((s >= 64), (128, 128)).astype(f32)
    c["tri4"] = np.ascontiguousarray(np.concatenate([tri, blk, blka, blkb], axis=1))
    q = np.arange(128)[:, None]
    kk = np.arange(128)[None, :]
    c["cmask"] = np.where(kk <= q, 0.0, NEG).astype(f32)
    c["hkc"] = np.ascontiguousarray(np.broadcast_to((0.5 ** (np.arange(NITER + 1) + 1)).reshape(1, NITER + 1), (128, NITER + 1))).astype(f32)
    return c


_NC_CACHE = {}


def kernel(**inputs):
    inp = {k: np.asarray(v) for k, v in inputs.items()}
    x = inp["x"]
    B, S, D = x.shape
    NB = S // 512
    topk = min(256, S // 4)
    key = (NB, topk)
    if key not in _NC_CACHE:
        _NC_CACHE[key] = build(NB, topk)
    nc = _NC_CACHE[key]
    c = host_consts(inp)
    shared = dict(c)
    shared["w_in"] = np.ascontiguousarray(inp["w_in"][0])
    shared["w_out"] = np.ascontiguousarray(inp["w_out"][0])
    shared["w_up"] = np.ascontiguousarray(inp["w_up"][0])
    shared["w_down"] = np.ascontiguousarray(inp["w_down"][0])
    in_maps = []
    for b in range(B):
        m = dict(shared)
        m["x"] = np.ascontiguousarray(x[b])
        in_maps.append(m)
    res = run_bass_kernel_spmd(nc, in_maps, core_ids=list(range(B)))
    out = np.stack([np.asarray(r["y"]) for r in res.results], axis=0).astype(np.float32)
    return out
```

```python
import math
import numpy as np
import ml_dtypes
import concourse.bass as bass
import concourse.mybir as mybir
from concourse.bass_utils import run_bass_kernel_spmd

F32 = mybir.dt.float32
BF16 = mybir.dt.bfloat16
AF = mybir.ActivationFunctionType
ALU = mybir.AluOpType
AX = mybir.AxisListType

EPS = 1e-6
import os
DBG_NOOVERLAP = os.environ.get('DBG_NOOVERLAP', '0') == '1'
DBG_NOX1 = os.environ.get('DBG_NOX1', '0') == '1'
NITER = 16
NEG = -1.0e30

WIN_GROUPS = [
    [(0, 512)],
    [(512, 512)],
    [(1024, 512)],
    [(1544, 512)],
    [(2056, 512)],
    [(2568, 512)],
    [(3080, 512)],
    [(3592, 72), (1536, 8)],
]


class V:
    __slots__ = ("ap", "key")

    def __init__(self, ap, key):
        if type(ap).__name__.endswith("Handle"):
            ap = ap[:]
        self.ap = ap
        self.key = key

    def __getitem__(self, idx):
        return V(self.ap[idx], self.key)

    def f(self, fn):
        return V(fn(self.ap), self.key)


class Em:
    ENG = ("pe", "act", "dve", "pool", "sp")

    def __init__(self, nc, stack):
        self.nc = nc
        self.stack = stack
        self.q = {e: [] for e in self.ENG}
        self.cnt = {}
        self.sems = {}
        self.lastw = {}
        self.readers = {}
        self.waited = {e: {} for e in self.ENG}
        for e in ("pe", "act", "dve", "pool"):
            self._sem(e)

    def _sem(self, key):
        if key not in self.sems:
            self.sems[key] = self.stack.enter_context(self.nc.semaphore("s_" + key))
            self.cnt[key] = 0
        return self.sems[key]

    def _emit(self, eng, meth, kw, reads, writes, semkey, inc):
        deps = {}

        def add(d):
            if d is None:
                return
            k, v = d
            if deps.get(k, 0) < v:
                deps[k] = v
        for b in reads:
            add(self.lastw.get(b))
        for b in writes:
            add(self.lastw.get(b))
            for k, v in self.readers.get(b, {}).items():
                add((k, v))
        waits = []
        for k, v in deps.items():
            if eng == "pe" and k == "pe":
                continue
            if self.waited[eng].get(k, 0) >= v:
                continue
            self.waited[eng][k] = v
            waits.append((k, v))
        self._sem(semkey)
        self.cnt[semkey] += inc
        val = self.cnt[semkey]
        self.q[eng].append((meth, kw, waits, semkey, inc))
        for b in reads:
            if b in writes:
                continue
            self.readers.setdefault(b, {})[semkey] = val
        for b in writes:
            self.lastw[b] = (semkey, val)
            self.readers[b] = {}
        return val

    def op(self, eng, meth, **kw):
        reads, writes, real = [], [], {}
        for k, v in kw.items():
            if isinstance(v, V):
                real[k] = v.ap
                keys = v.key if isinstance(v.key, (list, tuple)) else [v.key]
                if k in ("out", "accum_out"):
                    writes.extend(keys)
                else:
                    reads.extend(keys)
            else:
                real[k] = v
        return self._emit(eng, meth, real, reads, writes, eng, 1)

    def dma(self, eng, sem, out, in_, **extra):
        okeys = out.key if isinstance(out.key, (list, tuple)) else [out.key]
        ikeys = in_.key if isinstance(in_.key, (list, tuple)) else [in_.key]
        kw = dict(out=out.ap, in_=in_.ap)
        kw.update(extra)
        return self._emit(eng, "dma_start", kw, list(ikeys), list(okeys), sem, 16)

    def barrier(self):
        cur = dict(self.cnt)
        for e in self.ENG:
            waits = []
            for k, v in cur.items():
                if v == 0 or (e == k):
                    continue
                if self.waited[e].get(k, 0) >= v:
                    continue
                self.waited[e][k] = v
                waits.append((k, v))
            if waits:
                self.q[e].append((None, None, waits, None, 0))

    def replay(self, eng, engobj):
        for meth, kw, waits, semkey, inc in self.q[eng]:
            for k, v in waits:
                engobj.wait_ge(self.sems[k], v)
            if meth is None:
                continue
            ins = getattr(engobj, meth)(**kw)
            ins.then_inc(self.sems[semkey], inc)

    def final_wait(self, engobj, eng):
        for k, v in self.cnt.items():
            if v > 0 and self.waited[eng].get(k, 0) < v:
                engobj.wait_ge(self.sems[k], v)


def build(NB, topk):
    from contextlib import ExitStack
    S = NB * 512
    NT = NB * 4
    nc = bass.Bass("TRN2", target_bir_lowering=False)
    stack = ExitStack()
    E = Em(nc, stack)

    def din(name, shape, dt=F32):
        return nc.dram_tensor(name, list(shape), dt, kind="ExternalInput").ap()

    x = din("x", [S, 1024])
    w_in = din("w_in", [1024, 3664])
    w_out = din("w_out", [1024, 1024])
    w_up = din("w_up", [1024, 5632])
    w_down = din("w_down", [2816, 1024])
    d_gmix = din("gmix", [128, 8])
    d_gffn = din("gffn", [128, 8])
    d_gfin = din("gfin", [128, 1024])
    d_mcw = din("mcw", [128, 16])
    d_mcb = din("mcb", [128, 4])
    d_gbias = din("gbias", [128, 8])
    d_mnorm = din("mnorm", [128, 512])
    d_gk = din("gk", [128, 64])
    d_relb = din("relb", [32, 8])
    d_b31 = din("b31", [8, 1])
    d_oh = din("oh", [32, 384])
    d_fcw = din("fcw", [128, 132])
    d_fcb = din("fcb", [128, 44])
    d_ident = din("ident", [128, 128])
    d_tri4 = din("tri4", [128, 512])
    d_cmask = din("cmask", [128, 128])
    d_hkc = din("hkc", [128, NITER + 1])
    d_jrev = din("jrev", [128, 128])
    y = nc.dram_tensor("y", [S, 1024], F32, kind="ExternalOutput").ap()

    def dscratch(name, shape, dt=BF16):
        return nc.dram_tensor(name, list(shape), dt, kind="Internal").ap()

    sc_in = [dscratch("sc_in%d" % g, [128, 8, 512]) for g in range(8)]
    sc_out = [dscratch("sc_out%d" % g, [128, 8, 512]) for g in range(2)]
    sc_up = [dscratch("sc_up%d" % g, [128, 8, 512]) for g in range(11)]
    sc_dn = [dscratch("sc_dn%d" % g, [128, 8, 512]) for g in range(6)]
    tvd = dscratch("tvd", [8, 384])

    def sb(name, shape, dt):
        return stack.enter_context(nc.sbuf_tensor(name, list(shape), dt))

    KT = V(sb("KT", [128, 4, S], BF16), "KT")
    VP_t = sb("VP", [128, NT, 8, 65], BF16)
    IK = V(sb("IK", [128, S], BF16), "IK")
    WT = V(sb("WT", [128, 8, 256], BF16), "WT")
    WS = [V(sb("WS%d" % i, [128, 8, 512], BF16), "WS%d" % i) for i in range(3)]
    X1_t = sb("X1", [128, 4, 1024], F32)
    X1 = [V(X1_t[:, t, :], "X1_%d" % t) for t in range(4)]
    MIXT = sb("MIXT", [128, 8, 512], BF16)
    GMIX = V(sb("GMIX", [128, 8], F32), "c_gmix")
    GFFN = V(sb("GFFN", [128, 8], F32), "c_gffn")
    MCW = V(sb("MCW", [128, 16], F32), "c_mcw")
    MCB = V(sb("MCB", [128, 4], F32), "c_mcb")
    GBIAS = V(sb("GBIAS", [128, 8], F32), "c_gbias")
    GKB = V(sb("GKB", [128, 64], F32), "c_gk")
    FCW = V(sb("FCW", [128, 132], F32), "c_fcw")
    FCB = V(sb("FCB", [128, 44], F32), "c_fcb")
    IDENT = V(sb("IDENT", [128, 128], F32), "c_ident")
    IDENTB = V(sb("IDENTB", [128, 128], BF16), "c_identb")
    CMASK = V(sb("CMASK", [128, 128], F32), "c_cmask")
    HKC = V(sb("HKC", [128, NITER + 1], F32), "c_hkc")
    DG = V(sb("DG", [128, 8, 128], BF16), "DG")
    RELB = V(sb("RELB", [32, 8], F32), "c_relb")
    B31 = V(sb("B31", [8, 1], F32), "c_b31")
    OH = V(sb("OH", [32, 384], F32), "c_oh")
    ONES = V(sb("ONES", [128, 64], F32), "c_ones")
    CST_t = sb("CST", [128, 2, 129], F32)
    CB_t = sb("CB", [128, 4, 129], BF16)
    HM_t = sb("HM", [128, 4, 3], F32)
    HF_t = sb("HF", [128, 44, 2], F32)
    ST_t = sb("ST", [128, 320], F32)
    ARF = 18432
    AR = sb("ARENA", [128, ARF], F32)

    st_off = [0]

    def st(name, n):
        o = st_off[0]
        st_off[0] += n
        assert st_off[0] <= 320
        return V(ST_t[:, o:o + n], "st_" + name)

    def arv(off_bytes, nbytes, dt, key, pat=None, **kw):
        a = AR[:, off_bytes // 4:(off_bytes + nbytes) // 4]
        if dt == BF16:
            a = a.bitcast(BF16)
        if pat is not None:
            a = a.rearrange(pat, **kw)
        return V(a, key)

    PS = [stack.enter_context(nc.psum_tensor("ps%d" % i, [128, 512], F32)) for i in range(8)]
    psrot = {"mm": [0, 0, 4], "acc": [4, 0, 4], "qk": [0, 0, 6], "acc2": [6, 0, 2]}

    def psum(pool):
        b0, i, n = psrot[pool]
        psrot[pool][1] = (i + 1) % n
        k = b0 + i
        return V(PS[k][:, :], "ps%d" % k)

    def bfv(p):
        return V(p.ap.bitcast(BF16), p.key)

    for dst, src in ((GMIX, d_gmix), (GFFN, d_gffn), (MCW, d_mcw), (MCB, d_mcb), (GBIAS, d_gbias), (GKB, d_gk),
                     (FCW, d_fcw), (FCB, d_fcb), (IDENT, d_ident), (CMASK, d_cmask), (HKC, d_hkc), (RELB, d_relb),
                     (B31, d_b31), (OH, d_oh)):
        E.dma("sp", "d_" + dst.key, out=dst[:], in_=V(src, "dram_" + dst.key))
    E.op("dve", "tensor_copy", out=IDENTB[:], in_=IDENT[:])
    E._emit("dve", "memset", dict(ap=ONES.ap, constant=1.0), [], [ONES.key], "dve", 1)
    E._emit("pool", "memset", dict(ap=CST_t[:, :, :], constant=0.0), [], ["CST0", "CST1"], "pool", 1)
    E._emit("pool", "memset", dict(ap=CB_t[:, :, :], constant=0.0), [], ["CB0", "CB1", "CB2", "CB3"], "pool", 1)
    E._emit("pool", "memset", dict(ap=VP_t[:, :, :, 64:65], constant=1.0), [], ["VPones"], "pool", 1)

    TVS = arv(0, 768, BF16, "tvs")
    NB31 = st("nb31", 1)
    E.op("dve", "tensor_scalar", out=NB31[0:8, :], in0=B31[:], scalar1=-1.0, scalar2=None, op0=ALU.mult)
    pt = psum("mm")
    E.op("pe", "matmul", out=pt[0:8, 0:384], lhsT=RELB[:], rhs=OH[:], start=True, stop=True)
    E.op("act", "activation", out=TVS[0:8, :], in_=pt[0:8, 0:384], func=AF.Exp, bias=NB31[0:8, :], scale=1.0)
    E._emit("dve", "memset", dict(ap=TVS.ap[0:8, 0:127], constant=0.0), ["tvs"], ["tvs"], "dve", 1)
    TVD = V(tvd, "tvd")
    E.dma("sp", "d_tvd", out=TVD[:, :], in_=TVS[0:8, :])
    WTR = arv(1024, 4096, BF16, "WTR", "p (h c) -> p h c", h=8)
    JREVB = arv(5120, 256, BF16, "JREVB")
    JREVF = arv(5632, 512, F32, "JREVF")
    E.dma("sp", "d_jrev", out=JREVF[:], in_=V(d_jrev, "dram_jrev"))
    E.op("dve", "tensor_copy", out=JREVB[:], in_=JREVF[:])
    for h in range(8):
        src = bass.AP(tensor=tvd.tensor, offset=h * 384, ap=[[1, 128], [1, 256]])
        E.dma("sp", "d_wt", out=WTR[:, h, :], in_=V(src, "tvd"))
    for q4 in range(4):
        pw = psum("mm")
        E.op("pe", "matmul", out=pw[:, :], lhsT=JREVB[:],
             rhs=WTR[:, 2 * q4:2 * q4 + 2, :].f(lambda a: a.rearrange("p h c -> p (h c)")), start=True, stop=True)
        E.op("dve", "tensor_copy", out=WT[:, 2 * q4:2 * q4 + 2, :],
             in_=pw[:, :].f(lambda a: a.rearrange("p (h c) -> p h c", h=2)))
    E.barrier()

    STG = [arv(1024 + i * 16384, 16384, F32, "stg%d" % i, "p (a b) -> p a b", a=8) for i in range(3)]
    STB = [arv(1024 + 49152 + i * 8192, 8192, BF16, "stb%d" % i, "p (a b) -> p a b", a=8) for i in range(2)]
    groups = []
    w_in_r = w_in.rearrange("(c p) n -> p c n", p=128)
    w_out_r = w_out.rearrange("(c p) n -> p c n", p=128)
    w_up_r = w_up.rearrange("(c p) n -> p c n", p=128)
    w_dn_r = w_down.rearrange("(c p) n -> p c n", p=128)
    for g, pieces in enumerate(WIN_GROUPS):
        pcs, c0 = [], 0
        for (s0, n) in pieces:
            pcs.append((w_in_r[:, :, s0:s0 + n], c0, n))
            c0 += n
        groups.append((sc_in[g], "sc_in%d" % g, pcs, 8, c0, GMIX))
    for g in range(2):
        groups.append((sc_out[g], "sc_out%d" % g, [(w_out_r[:, :, g * 512:(g + 1) * 512], 0, 512)], 8, 512, None))
    for g in range(11):
        groups.append((sc_up[g], "sc_up%d" % g,
                       [(w_up_r[:, :, 256 * g:256 * g + 256], 0, 256),
                        (w_up_r[:, :, 2816 + 256 * g:2816 + 256 * g + 256], 256, 256)], 8, 512, GFFN))
    for hf in range(2):
        for kg in range(3):
            nk = 8 if kg < 2 else 6
            groups.append((sc_dn[hf * 3 + kg], "sc_dn%d" % (hf * 3 + kg),
                           [(w_dn_r[:, 8 * kg:8 * kg + nk, hf * 512:(hf + 1) * 512], 0, 512)], nk, 512, None))
    cast_engs = ["dve", "act"]
    for gi, (dst, dkey, pcs, kc, ncols, scl) in enumerate(groups):
        sl = gi % 3
        sb2 = gi % 2
        for (src, c0, n) in pcs:
            E.dma("sp", "d_stg%d" % sl, out=STG[sl][:, 0:kc, c0:c0 + n], in_=V(src, "dram_w"))
        eng = cast_engs[gi % 2]
        if scl is None:
            if eng == "act":
                E.op("act", "activation", out=STB[sb2][:, 0:kc, 0:ncols], in_=STG[sl][:, 0:kc, 0:ncols], func=AF.Copy)
            else:
                E.op(eng, "tensor_copy", out=STB[sb2][:, 0:kc, 0:ncols], in_=STG[sl][:, 0:kc, 0:ncols])
        else:
            for c in range(kc):
                if eng == "act":
                    E.op("act", "activation", out=STB[sb2][:, c, 0:ncols], in_=STG[sl][:, c, 0:ncols],
                         func=AF.Copy, scale=scl[:, c:c + 1])
                else:
                    E.op(eng, "tensor_scalar", out=STB[sb2][:, c, 0:ncols], in0=STG[sl][:, c, 0:ncols],
                         scalar1=scl[:, c:c + 1], scalar2=None, op0=ALU.mult)
        E.dma("pool", "d_stb%d" % sb2, out=V(dst[:, 0:kc, 0:ncols], dkey), in_=STB[sb2][:, 0:kc, 0:ncols])
    E.barrier()

    wseq = []
    for I in range(NB):
        for g in range(8):
            wseq.append((sc_in[g], "sc_in%d" % g, 8, 512 if g < 7 else 80))
        for g in range(2):
            wseq.append((sc_out[g], "sc_out%d" % g, 8, 512))
        for g in range(11):
            wseq.append((sc_up[g], "sc_up%d" % g, 8, 512))
        for g in range(6):
            wseq.append((sc_dn[g], "sc_dn%d" % g, 8 if g % 3 < 2 else 6, 512))
    wstate = {"loaded": 0, "next": 0}

    def get_w():
        idx = wstate["next"]
        wstate["next"] += 1
        while wstate["loaded"] <= idx + 2 and wstate["loaded"] < len(wseq):
            li = wstate["loaded"]
            src, skey, kc, ncols = wseq[li]
            slot = li % 3
            E.dma("sp", "d_ws%d" % slot, out=WS[slot][:, 0:kc, 0:ncols], in_=V(src[:, 0:kc, 0:ncols], skey))
            wstate["loaded"] += 1
        return WS[idx % 3]

    HB = arv(0, 2048, BF16, "HB")
    HBA = [HB, arv(38400, 2048, BF16, "HBb")]
    HBE = [HB, arv(67584, 2048, BF16, "HBc")]
    HT = arv(2048, 8192, BF16, "HT", "p (a b) -> p a b", a=8)
    QKT = arv(10240, 4096, BF16, "QKT", "p (a b) -> p a b", a=4)
    YC = [arv(14336 + i * 2048, 2048, F32, "YC%d" % i) for i in range(2)]
    VM_full = AR[:, 18432 // 4:(18432 + 4160) // 4].bitcast(BF16)[:, 0:2064].rearrange("p (t h d) -> p t h d", t=4, h=4)
    VM = V(VM_full, "VM")
    OG = arv(22592, 4096, BF16, "OG", "p (a b) -> p a b", a=4)
    TRI4 = arv(26688, 2048, F32, "TRI4")
    MNORM = arv(28736, 2048, F32, "MNORM")
    IKN = arv(30784, 128, BF16, "IKN")
    KW = arv(30912, 512, BF16, "KW", "p (h d) -> p h d", h=4)
    PTB = [arv(37056 + i * 256, 256, BF16, "PT%d" % i) for i in range(4)]
    HMO = arv(31936, 1024, BF16, "HMO")
    JUNK = arv(32960, 4096, F32, "JUNK")
    AQT = arv(65536, 4096, BF16, "AQT", "p (a b) -> p a b", a=4)
    IQT = arv(69632, 4096, BF16, "IQT", "p (a b) -> p a b", a=4)
    SCK = [V(AR[:, (0 + kc * 2048) // 4:(0 + (kc + 1) * 2048) // 4], "SC%d" % kc) for kc in range(8)]
    SCall = V(AR[:, 0:4096], ["SC%d" % kc for kc in range(8)])
    MQ = arv(16384, 8192, BF16, "MQ")
    MT = arv(24576, 32768, BF16, "MT", "p (j q) -> p j q", j=32)
    EP = [arv(57344 + i * 1024, 1024, BF16, "EP%d" % i) for i in range(8)]
    FT = [arv(i * 2048, 2048, F32, "FT%d" % i) for i in range(4)]
    RB = [arv(61440 + i * 2048, 2048, F32, "RB%d" % i) for i in range(2)]
    RBF = [arv(61440 + i * 1024, 1024, BF16, "RBF%d" % i) for i in range(4)]
    YE = [arv(10240 + i * 2048, 2048, F32, "YE%d" % i) for i in range(4)]
    AT = arv(18432, 22528, BF16, "AT", "p (c q) -> p c q", c=22)
    GFIN = arv(40960, 4096, F32, "GFIN")
    OUTB = [arv(45056 + i * 4096, 4096, F32, "OUTB%d" % i) for i in range(2)]
    JUNK2 = arv(53248, 4096, F32, "JUNK2")
    UE = [arv(57344 + i * 2064, 2056, F32, "UE%d" % i) for i in range(4)]

    SS = [st("ss%d" % t_, 1) for t_ in range(4)]
    RSTD = [st("rstd%d" % t_, 1) for t_ in range(4)]
    SSK = [st("ssk%d" % t_, 1) for t_ in range(4)]
    RK = [st("rk%d" % t_, 1) for t_ in range(4)]
    GATES = [st("gates%d" % t, 8) for t in range(4)]
    WQ = [st("wq%d" % t, 8) for t in range(4)]
    E1 = st("e1", 4)
    NLF = st("nlf", 4)
    GS = st("gs", 16)
    EB8 = st("eb8", 4)
    T1 = st("t1", 4)
    EK = st("ek", 4)
    T2 = st("t2", 4)
    WKs = st("wk", 4)
    EG = st("eg", 8)
    DN = st("dn", 4)
    RR = st("rr", 4)
    MS = st("ms", 4)
    TT = st("tt", 4)
    SCL = st("scl", 4)
    MX = st("mx", 1)
    MN2 = [st("mn0", 1), st("mn1", 1)]
    W0 = st("w0", 1)
    NHK = st("nhk", NITER + 1)
    NM = st("nm", 1)
    SACC = st("sacc", 1)
    SG = st("sg", 1)
    LO = st("lo", 1)
    MID = st("mid", 1)
    CNT = st("cnt", 1)
    TMP = st("tmp", 1)
    SS2 = [st("ss2%d" % t_, 1) for t_ in range(4)]
    RSTD2 = [st("rstd2%d" % t_, 1) for t_ in range(4)]
    SS3 = [st("ss3%d" % t_, 1) for t_ in range(4)]
    RSTD3 = [st("rstd3%d" % t_, 1) for t_ in range(4)]

    CST = [V(CST_t[:, pp, :], "CST%d" % pp) for pp in range(2)]
    CBh = [V(CB_t[:, h, :], "CB%d" % h) for h in range(4)]
    HMh = [V(HM_t[:, c, :], "HM%d" % c) for c in range(4)]
    HFh = [V(HF_t[:, c, :], "HF%d" % c) for c in range(44)]
    MIXc = [V(MIXT[:, c, :], "MIX%d" % c) for c in range(8)]
    MIX03 = V(MIXT[:, 0:4, :], ["MIX0", "MIX1", "MIX2", "MIX3"])

    def rstd_batch(srcs, sss, rvs, junk, n):
        for t_ in range(len(srcs)):
            E.op("act", "activation", out=junk, in_=srcs[t_], func=AF.Square, accum_out=sss[t_])
        for t_ in range(len(srcs)):
            E.op("dve", "tensor_scalar", out=rvs[t_], in0=sss[t_], scalar1=1.0 / n, scalar2=EPS, op0=ALU.mult, op1=ALU.add)
        for t_ in range(len(srcs)):
            E.op("act", "activation", out=rvs[t_], in_=rvs[t_], func=AF.Sqrt)
        for t_ in range(len(srcs)):
            E.op("dve", "reciprocal", out=rvs[t_], in_=rvs[t_])

    def rstd_from_ss(ssv, rv, n):
        E.op("dve", "tensor_scalar", out=rv, in0=ssv, scalar1=1.0 / n, scalar2=EPS, op0=ALU.mult, op1=ALU.add)
        E.op("act", "activation", out=rv, in_=rv, func=AF.Sqrt)
        E.op("dve", "reciprocal", out=rv, in_=rv)

    def transposes_to(dst_view, src, nchunk):
        p = psum("mm")
        pb = bfv(p)
        for c in range(nchunk):
            E.op("pe", "transpose", out=pb[:, c * 128:(c + 1) * 128], in_=src[:, c * 128:(c + 1) * 128],
                 identity=IDENTB[:])
        E.op("act", "activation", out=dst_view,
             in_=pb[:, 0:nchunk * 128].f(lambda a: a.rearrange("p (c q) -> p c q", c=nchunk)), func=AF.Copy)

    rr = {"i": 0}

    def alt(*engs):
        rr["i"] += 1
        return engs[rr["i"] % len(engs)]

    for I in range(NB):
        tok0 = I * 512
        E.dma("sp", "d_tri4", out=TRI4[:], in_=V(d_tri4, "dram_tri4"))
        E.dma("sp", "d_mnorm", out=MNORM[:], in_=V(d_mnorm, "dram_mnorm"))
        if I == 0:
            for t in range(4):
                E.dma("sp", "d_x%d" % t, out=X1[t][:], in_=V(x[tok0 + t * 128:tok0 + (t + 1) * 128, :], "dram_x"))
        rstd_batch([X1[t][:] for t in range(4)], [SS[t][:] for t in range(4)], [RSTD[t][:] for t in range(4)],
                   JUNK[:], 1024.0)
        for t in range(4):
            E.op("dve", "tensor_scalar", out=HBA[t % 2][:], in0=X1[t][:], scalar1=RSTD[t][:], scalar2=None, op0=ALU.mult)
            transposes_to(HT[:, :, t * 128:(t + 1) * 128], HBA[t % 2], 8)

        def fm_group(W, evac):
            for cc in range(4):
                p = psum("mm")
                for c in range(8):
                    E.op("pe", "matmul", out=p[:, :], lhsT=W[:, c, cc * 128:(cc + 1) * 128], rhs=HT[:, c, :],
                         start=(c == 0), stop=(c == 7))
                evac(cc, p)

        def tm_group(W, ncols, evac):
            for t in range(4):
                p = psum("mm")
                for c in range(8):
                    E.op("pe", "matmul", out=p[:, 0:ncols], lhsT=HT[:, c, t * 128:(t + 1) * 128], rhs=W[:, c, 0:ncols],
                         start=(c == 0), stop=(c == 7))
                evac(t, p)

        def ev_g0(cc, p):
            Y = YC[cc % 2]
            E.op("act", "activation", out=Y[:], in_=p[:, :], func=AF.Identity, scale=MCW[:, cc * 4 + 3:cc * 4 + 4],
                 bias=MCB[:, cc:cc + 1])
            for j, sh in ((2, 1), (1, 2), (0, 3)):
                E.op("dve", "scalar_tensor_tensor", out=Y[:, sh:512], in0=p[:, 0:512 - sh],
                     scalar=MCW[:, cc * 4 + j:cc * 4 + j + 1], in1=Y[:, sh:512], op0=ALU.mult, op1=ALU.add)
            if I > 0:
                H = HMh[cc]
                E.op("dve", "scalar_tensor_tensor", out=Y[:, 0:3], in0=H[:, 0:3], scalar=MCW[:, cc * 4:cc * 4 + 1],
                     in1=Y[:, 0:3], op0=ALU.mult, op1=ALU.add)
                E.op("dve", "scalar_tensor_tensor", out=Y[:, 0:2], in0=H[:, 1:3], scalar=MCW[:, cc * 4 + 1:cc * 4 + 2],
                     in1=Y[:, 0:2], op0=ALU.mult, op1=ALU.add)
                E.op("dve", "scalar_tensor_tensor", out=Y[:, 0:1], in0=H[:, 2:3], scalar=MCW[:, cc * 4 + 2:cc * 4 + 3],
                     in1=Y[:, 0:1], op0=ALU.mult, op1=ALU.add)
            E.op("act", "activation", out=HMh[cc][:], in_=p[:, 509:512], func=AF.Copy)
            E.op("act", "activation", out=QKT[:, cc, :], in_=Y[:], func=AF.Silu)
        fm_group(get_w(), ev_g0)

        def ev_g1(t, p):
            E.op("act", "activation", out=VM[:, t, :, 0:128],
                 in_=p[:, :].f(lambda a: a.rearrange("p (h d) -> p h d", h=4)), func=AF.Copy)
        E._emit("pool", "memset", dict(ap=VM.ap[:, :, :, 128:129], constant=1.0), [], ["VM"], "pool", 1)
        tm_group(get_w(), 512, ev_g1)

        def ev_g2(t, p):
            E.op("act", "activation", out=OG[:, t, :], in_=p[:, :], func=AF.Sigmoid)
            E.op("pool", "tensor_tensor", out=OG[:, t, :], in0=OG[:, t, :], in1=MNORM[:], op=ALU.mult)
        tm_group(get_w(), 512, ev_g2)

        def ev_g3(cc, p):
            E.op("act", "activation", out=AQT[:, cc, :], in_=p[:, :], func=AF.Copy, scale=0.125)
        fm_group(get_w(), ev_g3)

        def ev_g4(cc, p):
            E.op("dve", "tensor_copy", out=KT[:, cc, tok0:tok0 + 512], in_=p[:, :])
        fm_group(get_w(), ev_g4)

        def ev_g5(t, p):
            E.op("act", "activation", out=V(VP_t[:, I * 4 + t, :, 0:64], "VP"),
                 in_=p[:, :].f(lambda a: a.rearrange("p (h d) -> p h d", h=8)), func=AF.Copy)
        tm_group(get_w(), 512, ev_g5)

        def ev_g6(cc, p):
            E.op("dve", "tensor_copy", out=IQT[:, cc, :], in_=p[:, :])
        fm_group(get_w(), ev_g6)

        def ev_g7(t, p):
            E.op("act", "activation", out=JUNK[:, 0:64], in_=p[:, 0:64], func=AF.Square, accum_out=SSK[t][:])
            rstd_from_ss(SSK[t][:], RK[t][:], 64.0)
            E.op("dve", "scalar_tensor_tensor", out=IKN[:], in0=p[:, 0:64], scalar=RK[t][:], in1=GKB[:],
                 op0=ALU.mult, op1=ALU.mult)
            E.op("dve", "tensor_scalar", out=WQ[t][:], in0=p[:, 64:72], scalar1=float(8 ** -0.5 * 64 ** -0.5),
                 scalar2=None, op0=ALU.mult)
            E.op("dve", "tensor_tensor", out=GATES[t][:], in0=p[:, 72:80], in1=GBIAS[:], op=ALU.add)
            p2 = psum("mm")
            pb = bfv(p2)
            E.op("pe", "transpose", out=pb[0:64, 0:128], in_=IKN[:], identity=IDENTB[:])
            c0 = tok0 + t * 128
            E.op("dve", "tensor_copy", out=IK[0:64, c0:c0 + 128], in_=pb[0:64, 0:128])
            E.op("dve", "tensor_copy", out=IK[64:128, c0:c0 + 128], in_=pb[0:64, 0:128])
        tm_group(get_w(), 80, ev_g7)

        for t in range(4):
            c0 = t * 128
            pk = psum("mm")
            pkb = bfv(pk)
            for pp in range(2):
                E.op("pe", "transpose", out=pkb[:, pp * 128:(pp + 1) * 128], in_=QKT[:, 2 + pp, c0:c0 + 128],
                     identity=IDENTB[:])
            G = GATES[t]
            E.op("act", "activation", out=E1[:], in_=G[:, 4:8], func=AF.Exp, scale=-1.0)
            E.op("dve", "tensor_scalar", out=E1[:], in0=E1[:], scalar1=1.0, scalar2=None, op0=ALU.add)
            E.op("act", "activation", out=NLF[:], in_=E1[:], func=AF.Ln)
            pg = psum("mm")
            for k in range(4):
                E.op("pe", "matmul", out=pg[:, 4 * k:4 * k + 4], lhsT=TRI4[:, 128 * k:128 * (k + 1)], rhs=NLF[:],
                     start=True, stop=True)
            E.op("dve", "tensor_copy", out=GS[:], in_=pg[:, 0:16])
            E.op("act", "activation", out=EB8[:], in_=GS[:, 0:4], func=AF.Exp, scale=-1.0, bias=-math.log(8.0))
            E.op("dve", "tensor_tensor", out=T1[:], in0=G[:, 0:4], in1=GS[:, 0:4], op=ALU.add)
            E.op("act", "activation", out=EK[:], in_=T1[:], func=AF.Exp)
            E.op("dve", "tensor_tensor", out=T2[:], in0=T1[:], in1=GS[:, 4:8], op=ALU.subtract)
            E.op("act", "activation", out=WKs[:], in_=T2[:], func=AF.Exp)
            E.op("act", "activation", out=EG[:], in_=GS[:, 8:16], func=AF.Exp, scale=-1.0)
            E.op("dve", "tensor_tensor", out=KW[:],
                 in0=pkb[:, 0:256].f(lambda a: a.rearrange("p (h d) -> p h d", h=4)),
                 in1=WKs[:, 0:4].f(lambda a: a.unsqueeze(2).to_broadcast([128, 4, 64])), op=ALU.mult)
            npss = [psum("acc") for _ in range(4)]
            prs = []
            for h in range(4):
                pp, hh = h // 2, h % 2
                base = 64 * hh
                pr = psum("mm")
                E.op("pe", "matmul", out=pr[:, 0:128], lhsT=QKT[base:base + 64, 2 + pp, c0:c0 + 128],
                     rhs=QKT[base:base + 64, pp, c0:c0 + 128], start=True, stop=True)
                prs.append(pr)
            for h in range(4):
                E.op("dve", "scalar_tensor_tensor", out=PTB[h][:], in0=prs[h][:, 0:128], scalar=EK[:, h:h + 1],
                     in1=TRI4[:, 0:128], op0=ALU.mult, op1=ALU.mult)
            for h in range(4):
                E.op("pe", "matmul", out=npss[h][:, 0:129], lhsT=PTB[h][:], rhs=VM[:, t, h, :], start=True, stop=False,
                     skip_group_check=True)
            for ch in range(2):
                r0 = 64 * ch
                pkvs = []
                for h in range(4):
                    pp, hh = h // 2, h % 2
                    base, o = 64 * hh, 0
                    E.op("pe", "matmul", out=npss[h][r0:r0 + 64, o:o + 129], lhsT=QKT[:, pp, c0 + r0:c0 + r0 + 64],
                         rhs=CBh[h][:], start=False, stop=(ch == 1), skip_group_check=True)
                    pkv = psum("mm")
                    E.op("pe", "matmul", out=pkv[base:base + 64, 0:129], lhsT=KW[r0:r0 + 64, h, :],
                         rhs=VM[r0:r0 + 64, t, h, :], start=True, stop=True)
                    pkvs.append(pkv)
                for h in range(4):
                    pp, hh = h // 2, h % 2
                    base = 64 * hh
                    E.op("dve", "scalar_tensor_tensor", out=CST[pp][base:base + 64, :], in0=CST[pp][base:base + 64, :],
                         scalar=EG[base:base + 64, 4 * ch + h:4 * ch + h + 1], in1=pkvs[h][base:base + 64, 0:129],
                         op0=ALU.mult, op1=ALU.add)
                for h in range(4):
                    pp, hh = h // 2, h % 2
                    base = 64 * hh
                    E.op("act", "activation", out=CBh[h][base:base + 64, :], in_=CST[pp][base:base + 64, :],
                         func=AF.Copy)
            for h in range(4):
                E.op("dve", "tensor_tensor", out=DN[:, h:h + 1], in0=npss[h][:, 128:129], in1=EB8[:, h:h + 1], op=ALU.mult)
            E.op("act", "activation", out=DN[:], in_=DN[:], func=AF.Abs)
            E.op("dve", "tensor_scalar", out=DN[:], in0=DN[:], scalar1=1.0, scalar2=None, op0=ALU.max)
            E.op("dve", "reciprocal", out=DN[:], in_=DN[:])
            E.op("dve", "tensor_tensor", out=RR[:], in0=DN[:], in1=EB8[:, 0:4], op=ALU.mult)
            for h in range(4):
                E.op("act", "activation", out=JUNK[:, h * 128:(h + 1) * 128], in_=npss[h][:, 0:128], func=AF.Square,
                     accum_out=MS[:, h:h + 1])
            E.op("dve", "tensor_tensor", out=TT[:], in0=RR[:], in1=RR[:], op=ALU.mult)
            E.op("dve", "tensor_tensor", out=TT[:], in0=TT[:], in1=MS[:], op=ALU.mult)
            E.op("dve", "tensor_scalar", out=TT[:], in0=TT[:], scalar1=1.0 / 128.0, scalar2=EPS, op0=ALU.mult,
                 op1=ALU.add)
            E.op("act", "activation", out=TT[:], in_=TT[:], func=AF.Ln)
            E.op("act", "activation", out=TT[:], in_=TT[:], func=AF.Exp, scale=-0.5)
            E.op("dve", "tensor_tensor", out=SCL[:], in0=TT[:], in1=RR[:], op=ALU.mult)
            for h in range(4):
                E.op("dve", "scalar_tensor_tensor", out=HMO[:, h * 128:(h + 1) * 128],
                     in0=npss[h][:, 0:128], scalar=SCL[:, h:h + 1], in1=OG[:, t, h * 128:(h + 1) * 128],
                     op0=ALU.mult, op1=ALU.mult)
            transposes_to(MIX03[:, :, c0:c0 + 128], HMO, 4)

        E.barrier()
        nkt = 4 * I + 4
        X1flat = X1_t[:].rearrange("p a b -> p (a b)")
        SCB = [
            ([SCK[kc] for kc in range(8)], SCall),
            ([V(X1_t[:, kc // 2, (kc % 2) * 512:(kc % 2) * 512 + 512], "X1_%d" % (kc // 2)) for kc in range(8)],
             V(X1flat, ["X1_0", "X1_1", "X1_2", "X1_3"])),
        ]

        def score_gen(t):
            i = 4 * I + t
            n = 128 * (i + 1)
            nch = (n + 511) // 512
            sck, scall = SCB[0 if DBG_NOX1 else t % 2]
            for h in range(8):
                E.op("dve", "tensor_scalar", out=DG[:, h, :], in0=IDENT[:], scalar1=WQ[t][:, h:h + 1], scalar2=None,
                     op0=ALU.mult)
            dk = i // 4
            for kc in range(nch):
                w = min(512, n - 512 * kc)
                pacc = psum("acc")
                pis = {}

                def emit_isc(h, kc=kc, w=w):
                    base, pp = 64 * (h % 2), h // 2
                    p = psum("mm")
                    E.op("pe", "matmul", out=p[:, 0:w], lhsT=IQT[base:base + 64, pp, t * 128:(t + 1) * 128],
                         rhs=IK[base:base + 64, 512 * kc:512 * kc + w], start=True, stop=True)
                    pis[h] = p
                for h in range(3):
                    emit_isc(h)
                for h in range(8):
                    if h + 3 < 8:
                        emit_isc(h + 3)
                    p = pis.pop(h)
                    R = RBF[h % 4]
                    if h % 2 == 0:
                        E.op("act", "activation", out=R[:, 0:w], in_=p[:, 0:w], func=AF.Relu)
                    else:
                        E.op("dve", "tensor_scalar", out=R[:, 0:w], in0=p[:, 0:w], scalar1=0.0, scalar2=None, op0=ALU.max)
                    E.op("pe", "matmul", out=pacc[:, 0:w], lhsT=DG[:, h, :], rhs=R[:, 0:w], start=(h == 0),
                         stop=(h == 7), skip_group_check=True)
                    if h == 7:
                        E.op("dve", "tensor_copy", out=sck[kc][:, 0:w], in_=pacc[:, 0:w])
                    yield (h == 7)
            if n > topk:
                E.op("dve", "tensor_reduce", out=MN2[t % 2][:], in_=scall[:, 0:n], axis=AX.X, op=ALU.min)
            E.op("dve", "tensor_tensor", out=sck[dk][:, (i % 4) * 128:(i % 4) * 128 + 128],
                 in0=sck[dk][:, (i % 4) * 128:(i % 4) * 128 + 128], in1=CMASK[:], op=ALU.add)
            yield True

        def bisect_gen(t):
            i = 4 * I + t
            n = 128 * (i + 1)
            sck, scall = SCB[0 if DBG_NOX1 else t % 2]
            MNt = MN2[t % 2]
            if n > topk:
                E.op("dve", "tensor_reduce", out=MX[:], in_=scall[:, 0:n], axis=AX.X, op=ALU.max)
                E.op("dve", "tensor_tensor", out=W0[:], in0=MX[:], in1=MNt[:], op=ALU.subtract)
                E.op("dve", "tensor_scalar", out=W0[:], in0=W0[:], scalar1=1.0001, scalar2=1e-6, op0=ALU.mult, op1=ALU.add)
                E.op("dve", "tensor_scalar", out=NHK[:], in0=HKC[:], scalar1=W0[:], scalar2=-1.0, op0=ALU.mult, op1=ALU.mult)
                E.op("dve", "scalar_tensor_tensor", out=NM[:], in0=MNt[:], scalar=-1.0, in1=NHK[:, 0:1], op0=ALU.mult,
                     op1=ALU.add)
                cthr = float(2 * topk - n) - 0.5
                split = n >= 1536
                if split:
                    nA = ((int(0.56 * n) + 127) // 128) * 128
                    nB = n - nA
                    MQa = V(MQ.ap[:, 0:nA], "MQa")
                    MQb = V(MQ.ap[:, nA:n], "MQb")
                    E.op("dve", "tensor_scalar", out=MID[:], in0=NM[:], scalar1=-1.0, scalar2=None, op0=ALU.mult)
                yield
                for k in range(NITER):
                    if split:
                        E.op("act", "activation", out=MQa, in_=scall[:, 0:nA], func=AF.Sign, bias=NM[:], scale=1.0,
                             accum_out=SACC[:])
                        E.op("dve", "tensor_scalar", out=MQb, in0=scall[:, nA:n], scalar1=MID[:], scalar2=None,
                             op0=ALU.is_ge, op1=ALU.add, accum_out=CNT[:])
                        yield
                        E.op("dve", "tensor_scalar", out=TMP[:], in0=CNT[:], scalar1=2.0, scalar2=-(float(nB) + cthr),
                             op0=ALU.mult, op1=ALU.add)
                        E.op("act", "activation", out=SG[:], in_=SACC[:], func=AF.Sign, bias=TMP[:], scale=1.0)
                    else:
                        E.op("act", "activation", out=MQ[:, 0:n], in_=scall[:, 0:n], func=AF.Sign, bias=NM[:], scale=1.0,
                             accum_out=SACC[:])
                        yield
                        E.op("act", "activation", out=SG[:], in_=SACC[:], func=AF.Sign, bias=-cthr, scale=1.0)
                    E.op("act", "activation", out=NM[:], in_=SG[:], func=AF.Identity, scale=NHK[:, k + 1:k + 2], bias=NM[:])
                    if split and k + 1 < NITER:
                        E.op("dve", "tensor_scalar", out=MID[:], in0=NM[:], scalar1=-1.0, scalar2=None, op0=ALU.mult)
                E.op("act", "activation", out=LO[:], in_=NM[:], func=AF.Identity, scale=-1.0, bias=NHK[:, NITER:NITER + 1])
                E.op("dve", "tensor_scalar", out=V(MQ.ap[:, 0:n], ["MQ", "MQa", "MQb"]), in0=scall[:, 0:n], scalar1=LO[:],
                     scalar2=None, op0=ALU.is_ge)
            else:
                E.op("dve", "tensor_scalar", out=MQ[:, 0:n], in0=scall[:, 0:n], scalar1=-1.0e29, scalar2=None,
                     op0=ALU.is_ge)
            if t < 3:
                E._emit("pool", "memset", dict(ap=MQ.ap[:, n:128 * nkt], constant=0.0), [], ["MQ"], "pool", 1)
            yield "pre_transpose"
            for jg in range((nkt + 3) // 4):
                p = psum("mm")
                pb = bfv(p)
                for jj in range(4):
                    j = 4 * jg + jj
                    E.op("pe", "transpose", out=pb[:, jj * 128:(jj + 1) * 128], in_=MQ[:, 128 * j:128 * (j + 1)],
                         identity=IDENTB[:])
                E.op("dve", "tensor_copy", out=MT[:, 4 * jg:4 * jg + 4, t * 128:(t + 1) * 128],
                     in_=pb[:, 0:512].f(lambda a: a.rearrange("p (j q) -> p j q", j=4)))
            yield

        for _ in score_gen(0):
            pass
        for t in range(4):
            bg = bisect_gen(t)
            sg = score_gen(t + 1) if t + 1 < 4 else None
            nb_steps = NITER + 2
            ns_steps = (8 * ((128 * (4 * I + t + 2) + 511) // 512) + 1) if sg is not None else 0
            ratio = float(ns_steps) / nb_steps
            if DBG_NOOVERLAP:
                ratio = 0.0
            accr = 0.0
            state = {"sg": sg, "bnd": True}

            def adv():
                try:
                    state["bnd"] = bool(next(state["sg"]))
                except StopIteration:
                    state["sg"] = None
                    state["bnd"] = True
            for tag in bg:
                if tag == "pre_transpose":
                    while state["sg"] is not None and not state["bnd"]:
                        adv()
                    continue
                accr += ratio
                while state["sg"] is not None and accr >= 1.0:
                    accr -= 1.0
                    adv()
            while state["sg"] is not None:
                adv()
        for t in range(4):
            E.dma("sp", "d_x%d" % t, out=X1[t][:], in_=V(x[tok0 + t * 128:tok0 + (t + 1) * 128, :], "dram_x"))
        for pp in range(4):
            accs = [psum("acc2"), psum("acc2")]
            qk = {}

            def emit_qk(j, pp=pp):
                for hh in range(2):
                    base = 64 * hh
                    p = psum("qk")
                    E.op("pe", "matmul", out=p[:, :], lhsT=KT[base:base + 64, pp, 128 * j:128 * (j + 1)],
                         rhs=AQT[base:base + 64, pp, :], start=True, stop=True)
                    qk[(j, hh)] = p
            for j in range(min(2, nkt)):
                emit_qk(j)
            for j in range(nkt):
                if j + 2 < nkt:
                    emit_qk(j + 2)
                for hh in range(2):
                    h = 2 * pp + hh
                    p = qk.pop((j, hh))
                    e = EP[(2 * j + hh) % 8]
                    E.op("act", "activation", out=e[:], in_=p[:, :], func=AF.Exp)
                    E.op("dve", "tensor_tensor", out=e[:], in0=e[:], in1=MT[:, j, :], op=ALU.mult)
                    r = j - 4 * I
                    if r >= -1:
                        for tq, v in ((r, 0), (r + 1, 1)):
                            if 0 <= tq <= 3:
                                E.op("dve", "tensor_tensor", out=e[:, tq * 128:(tq + 1) * 128],
                                     in0=e[:, tq * 128:(tq + 1) * 128], in1=WT[:, h, v * 128:(v + 1) * 128], op=ALU.mult)
                    E.op("pe", "matmul", out=accs[hh][0:65, :], lhsT=V(VP_t[:, j, h, :], ["VP", "VPones"]), rhs=e[:],
                         start=(j == 0), stop=(j == nkt - 1))
            for hh in range(2):
                E.op("act", "activation", out=FT[2 * hh][64:65, :], in_=accs[hh][64:65, :], func=AF.Ln)
            for hh in range(2):
                E.op("act", "activation", out=FT[2 * hh][64:65, :], in_=FT[2 * hh][64:65, :], func=AF.Exp, scale=-1.0)
            pbcs = []
            for hh in range(2):
                pbc = psum("qk")
                E.op("pe", "matmul", out=pbc[0:64, :], lhsT=ONES[64:65, 0:64], rhs=FT[2 * hh][64:65, :], start=True,
                     stop=True)
                pbcs.append(pbc)
            for hh in range(2):
                E.op("act", "activation", out=FT[2 * hh + 1][0:64, :], in_=pbcs[hh][0:64, :], func=AF.Copy)
            for hh in range(2):
                base = 64 * hh
                E.op("dve", "tensor_tensor", out=MIXc[4 + pp][base:base + 64, :], in0=accs[hh][0:64, :],
                     in1=FT[2 * hh + 1][0:64, :], op=ALU.mult)

        E.barrier()
        E.dma("sp", "d_gfin", out=GFIN[:], in_=V(d_gfin, "dram_gfin"))
        for hf in range(2):
            W = get_w()
            for t in range(4):
                p = psum("mm")
                for c in range(8):
                    E.op("pe", "matmul", out=p[:, :], lhsT=MIXc[c][:, t * 128:(t + 1) * 128], rhs=W[:, c, :],
                         start=(c == 0), stop=(c == 7))
                E.op("dve", "tensor_tensor", out=X1[t][:, hf * 512:(hf + 1) * 512], in0=p[:, :],
                     in1=X1[t][:, hf * 512:(hf + 1) * 512], op=ALU.add)
        rstd_batch([X1[t][:] for t in range(4)], [SS2[t][:] for t in range(4)], [RSTD2[t][:] for t in range(4)],
                   JUNK2[:], 1024.0)
        for t in range(4):
            E.op("dve", "tensor_scalar", out=HBE[t % 2][:], in0=X1[t][:], scalar1=RSTD2[t][:], scalar2=None, op0=ALU.mult)
            transposes_to(HT[:, :, t * 128:(t + 1) * 128], HBE[t % 2], 8)

        def ffn_conv(p, Y, U, ci):
            E.op("act", "activation", out=Y[:], in_=p[:, :], func=AF.Identity, scale=FCW[:, 3 * ci + 2:3 * ci + 3],
                 bias=FCB[:, ci:ci + 1])
            E.op("act", "activation", out=U[:, 2:514], in_=p[:, :], func=AF.Copy)
            if I > 0:
                E.op("act", "activation", out=U[:, 0:2], in_=HFh[ci][:], func=AF.Copy)
            else:
                E._emit("act", "memzero", dict(ap=U.ap[:, 0:2]), [], [U.key], "act", 1)
            E.op("act", "activation", out=HFh[ci][:], in_=U[:, 512:514], func=AF.Copy)
            E.op("dve", "scalar_tensor_tensor", out=Y[:], in0=U[:, 1:513], scalar=FCW[:, 3 * ci + 1:3 * ci + 2], in1=Y[:],
                 op0=ALU.mult, op1=ALU.add)
            E.op("dve", "scalar_tensor_tensor", out=Y[:], in0=U[:, 0:512], scalar=FCW[:, 3 * ci:3 * ci + 1], in1=Y[:],
                 op0=ALU.mult, op1=ALU.add)

        for g in range(11):
            W = get_w()
            for sub in range(2):
                c = 2 * g + sub
                pg_ = psum("mm")
                pv_ = psum("mm")
                for kc in range(8):
                    E.op("pe", "matmul", out=pg_[:, :], lhsT=W[:, kc, sub * 128:(sub + 1) * 128], rhs=HT[:, kc, :],
                         start=(kc == 0), stop=(kc == 7))
                for kc in range(8):
                    E.op("pe", "matmul", out=pv_[:, :], lhsT=W[:, kc, 256 + sub * 128:256 + (sub + 1) * 128],
                         rhs=HT[:, kc, :], start=(kc == 0), stop=(kc == 7))
                Yg, Yv = YE[(c % 2) * 2], YE[(c % 2) * 2 + 1]
                ffn_conv(pg_, Yg, UE[(c % 2) * 2], c)
                ffn_conv(pv_, Yv, UE[(c % 2) * 2 + 1], 22 + c)
                E.op("act", "activation", out=Yg[:], in_=Yg[:], func=AF.Silu)
                E.op("pool", "tensor_tensor", out=V(AT.ap[:, c, :], "AT%d" % c), in0=Yg[:], in1=Yv[:], op=ALU.mult)
        for hf in range(2):
            accs = [psum("acc") for _ in range(4)]
            for kg in range(3):
                W = get_w()
                nk = 8 if kg < 2 else 6
                for t in range(4):
                    for kk in range(nk):
                        c = 8 * kg + kk
                        E.op("pe", "matmul", out=accs[t][:, :], lhsT=V(AT.ap[:, c, t * 128:(t + 1) * 128], "AT%d" % c), rhs=W[:, kk, :],
                             start=(c == 0), stop=(c == 21), skip_group_check=True)
            for t in range(4):
                E.op("dve", "tensor_tensor", out=X1[t][:, hf * 512:(hf + 1) * 512], in0=accs[t][:, :],
                     in1=X1[t][:, hf * 512:(hf + 1) * 512], op=ALU.add)
        rstd_batch([X1[t][:] for t in range(4)], [SS3[t][:] for t in range(4)], [RSTD3[t][:] for t in range(4)],
                   JUNK2[:], 1024.0)
        for t in range(4):
            ob = OUTB[t % 2]
            E.op("dve", "scalar_tensor_tensor", out=ob[:], in0=X1[t][:], scalar=RSTD3[t][:], in1=GFIN[:],
                 op0=ALU.mult, op1=ALU.mult)
            E.dma("pool", "d_out%d" % (t % 2), out=V(y[tok0 + t * 128:tok0 + (t + 1) * 128, :], "dram_y"), in_=ob[:])
        if I + 1 < NB:
            for t in range(4):
                E.dma("sp", "d_x%d" % t, out=X1[t][:],
                      in_=V(x[tok0 + 512 + t * 128:tok0 + 512 + (t + 1) * 128, :], "dram_x"))
        E.barrier()

    with nc.Block() as block:
        @block.sync
        def _(eng):
            E.replay("sp", eng)

        @block.tensor
        def _(eng):
            E.replay("pe", eng)

        @block.scalar
        def _(eng):
            E.replay("act", eng)

        @block.vector
        def _(eng):
            E.replay("dve", eng)

        @block.gpsimd
        def _(eng):
            E.replay("pool", eng)
            E.final_wait(eng, "pool")
    stack.close()
    return nc


def _rel_bucket(d):
    d = np.maximum(d, 0)
    large = 16 + (np.log(np.maximum(d, 1).astype(np.float32) / 16) / math.log(128 / 16) * 16).astype(np.int32)
    large = np.minimum(large, 31)
    return np.where(d < 16, d, large)


def host_consts(inp):
    f32 = np.float32
    c = {}
    c["gmix"] = np.ascontiguousarray(inp["norm_mix"].reshape(8, 128).T).astype(f32)
    c["gffn"] = np.ascontiguousarray(inp["norm_ffn"].reshape(8, 128).T).astype(f32)
    c["gfin"] = np.ascontiguousarray(np.broadcast_to(inp["norm_final"].reshape(1, 1024), (128, 1024))).astype(f32)
    mcw = inp["mlstm_conv_w"].reshape(4, 4, 128)
    c["mcw"] = np.ascontiguousarray(mcw.transpose(2, 1, 0).reshape(128, 16)).astype(f32)
    c["mcb"] = np.ascontiguousarray(inp["mlstm_conv_b"].reshape(4, 128).T).astype(f32)
    gb = np.concatenate([inp["i_bias"].reshape(4), inp["f_bias"].reshape(4)])
    c["gbias"] = np.ascontiguousarray(np.broadcast_to(gb.reshape(1, 8), (128, 8))).astype(f32)
    c["mnorm"] = np.ascontiguousarray(np.broadcast_to(inp["mlstm_norm"].reshape(1, 512), (128, 512))).astype(f32)
    c["gk"] = np.ascontiguousarray(np.broadcast_to(inp["idx_k_norm"].reshape(1, 64), (128, 64))).astype(f32)
    c["relb"] = np.ascontiguousarray(inp["rel_bias"]).astype(f32)
    c["b31"] = np.ascontiguousarray(inp["rel_bias"][31].reshape(8, 1)).astype(f32)
    oh = np.zeros((32, 384), f32)
    for k in range(384):
        d = k - 127
        if d >= 0:
            oh[int(_rel_bucket(np.array([d]))[0]), k] = 1.0
    c["oh"] = oh
    fw = inp["ffn_conv_w"].reshape(3, 44, 128)
    c["fcw"] = np.ascontiguousarray(fw.transpose(2, 1, 0).reshape(128, 132)).astype(f32)
    c["fcb"] = np.ascontiguousarray(inp["ffn_conv_b"].reshape(44, 128).T).astype(f32)
    c["ident"] = np.eye(128, dtype=f32)
    c["jrev"] = np.ascontiguousarray(np.eye(128, dtype=f32)[::-1])
    s = np.arange(128)[:, None]
    l = np.arange(128)[None, :]
    same = (s // 64) == (l // 64)
    tri = (same & (s <= l)).astype(f32)
    blk = same.astype(f32)
    blka = np.broadcast_to((s < 64), (128, 128)).astype(f32)
    blkb = np.broadcast_to((s >= 64), (128, 128)).astype(f32)
    c["tri4"] = np.ascontiguousarray(np.concatenate([tri, blk, blka, blkb], axis=1))
    q = np.arange(128)[:, None]
    kk = np.arange(128)[None, :]
    c["cmask"] = np.where(kk <= q, 0.0, NEG).astype(f32)
    c["hkc"] = np.ascontiguousarray(np.broadcast_to((0.5 ** (np.arange(NITER + 1) + 1)).reshape(1, NITER + 1), (128, NITER + 1))).astype(f32)
    return c


_NC_CACHE = {}


def kernel(**inputs):
    inp = {k: np.asarray(v) for k, v in inputs.items()}
    x = inp["x"]
    B, S, D = x.shape
    NB = S // 512
    topk = min(256, S // 4)
    key = (NB, topk)
    if key not in _NC_CACHE:
        _NC_CACHE[key] = build(NB, topk)
    nc = _NC_CACHE[key]
    c = host_consts(inp)
    shared = dict(c)
    shared["w_in"] = np.ascontiguousarray(inp["w_in"][0])
    shared["w_out"] = np.ascontiguousarray(inp["w_out"][0])
    shared["w_up"] = np.ascontiguousarray(inp["w_up"][0])
    shared["w_down"] = np.ascontiguousarray(inp["w_down"][0])
    in_maps = []
    for b in range(B):
        m = dict(shared)
        m["x"] = np.ascontiguousarray(x[b])
        in_maps.append(m)
    res = run_bass_kernel_spmd(nc, in_maps, core_ids=list(range(B)))
    out = np.stack([np.asarray(r["y"]) for r in res.results], axis=0).astype(np.float32)
    return out
```

```python
import math
import numpy as np
import ml_dtypes
import concourse.bass as bass
import concourse.mybir as mybir
from concourse.bass_utils import run_bass_kernel_spmd

F32 = mybir.dt.float32
BF16 = mybir.dt.bfloat16
AF = mybir.ActivationFunctionType
ALU = mybir.AluOpType
AX = mybir.AxisListType

EPS = 1e-6
import os
DBG_NOOVERLAP = os.environ.get('DBG_NOOVERLAP', '0') == '1'
DBG_NOX1 = os.environ.get('DBG_NOX1', '0') == '1'
NITER = 16
NEG = -1.0e30

WIN_GROUPS = [
    [(0, 512)],
    [(512, 512)],
    [(1024, 512)],
    [(1544, 512)],
    [(2056, 512)],
    [(2568, 512)],
    [(3080, 512)],
    [(3592, 72), (1536, 8)],
]


class V:
    __slots__ = ("ap", "key")

    def __init__(self, ap, key):
        if type(ap).__name__.endswith("Handle"):
            ap = ap[:]
        self.ap = ap
        self.key = key

    def __getitem__(self, idx):
        return V(self.ap[idx], self.key)

    def f(self, fn):
        return V(fn(self.ap), self.key)


class Em:
    ENG = ("pe", "act", "dve", "pool", "sp")

    def __init__(self, nc, stack):
        self.nc = nc
        self.stack = stack
        self.q = {e: [] for e in self.ENG}
        self.cnt = {}
        self.sems = {}
        self.lastw = {}
        self.readers = {}
        self.waited = {e: {} for e in self.ENG}
        for e in ("pe", "act", "dve", "pool"):
            self._sem(e)

    def _sem(self, key):
        if key not in self.sems:
            self.sems[key] = self.stack.enter_context(self.nc.semaphore("s_" + key))
            self.cnt[key] = 0
        return self.sems[key]

    def _emit(self, eng, meth, kw, reads, writes, semkey, inc):
        deps = {}

        def add(d):
            if d is None:
                return
            k, v = d
            if deps.get(k, 0) < v:
                deps[k] = v
        for b in reads:
            add(self.lastw.get(b))
        for b in writes:
            add(self.lastw.get(b))
            for k, v in self.readers.get(b, {}).items():
                add((k, v))
        waits = []
        for k, v in deps.items():
            if eng == "pe" and k == "pe":
                continue
            if self.waited[eng].get(k, 0) >= v:
                continue
            self.waited[eng][k] = v
            waits.append((k, v))
        self._sem(semkey)
        self.cnt[semkey] += inc
        val = self.cnt[semkey]
        self.q[eng].append((meth, kw, waits, semkey, inc))
        for b in reads:
            if b in writes:
                continue
            self.readers.setdefault(b, {})[semkey] = val
        for b in writes:
            self.lastw[b] = (semkey, val)
            self.readers[b] = {}
        return val

    def op(self, eng, meth, **kw):
        reads, writes, real = [], [], {}
        for k, v in kw.items():
            if isinstance(v, V):
                real[k] = v.ap
                keys = v.key if isinstance(v.key, (list, tuple)) else [v.key]
                if k in ("out", "accum_out"):
                    writes.extend(keys)
                else:
                    reads.extend(keys)
            else:
                real[k] = v
        return self._emit(eng, meth, real, reads, writes, eng, 1)

    def dma(self, eng, sem, out, in_, **extra):
        okeys = out.key if isinstance(out.key, (list, tuple)) else [out.key]
        ikeys = in_.key if isinstance(in_.key, (list, tuple)) else [in_.key]
        kw = dict(out=out.ap, in_=in_.ap)
        kw.update(extra)
        return self._emit(eng, "dma_start", kw, list(ikeys), list(okeys), sem, 16)

    def barrier(self):
        cur = dict(self.cnt)
        for e in self.ENG:
            waits = []
            for k, v in cur.items():
                if v == 0 or (e == k):
                    continue
                if self.waited[e].get(k, 0) >= v:
                    continue
                self.waited[e][k] = v
                waits.append((k, v))
            if waits:
                self.q[e].append((None, None, waits, None, 0))

    def replay(self, eng, engobj):
        for meth, kw, waits, semkey, inc in self.q[eng]:
            for k, v in waits:
                engobj.wait_ge(self.sems[k], v)
            if meth is None:
                continue
            ins = getattr(engobj, meth)(**kw)
            ins.then_inc(self.sems[semkey], inc)

    def final_wait(self, engobj, eng):
        for k, v in self.cnt.items():
            if v > 0 and self.waited[eng].get(k, 0) < v:
                engobj.wait_ge(self.sems[k], v)


def build(NB, topk):
    from contextlib import ExitStack
    S = NB * 512
    NT = NB * 4
    nc = bass.Bass("TRN2", target_bir_lowering=False)
    stack = ExitStack()
    E = Em(nc, stack)

    def din(name, shape, dt=F32):
        return nc.dram_tensor(name, list(shape), dt, kind="ExternalInput").ap()

    x = din("x", [S, 1024])
    w_in = din("w_in", [1024, 3664])
    w_out = din("w_out", [1024, 1024])
    w_up = din("w_up", [1024, 5632])
    w_down = din("w_down", [2816, 1024])
    d_gmix = din("gmix", [128, 8])
    d_gffn = din("gffn", [128, 8])
    d_gfin = din("gfin", [128, 1024])
    d_mcw = din("mcw", [128, 16])
    d_mcb = din("mcb", [128, 4])
    d_gbias = din("gbias", [128, 8])
    d_mnorm = din("mnorm", [128, 512])
    d_gk = din("gk", [128, 64])
    d_relb = din("relb", [32, 8])
    d_b31 = din("b31", [8, 1])
    d_oh = din("oh", [32, 384])
    d_fcw = din("fcw", [128, 132])
    d_fcb = din("fcb", [128, 44])
    d_ident = din("ident", [128, 128])
    d_tri4 = din("tri4", [128, 512])
    d_cmask = din("cmask", [128, 128])
    d_hkc = din("hkc", [128, NITER + 1])
    d_jrev = din("jrev", [128, 128])
    y = nc.dram_tensor("y", [S, 1024], F32, kind="ExternalOutput").ap()

    def dscratch(name, shape, dt=BF16):
        return nc.dram_tensor(name, list(shape), dt, kind="Internal").ap()

    sc_in = [dscratch("sc_in%d" % g, [128, 8, 512]) for g in range(8)]
    sc_out = [dscratch("sc_out%d" % g, [128, 8, 512]) for g in range(2)]
    sc_up = [dscratch("sc_up%d" % g, [128, 8, 512]) for g in range(11)]
    sc_dn = [dscratch("sc_dn%d" % g, [128, 8, 512]) for g in range(6)]
    tvd = dscratch("tvd", [8, 384])

    def sb(name, shape, dt):
        return stack.enter_context(nc.sbuf_tensor(name, list(shape), dt))

    KT = V(sb("KT", [128, 4, S], BF16), "KT")
    VP_t = sb("VP", [128, NT, 8, 65], BF16)
    IK = V(sb("IK", [128, S], BF16), "IK")
    WT = V(sb("WT", [128, 8, 256], BF16), "WT")
    WS = [V(sb("WS%d" % i, [128, 8, 512], BF16), "WS%d" % i) for i in range(3)]
    X1_t = sb("X1", [128, 4, 1024], F32)
    X1 = [V(X1_t[:, t, :], "X1_%d" % t) for t in range(4)]
    MIXT = sb("MIXT", [128, 8, 512], BF16)
    GMIX = V(sb("GMIX", [128, 8], F32), "c_gmix")
    GFFN = V(sb("GFFN", [128, 8], F32), "c_gffn")
    MCW = V(sb("MCW", [128, 16], F32), "c_mcw")
    MCB = V(sb("MCB", [128, 4], F32), "c_mcb")
    GBIAS = V(sb("GBIAS", [128, 8], F32), "c_gbias")
    GKB = V(sb("GKB", [128, 64], F32), "c_gk")
    FCW = V(sb("FCW", [128, 132], F32), "c_fcw")
    FCB = V(sb("FCB", [128, 44], F32), "c_fcb")
    IDENT = V(sb("IDENT", [128, 128], F32), "c_ident")
    IDENTB = V(sb("IDENTB", [128, 128], BF16), "c_identb")
    CMASK = V(sb("CMASK", [128, 128], F32), "c_cmask")
    HKC = V(sb("HKC", [128, NITER + 1], F32), "c_hkc")
    DG = V(sb("DG", [128, 8, 128], BF16), "DG")
    RELB = V(sb("RELB", [32, 8], F32), "c_relb")
    B31 = V(sb("B31", [8, 1], F32), "c_b31")
    OH = V(sb("OH", [32, 384], F32), "c_oh")
    ONES = V(sb("ONES", [128, 64], F32), "c_ones")
    CST_t = sb("CST", [128, 2, 129], F32)
    CB_t = sb("CB", [128, 4, 129], BF16)
    HM_t = sb("HM", [128, 4, 3], F32)
    HF_t = sb("HF", [128, 44, 2], F32)
    ST_t = sb("ST", [128, 320], F32)
    ARF = 18432
    AR = sb("ARENA", [128, ARF], F32)

    st_off = [0]

    def st(name, n):
        o = st_off[0]
        st_off[0] += n
        assert st_off[0] <= 320
        return V(ST_t[:, o:o + n], "st_" + name)

    def arv(off_bytes, nbytes, dt, key, pat=None, **kw):
        a = AR[:, off_bytes // 4:(off_bytes + nbytes) // 4]
        if dt == BF16:
            a = a.bitcast(BF16)
        if pat is not None:
            a = a.rearrange(pat, **kw)
        return V(a, key)

    PS = [stack.enter_context(nc.psum_tensor("ps%d" % i, [128, 512], F32)) for i in range(8)]
    psrot = {"mm": [0, 0, 4], "acc": [4, 0, 4], "qk": [0, 0, 6], "acc2": [6, 0, 2]}

    def psum(pool):
        b0, i, n = psrot[pool]
        psrot[pool][1] = (i + 1) % n
        k = b0 + i
        return V(PS[k][:, :], "ps%d" % k)

    def bfv(p):
        return V(p.ap.bitcast(BF16), p.key)

    for dst, src in ((GMIX, d_gmix), (GFFN, d_gffn), (MCW, d_mcw), (MCB, d_mcb), (GBIAS, d_gbias), (GKB, d_gk),
                     (FCW, d_fcw), (FCB, d_fcb), (IDENT, d_ident), (CMASK, d_cmask), (HKC, d_hkc), (RELB, d_relb),
                     (B31, d_b31), (OH, d_oh)):
        E.dma("sp", "d_" + dst.key, out=dst[:], in_=V(src, "dram_" + dst.key))
    E.op("dve", "tensor_copy", out=IDENTB[:], in_=IDENT[:])
    E._emit("dve", "memset", dict(ap=ONES.ap, constant=1.0), [], [ONES.key], "dve", 1)
    E._emit("pool", "memset", dict(ap=CST_t[:, :, :], constant=0.0), [], ["CST0", "CST1"], "pool", 1)
    E._emit("pool", "memset", dict(ap=CB_t[:, :, :], constant=0.0), [], ["CB0", "CB1", "CB2", "CB3"], "pool", 1)
    E._emit("pool", "memset", dict(ap=VP_t[:, :, :, 64:65], constant=1.0), [], ["VPones"], "pool", 1)

    TVS = arv(0, 768, BF16, "tvs")
    NB31 = st("nb31", 1)
    E.op("dve", "tensor_scalar", out=NB31[0:8, :], in0=B31[:], scalar1=-1.0, scalar2=None, op0=ALU.mult)
    pt = psum("mm")
    E.op("pe", "matmul", out=pt[0:8, 0:384], lhsT=RELB[:], rhs=OH[:], start=True, stop=True)
    E.op("act", "activation", out=TVS[0:8, :], in_=pt[0:8, 0:384], func=AF.Exp, bias=NB31[0:8, :], scale=1.0)
    E._emit("dve", "memset", dict(ap=TVS.ap[0:8, 0:127], constant=0.0), ["tvs"], ["tvs"], "dve", 1)
    TVD = V(tvd, "tvd")
    E.dma("sp", "d_tvd", out=TVD[:, :], in_=TVS[0:8, :])
    WTR = arv(1024, 4096, BF16, "WTR", "p (h c) -> p h c", h=8)
    JREVB = arv(5120, 256, BF16, "JREVB")
    JREVF = arv(5632, 512, F32, "JREVF")
    E.dma("sp", "d_jrev", out=JREVF[:], in_=V(d_jrev, "dram_jrev"))
    E.op("dve", "tensor_copy", out=JREVB[:], in_=JREVF[:])
    for h in range(8):
        src = bass.AP(tensor=tvd.tensor, offset=h * 384, ap=[[1, 128], [1, 256]])
        E.dma("sp", "d_wt", out=WTR[:, h, :], in_=V(src, "tvd"))
    for q4 in range(4):
        pw = psum("mm")
        E.op("pe", "matmul", out=pw[:, :], lhsT=JREVB[:],
             rhs=WTR[:, 2 * q4:2 * q4 + 2, :].f(lambda a: a.rearrange("p h c -> p (h c)")), start=True, stop=True)
        E.op("dve", "tensor_copy", out=WT[:, 2 * q4:2 * q4 + 2, :],
             in_=pw[:, :].f(lambda a: a.rearrange("p (h c) -> p h c", h=2)))
    E.barrier()

    STG = [arv(1024 + i * 16384, 16384, F32, "stg%d" % i, "p (a b) -> p a b", a=8) for i in range(3)]
    STB = [arv(1024 + 49152 + i * 8192, 8192, BF16, "stb%d" % i, "p (a b) -> p a b", a=8) for i in range(2)]
    groups = []
    w_in_r = w_in.rearrange("(c p) n -> p c n", p=128)
    w_out_r = w_out.rearrange("(c p) n -> p c n", p=128)
    w_up_r = w_up.rearrange("(c p) n -> p c n", p=128)
    w_dn_r = w_down.rearrange("(c p) n -> p c n", p=128)
    for g, pieces in enumerate(WIN_GROUPS):
        pcs, c0 = [], 0
        for (s0, n) in pieces:
            pcs.append((w_in_r[:, :, s0:s0 + n], c0, n))
            c0 += n
        groups.append((sc_in[g], "sc_in%d" % g, pcs, 8, c0, GMIX))
    for g in range(2):
        groups.append((sc_out[g], "sc_out%d" % g, [(w_out_r[:, :, g * 512:(g + 1) * 512], 0, 512)], 8, 512, None))
    for g in range(11):
        groups.append((sc_up[g], "sc_up%d" % g,
                       [(w_up_r[:, :, 256 * g:256 * g + 256], 0, 256),
                        (w_up_r[:, :, 2816 + 256 * g:2816 + 256 * g + 256], 256, 256)], 8, 512, GFFN))
    for hf in range(2):
        for kg in range(3):
            nk = 8 if kg < 2 else 6
            groups.append((sc_dn[hf * 3 + kg], "sc_dn%d" % (hf * 3 + kg),
                           [(w_dn_r[:, 8 * kg:8 * kg + nk, hf * 512:(hf + 1) * 512], 0, 512)], nk, 512, None))
    cast_engs = ["dve", "act"]
    for gi, (dst, dkey, pcs, kc, ncols, scl) in enumerate(groups):
        sl = gi % 3
        sb2 = gi % 2
        for (src, c0, n) in pcs:
            E.dma("sp", "d_stg%d" % sl, out=STG[sl][:, 0:kc, c0:c0 + n], in_=V(src, "dram_w"))
        eng = cast_engs[gi % 2]
        if scl is None:
            if eng == "act":
                E.op("act", "activation", out=STB[sb2][:, 0:kc, 0:ncols], in_=STG[sl][:, 0:kc, 0:ncols], func=AF.Copy)
            else:
                E.op(eng, "tensor_copy", out=STB[sb2][:, 0:kc, 0:ncols], in_=STG[sl][:, 0:kc, 0:ncols])
        else:
            for c in range(kc):
                if eng == "act":
                    E.op("act", "activation", out=STB[sb2][:, c, 0:ncols], in_=STG[sl][:, c, 0:ncols],
                         func=AF.Copy, scale=scl[:, c:c + 1])
                else:
                    E.op(eng, "tensor_scalar", out=STB[sb2][:, c, 0:ncols], in0=STG[sl][:, c, 0:ncols],
                         scalar1=scl[:, c:c + 1], scalar2=None, op0=ALU.mult)
        E.dma("pool", "d_stb%d" % sb2, out=V(dst[:, 0:kc, 0:ncols], dkey), in_=STB[sb2][:, 0:kc, 0:ncols])
    E.barrier()

    wseq = []
    for I in range(NB):
        for g in range(8):
            wseq.append((sc_in[g], "sc_in%d" % g, 8, 512 if g < 7 else 80))
        for g in range(2):
            wseq.append((sc_out[g], "sc_out%d" % g, 8, 512))
        for g in range(11):
            wseq.append((sc_up[g], "sc_up%d" % g, 8, 512))
        for g in range(6):
            wseq.append((sc_dn[g], "sc_dn%d" % g, 8 if g % 3 < 2 else 6, 512))
    wstate = {"loaded": 0, "next": 0}

    def get_w():
        idx = wstate["next"]
        wstate["next"] += 1
        while wstate["loaded"] <= idx + 2 and wstate["loaded"] < len(wseq):
            li = wstate["loaded"]
            src, skey, kc, ncols = wseq[li]
            slot = li % 3
            E.dma("sp", "d_ws%d" % slot, out=WS[slot][:, 0:kc, 0:ncols], in_=V(src[:, 0:kc, 0:ncols], skey))
            wstate["loaded"] += 1
        return WS[idx % 3]

    HB = arv(0, 2048, BF16, "HB")
    HBA = [HB, arv(38400, 2048, BF16, "HBb")]
    HBE = [HB, arv(67584, 2048, BF16, "HBc")]
    HT = arv(2048, 8192, BF16, "HT", "p (a b) -> p a b", a=8)
    QKT = arv(10240, 4096, BF16, "QKT", "p (a b) -> p a b", a=4)
    YC = [arv(14336 + i * 2048, 2048, F32, "YC%d" % i) for i in range(2)]
    VM_full = AR[:, 18432 // 4:(18432 + 4160) // 4].bitcast(BF16)[:, 0:2064].rearrange("p (t h d) -> p t h d", t=4, h=4)
    VM = V(VM_full, "VM")
    OG = arv(22592, 4096, BF16, "OG", "p (a b) -> p a b", a=4)
    TRI4 = arv(26688, 2048, F32, "TRI4")
    MNORM = arv(28736, 2048, F32, "MNORM")
    IKN = arv(30784, 128, BF16, "IKN")
    KW = arv(30912, 512, BF16, "KW", "p (h d) -> p h d", h=4)
    PTB = [arv(37056 + i * 256, 256, BF16, "PT%d" % i) for i in range(4)]
    HMO = arv(31936, 1024, BF16, "HMO")
    JUNK = arv(32960, 4096, F32, "JUNK")
    AQT = arv(65536, 4096, BF16, "AQT", "p (a b) -> p a b", a=4)
    IQT = arv(69632, 4096, BF16, "IQT", "p (a b) -> p a b", a=4)
    SCK = [V(AR[:, (0 + kc * 2048) // 4:(0 + (kc + 1) * 2048) // 4], "SC%d" % kc) for kc in range(8)]
    SCall = V(AR[:, 0:4096], ["SC%d" % kc for kc in range(8)])
    MQ = arv(16384, 8192, BF16, "MQ")
    MT = arv(24576, 32768, BF16, "MT", "p (j q) -> p j q", j=32)
    EP = [arv(57344 + i * 1024, 1024, BF16, "EP%d" % i) for i in range(8)]
    FT = [arv(i * 2048, 2048, F32, "FT%d" % i) for i in range(4)]
    RB = [arv(61440 + i * 2048, 2048, F32, "RB%d" % i) for i in range(2)]
    RBF = [arv(61440 + i * 1024, 1024, BF16, "RBF%d" % i) for i in range(4)]
    YE = [arv(10240 + i * 2048, 2048, F32, "YE%d" % i) for i in range(4)]
    AT = arv(18432, 22528, BF16, "AT", "p (c q) -> p c q", c=22)
    GFIN = arv(40960, 4096, F32, "GFIN")
    OUTB = [arv(45056 + i * 4096, 4096, F32, "OUTB%d" % i) for i in range(2)]
    JUNK2 = arv(53248, 4096, F32, "JUNK2")
    X2 = [V(AR[:, 10240 // 4:(10240 + 4096) // 4], ["X2_0", "YE0", "YE1"]),
          V(AR[:, 14336 // 4:(14336 + 4096) // 4], ["X2_1", "YE2", "YE3"]),
          V(AR[:, 57344 // 4:(57344 + 4096) // 4], ["X2_2", "UE0", "UE1"]),
          V(AR[:, 61440 // 4:(61440 + 4096) // 4], ["X2_3", "UE1", "UE2", "UE3"])]
    UE = [arv(57344 + i * 2064, 2056, F32, "UE%d" % i) for i in range(4)]

    SS = [st("ss%d" % t_, 1) for t_ in range(4)]
    RSTD = [st("rstd%d" % t_, 1) for t_ in range(4)]
    SSK = [st("ssk%d" % t_, 1) for t_ in range(4)]
    RK = [st("rk%d" % t_, 1) for t_ in range(4)]
    GATES = [st("gates%d" % t, 8) for t in range(4)]
    WQ = [st("wq%d" % t, 8) for t in range(4)]
    E1 = st("e1", 4)
    NLF = st("nlf", 4)
    GS = st("gs", 16)
    EB8 = st("eb8", 4)
    T1 = st("t1", 4)
    EK = st("ek", 4)
    T2 = st("t2", 4)
    WKs = st("wk", 4)
    EG = st("eg", 8)
    DN = st("dn", 4)
    RR = st("rr", 4)
    MS = st("ms", 4)
    TT = st("tt", 4)
    SCL = st("scl", 4)
    MX = st("mx", 1)
    MN2 = [st("mn0", 1), st("mn1", 1)]
    W0 = st("w0", 1)
    NHK = st("nhk", NITER + 1)
    NM = st("nm", 1)
    SACC = st("sacc", 1)
    SG = st("sg", 1)
    LO = st("lo", 1)
    MID = st("mid", 1)
    CNT = st("cnt", 1)
    TMP = st("tmp", 1)
    SS2 = [st("ss2%d" % t_, 1) for t_ in range(4)]
    RSTD2 = [st("rstd2%d" % t_, 1) for t_ in range(4)]
    SS3 = [st("ss3%d" % t_, 1) for t_ in range(4)]
    RSTD3 = [st("rstd3%d" % t_, 1) for t_ in range(4)]

    CST = [V(CST_t[:, pp, :], "CST%d" % pp) for pp in range(2)]
    CBh = [V(CB_t[:, h, :], "CB%d" % h) for h in range(4)]
    HMh = [V(HM_t[:, c, :], "HM%d" % c) for c in range(4)]
    HFh = [V(HF_t[:, c, :], "HF%d" % c) for c in range(44)]
    MIXc = [V(MIXT[:, c, :], "MIX%d" % c) for c in range(8)]
    MIX03 = V(MIXT[:, 0:4, :], ["MIX0", "MIX1", "MIX2", "MIX3"])

    def rstd_batch(srcs, sss, rvs, junk, n):
        for t_ in range(len(srcs)):
            E.op("act", "activation", out=junk, in_=srcs[t_], func=AF.Square, accum_out=sss[t_])
        for t_ in range(len(srcs)):
            E.op("dve", "tensor_scalar", out=rvs[t_], in0=sss[t_], scalar1=1.0 / n, scalar2=EPS, op0=ALU.mult, op1=ALU.add)
        for t_ in range(len(srcs)):
            E.op("act", "activation", out=rvs[t_], in_=rvs[t_], func=AF.Sqrt)
        for t_ in range(len(srcs)):
            E.op("dve", "reciprocal", out=rvs[t_], in_=rvs[t_])

    def rstd_from_ss(ssv, rv, n):
        E.op("dve", "tensor_scalar", out=rv, in0=ssv, scalar1=1.0 / n, scalar2=EPS, op0=ALU.mult, op1=ALU.add)
        E.op("act", "activation", out=rv, in_=rv, func=AF.Sqrt)
        E.op("dve", "reciprocal", out=rv, in_=rv)

    def transposes_to(dst_view, src, nchunk):
        p = psum("mm")
        pb = bfv(p)
        for c in range(nchunk):
            E.op("pe", "transpose", out=pb[:, c * 128:(c + 1) * 128], in_=src[:, c * 128:(c + 1) * 128],
                 identity=IDENTB[:])
        E.op("act", "activation", out=dst_view,
             in_=pb[:, 0:nchunk * 128].f(lambda a: a.rearrange("p (c q) -> p c q", c=nchunk)), func=AF.Copy)

    rr = {"i": 0}

    def alt(*engs):
        rr["i"] += 1
        return engs[rr["i"] % len(engs)]

    for I in range(NB):
        tok0 = I * 512
        E.dma("sp", "d_tri4", out=TRI4[:], in_=V(d_tri4, "dram_tri4"))
        E.dma("sp", "d_mnorm", out=MNORM[:], in_=V(d_mnorm, "dram_mnorm"))
        if I == 0:
            for t in range(4):
                E.dma("sp", "d_x%d" % t, out=X1[t][:], in_=V(x[tok0 + t * 128:tok0 + (t + 1) * 128, :], "dram_x"))
        rstd_batch([X1[t][:] for t in range(4)], [SS[t][:] for t in range(4)], [RSTD[t][:] for t in range(4)],
                   JUNK[:], 1024.0)
        for t in range(4):
            E.op("dve", "tensor_scalar", out=HBA[t % 2][:], in0=X1[t][:], scalar1=RSTD[t][:], scalar2=None, op0=ALU.mult)
            transposes_to(HT[:, :, t * 128:(t + 1) * 128], HBA[t % 2], 8)

        def fm_group(W, evac):
            for cc in range(4):
                p = psum("mm")
                for c in range(8):
                    E.op("pe", "matmul", out=p[:, :], lhsT=W[:, c, cc * 128:(cc + 1) * 128], rhs=HT[:, c, :],
                         start=(c == 0), stop=(c == 7))
                evac(cc, p)

        def tm_group(W, ncols, evac):
            for t in range(4):
                p = psum("mm")
                for c in range(8):
                    E.op("pe", "matmul", out=p[:, 0:ncols], lhsT=HT[:, c, t * 128:(t + 1) * 128], rhs=W[:, c, 0:ncols],
                         start=(c == 0), stop=(c == 7))
                evac(t, p)

        def ev_g0(cc, p):
            Y = YC[cc % 2]
            E.op("act", "activation", out=Y[:], in_=p[:, :], func=AF.Identity, scale=MCW[:, cc * 4 + 3:cc * 4 + 4],
                 bias=MCB[:, cc:cc + 1])
            for j, sh in ((2, 1), (1, 2), (0, 3)):
                E.op("dve", "scalar_tensor_tensor", out=Y[:, sh:512], in0=p[:, 0:512 - sh],
                     scalar=MCW[:, cc * 4 + j:cc * 4 + j + 1], in1=Y[:, sh:512], op0=ALU.mult, op1=ALU.add)
            if I > 0:
                H = HMh[cc]
                E.op("dve", "scalar_tensor_tensor", out=Y[:, 0:3], in0=H[:, 0:3], scalar=MCW[:, cc * 4:cc * 4 + 1],
                     in1=Y[:, 0:3], op0=ALU.mult, op1=ALU.add)
                E.op("dve", "scalar_tensor_tensor", out=Y[:, 0:2], in0=H[:, 1:3], scalar=MCW[:, cc * 4 + 1:cc * 4 + 2],
                     in1=Y[:, 0:2], op0=ALU.mult, op1=ALU.add)
                E.op("dve", "scalar_tensor_tensor", out=Y[:, 0:1], in0=H[:, 2:3], scalar=MCW[:, cc * 4 + 2:cc * 4 + 3],
                     in1=Y[:, 0:1], op0=ALU.mult, op1=ALU.add)
            E.op("act", "activation", out=HMh[cc][:], in_=p[:, 509:512], func=AF.Copy)
            E.op("act", "activation", out=QKT[:, cc, :], in_=Y[:], func=AF.Silu)
        fm_group(get_w(), ev_g0)

        def ev_g1(t, p):
            E.op("act", "activation", out=VM[:, t, :, 0:128],
                 in_=p[:, :].f(lambda a: a.rearrange("p (h d) -> p h d", h=4)), func=AF.Copy)
        E._emit("pool", "memset", dict(ap=VM.ap[:, :, :, 128:129], constant=1.0), [], ["VM"], "pool", 1)
        tm_group(get_w(), 512, ev_g1)

        def ev_g2(t, p):
            E.op("act", "activation", out=OG[:, t, :], in_=p[:, :], func=AF.Sigmoid)
            E.op("pool", "tensor_tensor", out=OG[:, t, :], in0=OG[:, t, :], in1=MNORM[:], op=ALU.mult)
        tm_group(get_w(), 512, ev_g2)

        def ev_g3(cc, p):
            E.op("act", "activation", out=AQT[:, cc, :], in_=p[:, :], func=AF.Copy, scale=0.125)
        fm_group(get_w(), ev_g3)

        def ev_g4(cc, p):
            E.op("dve", "tensor_copy", out=KT[:, cc, tok0:tok0 + 512], in_=p[:, :])
        fm_group(get_w(), ev_g4)

        def ev_g5(t, p):
            E.op("act", "activation", out=V(VP_t[:, I * 4 + t, :, 0:64], "VP"),
                 in_=p[:, :].f(lambda a: a.rearrange("p (h d) -> p h d", h=8)), func=AF.Copy)
        tm_group(get_w(), 512, ev_g5)

        def ev_g6(cc, p):
            E.op("dve", "tensor_copy", out=IQT[:, cc, :], in_=p[:, :])
        fm_group(get_w(), ev_g6)

        def ev_g7(t, p):
            E.op("act", "activation", out=JUNK[:, 0:64], in_=p[:, 0:64], func=AF.Square, accum_out=SSK[t][:])
            rstd_from_ss(SSK[t][:], RK[t][:], 64.0)
            E.op("dve", "scalar_tensor_tensor", out=IKN[:], in0=p[:, 0:64], scalar=RK[t][:], in1=GKB[:],
                 op0=ALU.mult, op1=ALU.mult)
            E.op("dve", "tensor_scalar", out=WQ[t][:], in0=p[:, 64:72], scalar1=float(8 ** -0.5 * 64 ** -0.5),
                 scalar2=None, op0=ALU.mult)
            E.op("dve", "tensor_tensor", out=GATES[t][:], in0=p[:, 72:80], in1=GBIAS[:], op=ALU.add)
            p2 = psum("mm")
            pb = bfv(p2)
            E.op("pe", "transpose", out=pb[0:64, 0:128], in_=IKN[:], identity=IDENTB[:])
            c0 = tok0 + t * 128
            E.op("dve", "tensor_copy", out=IK[0:64, c0:c0 + 128], in_=pb[0:64, 0:128])
            E.op("dve", "tensor_copy", out=IK[64:128, c0:c0 + 128], in_=pb[0:64, 0:128])
        tm_group(get_w(), 80, ev_g7)

        for t in range(4):
            c0 = t * 128
            pk = psum("mm")
            pkb = bfv(pk)
            for pp in range(2):
                E.op("pe", "transpose", out=pkb[:, pp * 128:(pp + 1) * 128], in_=QKT[:, 2 + pp, c0:c0 + 128],
                     identity=IDENTB[:])
            G = GATES[t]
            E.op("act", "activation", out=E1[:], in_=G[:, 4:8], func=AF.Exp, scale=-1.0)
            E.op("dve", "tensor_scalar", out=E1[:], in0=E1[:], scalar1=1.0, scalar2=None, op0=ALU.add)
            E.op("act", "activation", out=NLF[:], in_=E1[:], func=AF.Ln)
            pg = psum("mm")
            for k in range(4):
                E.op("pe", "matmul", out=pg[:, 4 * k:4 * k + 4], lhsT=TRI4[:, 128 * k:128 * (k + 1)], rhs=NLF[:],
                     start=True, stop=True)
            E.op("dve", "tensor_copy", out=GS[:], in_=pg[:, 0:16])
            E.op("act", "activation", out=EB8[:], in_=GS[:, 0:4], func=AF.Exp, scale=-1.0, bias=-math.log(8.0))
            E.op("dve", "tensor_tensor", out=T1[:], in0=G[:, 0:4], in1=GS[:, 0:4], op=ALU.add)
            E.op("act", "activation", out=EK[:], in_=T1[:], func=AF.Exp)
            E.op("dve", "tensor_tensor", out=T2[:], in0=T1[:], in1=GS[:, 4:8], op=ALU.subtract)
            E.op("act", "activation", out=WKs[:], in_=T2[:], func=AF.Exp)
            E.op("act", "activation", out=EG[:], in_=GS[:, 8:16], func=AF.Exp, scale=-1.0)
            E.op("dve", "tensor_tensor", out=KW[:],
                 in0=pkb[:, 0:256].f(lambda a: a.rearrange("p (h d) -> p h d", h=4)),
                 in1=WKs[:, 0:4].f(lambda a: a.unsqueeze(2).to_broadcast([128, 4, 64])), op=ALU.mult)
            npss = [psum("acc") for _ in range(4)]
            prs = []
            for h in range(4):
                pp, hh = h // 2, h % 2
                base = 64 * hh
                pr = psum("mm")
                E.op("pe", "matmul", out=pr[:, 0:128], lhsT=QKT[base:base + 64, 2 + pp, c0:c0 + 128],
                     rhs=QKT[base:base + 64, pp, c0:c0 + 128], start=True, stop=True)
                prs.append(pr)
            for h in range(4):
                E.op("dve", "scalar_tensor_tensor", out=PTB[h][:], in0=prs[h][:, 0:128], scalar=EK[:, h:h + 1],
                     in1=TRI4[:, 0:128], op0=ALU.mult, op1=ALU.mult)
            for h in range(4):
                E.op("pe", "matmul", out=npss[h][:, 0:129], lhsT=PTB[h][:], rhs=VM[:, t, h, :], start=True, stop=False,
                     skip_group_check=True)
            for ch in range(2):
                r0 = 64 * ch
                pkvs = []
                for h in range(4):
                    pp, hh = h // 2, h % 2
                    base, o = 64 * hh, 0
                    E.op("pe", "matmul", out=npss[h][r0:r0 + 64, o:o + 129], lhsT=QKT[:, pp, c0 + r0:c0 + r0 + 64],
                         rhs=CBh[h][:], start=False, stop=(ch == 1), skip_group_check=True)
                    pkv = psum("mm")
                    E.op("pe", "matmul", out=pkv[base:base + 64, 0:129], lhsT=KW[r0:r0 + 64, h, :],
                         rhs=VM[r0:r0 + 64, t, h, :], start=True, stop=True)
                    pkvs.append(pkv)
                for h in range(4):
                    pp, hh = h // 2, h % 2
                    base = 64 * hh
                    E.op("dve", "scalar_tensor_tensor", out=CST[pp][base:base + 64, :], in0=CST[pp][base:base + 64, :],
                         scalar=EG[base:base + 64, 4 * ch + h:4 * ch + h + 1], in1=pkvs[h][base:base + 64, 0:129],
                         op0=ALU.mult, op1=ALU.add)
                for h in range(4):
                    pp, hh = h // 2, h % 2
                    base = 64 * hh
                    E.op("act", "activation", out=CBh[h][base:base + 64, :], in_=CST[pp][base:base + 64, :],
                         func=AF.Copy)
            for h in range(4):
                E.op("dve", "tensor_tensor", out=DN[:, h:h + 1], in0=npss[h][:, 128:129], in1=EB8[:, h:h + 1], op=ALU.mult)
            E.op("act", "activation", out=DN[:], in_=DN[:], func=AF.Abs)
            E.op("dve", "tensor_scalar", out=DN[:], in0=DN[:], scalar1=1.0, scalar2=None, op0=ALU.max)
            E.op("dve", "reciprocal", out=DN[:], in_=DN[:])
            E.op("dve", "tensor_tensor", out=RR[:], in0=DN[:], in1=EB8[:, 0:4], op=ALU.mult)
            for h in range(4):
                E.op("act", "activation", out=JUNK[:, h * 128:(h + 1) * 128], in_=npss[h][:, 0:128], func=AF.Square,
                     accum_out=MS[:, h:h + 1])
            E.op("dve", "tensor_tensor", out=TT[:], in0=RR[:], in1=RR[:], op=ALU.mult)
            E.op("dve", "tensor_tensor", out=TT[:], in0=TT[:], in1=MS[:], op=ALU.mult)
            E.op("dve", "tensor_scalar", out=TT[:], in0=TT[:], scalar1=1.0 / 128.0, scalar2=EPS, op0=ALU.mult,
                 op1=ALU.add)
            E.op("act", "activation", out=TT[:], in_=TT[:], func=AF.Ln)
            E.op("act", "activation", out=TT[:], in_=TT[:], func=AF.Exp, scale=-0.5)
            E.op("dve", "tensor_tensor", out=SCL[:], in0=TT[:], in1=RR[:], op=ALU.mult)
            for h in range(4):
                E.op("dve", "scalar_tensor_tensor", out=HMO[:, h * 128:(h + 1) * 128],
                     in0=npss[h][:, 0:128], scalar=SCL[:, h:h + 1], in1=OG[:, t, h * 128:(h + 1) * 128],
                     op0=ALU.mult, op1=ALU.mult)
            transposes_to(MIX03[:, :, c0:c0 + 128], HMO, 4)

        E.barrier()
        nkt = 4 * I + 4
        X1flat = X1_t[:].rearrange("p a b -> p (a b)")
        SCB = [
            ([SCK[kc] for kc in range(8)], SCall),
            ([V(X1_t[:, kc // 2, (kc % 2) * 512:(kc % 2) * 512 + 512], "X1_%d" % (kc // 2)) for kc in range(8)],
             V(X1flat, ["X1_0", "X1_1", "X1_2", "X1_3"])),
        ]

        def score_gen(t):
            i = 4 * I + t
            n = 128 * (i + 1)
            nch = (n + 511) // 512
            sck, scall = SCB[0 if DBG_NOX1 else t % 2]
            for h in range(8):
                E.op("dve", "tensor_scalar", out=DG[:, h, :], in0=IDENT[:], scalar1=WQ[t][:, h:h + 1], scalar2=None,
                     op0=ALU.mult)
            dk = i // 4
            for kc in range(nch):
                w = min(512, n - 512 * kc)
                pacc = psum("acc")
                pis = {}

                def emit_isc(h, kc=kc, w=w):
                    base, pp = 64 * (h % 2), h // 2
                    p = psum("mm")
                    E.op("pe", "matmul", out=p[:, 0:w], lhsT=IQT[base:base + 64, pp, t * 128:(t + 1) * 128],
                         rhs=IK[base:base + 64, 512 * kc:512 * kc + w], start=True, stop=True)
                    pis[h] = p
                for h in range(3):
                    emit_isc(h)
                for h in range(8):
                    if h + 3 < 8:
                        emit_isc(h + 3)
                    p = pis.pop(h)
                    R = RBF[h % 4]
                    if h % 2 == 0:
                        E.op("act", "activation", out=R[:, 0:w], in_=p[:, 0:w], func=AF.Relu)
                    else:
                        E.op("dve", "tensor_scalar", out=R[:, 0:w], in0=p[:, 0:w], scalar1=0.0, scalar2=None, op0=ALU.max)
                    E.op("pe", "matmul", out=pacc[:, 0:w], lhsT=DG[:, h, :], rhs=R[:, 0:w], start=(h == 0),
                         stop=(h == 7), skip_group_check=True)
                    if h == 7:
                        E.op("dve", "tensor_copy", out=sck[kc][:, 0:w], in_=pacc[:, 0:w])
                    yield (h == 7)
            if n > topk:
                E.op("dve", "tensor_reduce", out=MN2[t % 2][:], in_=scall[:, 0:n], axis=AX.X, op=ALU.min)
            E.op("dve", "tensor_tensor", out=sck[dk][:, (i % 4) * 128:(i % 4) * 128 + 128],
                 in0=sck[dk][:, (i % 4) * 128:(i % 4) * 128 + 128], in1=CMASK[:], op=ALU.add)
            yield True

        def bisect_gen(t):
            i = 4 * I + t
            n = 128 * (i + 1)
            sck, scall = SCB[0 if DBG_NOX1 else t % 2]
            MNt = MN2[t % 2]
            if n > topk:
                E.op("dve", "tensor_reduce", out=MX[:], in_=scall[:, 0:n], axis=AX.X, op=ALU.max)
                E.op("dve", "tensor_tensor", out=W0[:], in0=MX[:], in1=MNt[:], op=ALU.subtract)
                E.op("dve", "tensor_scalar", out=W0[:], in0=W0[:], scalar1=1.0001, scalar2=1e-6, op0=ALU.mult, op1=ALU.add)
                E.op("dve", "tensor_scalar", out=NHK[:], in0=HKC[:], scalar1=W0[:], scalar2=-1.0, op0=ALU.mult, op1=ALU.mult)
                E.op("dve", "scalar_tensor_tensor", out=NM[:], in0=MNt[:], scalar=-1.0, in1=NHK[:, 0:1], op0=ALU.mult,
                     op1=ALU.add)
                cthr = float(2 * topk - n) - 0.5
                split = n >= 1536
                if split:
                    nA = ((int(0.56 * n) + 127) // 128) * 128
                    nB = n - nA
                    MQa = V(MQ.ap[:, 0:nA], "MQa")
                    MQb = V(MQ.ap[:, nA:n], "MQb")
                    E.op("dve", "tensor_scalar", out=MID[:], in0=NM[:], scalar1=-1.0, scalar2=None, op0=ALU.mult)
                yield
                for k in range(NITER):
                    if split:
                        E.op("act", "activation", out=MQa, in_=scall[:, 0:nA], func=AF.Sign, bias=NM[:], scale=1.0,
                             accum_out=SACC[:])
                        E.op("dve", "tensor_scalar", out=MQb, in0=scall[:, nA:n], scalar1=MID[:], scalar2=None,
                             op0=ALU.is_ge, op1=ALU.add, accum_out=CNT[:])
                        yield
                        E.op("dve", "tensor_scalar", out=TMP[:], in0=CNT[:], scalar1=2.0, scalar2=-(float(nB) + cthr),
                             op0=ALU.mult, op1=ALU.add)
                        E.op("act", "activation", out=SG[:], in_=SACC[:], func=AF.Sign, bias=TMP[:], scale=1.0)
                    else:
                        E.op("act", "activation", out=MQ[:, 0:n], in_=scall[:, 0:n], func=AF.Sign, bias=NM[:], scale=1.0,
                             accum_out=SACC[:])
                        yield
                        E.op("act", "activation", out=SG[:], in_=SACC[:], func=AF.Sign, bias=-cthr, scale=1.0)
                    E.op("act", "activation", out=NM[:], in_=SG[:], func=AF.Identity, scale=NHK[:, k + 1:k + 2], bias=NM[:])
                    if split and k + 1 < NITER:
                        E.op("dve", "tensor_scalar", out=MID[:], in0=NM[:], scalar1=-1.0, scalar2=None, op0=ALU.mult)
                E.op("act", "activation", out=LO[:], in_=NM[:], func=AF.Identity, scale=-1.0, bias=NHK[:, NITER:NITER + 1])
                E.op("dve", "tensor_scalar", out=V(MQ.ap[:, 0:n], ["MQ", "MQa", "MQb"]), in0=scall[:, 0:n], scalar1=LO[:],
                     scalar2=None, op0=ALU.is_ge)
            else:
                E.op("dve", "tensor_scalar", out=MQ[:, 0:n], in0=scall[:, 0:n], scalar1=-1.0e29, scalar2=None,
                     op0=ALU.is_ge)
            if t < 3:
                E._emit("pool", "memset", dict(ap=MQ.ap[:, n:128 * nkt], constant=0.0), [], ["MQ"], "pool", 1)
            yield "pre_transpose"
            for jg in range((nkt + 3) // 4):
                p = psum("mm")
                pb = bfv(p)
                for jj in range(4):
                    j = 4 * jg + jj
                    E.op("pe", "transpose", out=pb[:, jj * 128:(jj + 1) * 128], in_=MQ[:, 128 * j:128 * (j + 1)],
                         identity=IDENTB[:])
                E.op("dve", "tensor_copy", out=MT[:, 4 * jg:4 * jg + 4, t * 128:(t + 1) * 128],
                     in_=pb[:, 0:512].f(lambda a: a.rearrange("p (j q) -> p j q", j=4)))
            yield

        for _ in score_gen(0):
            pass
        for t in range(4):
            bg = bisect_gen(t)
            sg = score_gen(t + 1) if t + 1 < 4 else None
            nb_steps = NITER + 2
            ns_steps = (8 * ((128 * (4 * I + t + 2) + 511) // 512) + 1) if sg is not None else 0
            ratio = float(ns_steps) / nb_steps
            if DBG_NOOVERLAP:
                ratio = 0.0
            accr = 0.0
            state = {"sg": sg, "bnd": True}

            def adv():
                try:
                    state["bnd"] = bool(next(state["sg"]))
                except StopIteration:
                    state["sg"] = None
                    state["bnd"] = True
            for tag in bg:
                if tag == "pre_transpose":
                    while state["sg"] is not None and not state["bnd"]:
                        adv()
                    continue
                accr += ratio
                while state["sg"] is not None and accr >= 1.0:
                    accr -= 1.0
                    adv()
            while state["sg"] is not None:
                adv()
        for t in range(4):
            E.dma("sp", "d_x%d" % t, out=X1[t][:], in_=V(x[tok0 + t * 128:tok0 + (t + 1) * 128, :], "dram_x"))
        for pp in range(4):
            accs = [psum("acc2"), psum("acc2")]
            qk = {}

            def emit_qk(j, pp=pp):
                for hh in range(2):
                    base = 64 * hh
                    p = psum("qk")
                    E.op("pe", "matmul", out=p[:, :], lhsT=KT[base:base + 64, pp, 128 * j:128 * (j + 1)],
                         rhs=AQT[base:base + 64, pp, :], start=True, stop=True)
                    qk[(j, hh)] = p
            for j in range(min(2, nkt)):
                emit_qk(j)
            for j in range(nkt):
                if j + 2 < nkt:
                    emit_qk(j + 2)
                for hh in range(2):
                    h = 2 * pp + hh
                    p = qk.pop((j, hh))
                    e = EP[(2 * j + hh) % 8]
                    E.op("act", "activation", out=e[:], in_=p[:, :], func=AF.Exp)
                    E.op("dve", "tensor_tensor", out=e[:], in0=e[:], in1=MT[:, j, :], op=ALU.mult)
                    r = j - 4 * I
                    if r >= -1:
                        for tq, v in ((r, 0), (r + 1, 1)):
                            if 0 <= tq <= 3:
                                E.op("dve", "tensor_tensor", out=e[:, tq * 128:(tq + 1) * 128],
                                     in0=e[:, tq * 128:(tq + 1) * 128], in1=WT[:, h, v * 128:(v + 1) * 128], op=ALU.mult)
                    E.op("pe", "matmul", out=accs[hh][0:65, :], lhsT=V(VP_t[:, j, h, :], ["VP", "VPones"]), rhs=e[:],
                         start=(j == 0), stop=(j == nkt - 1))
            for hh in range(2):
                E.op("act", "activation", out=FT[2 * hh][64:65, :], in_=accs[hh][64:65, :], func=AF.Ln)
            for hh in range(2):
                E.op("act", "activation", out=FT[2 * hh][64:65, :], in_=FT[2 * hh][64:65, :], func=AF.Exp, scale=-1.0)
            pbcs = []
            for hh in range(2):
                pbc = psum("qk")
                E.op("pe", "matmul", out=pbc[0:64, :], lhsT=ONES[64:65, 0:64], rhs=FT[2 * hh][64:65, :], start=True,
                     stop=True)
                pbcs.append(pbc)
            for hh in range(2):
                E.op("act", "activation", out=FT[2 * hh + 1][0:64, :], in_=pbcs[hh][0:64, :], func=AF.Copy)
            for hh in range(2):
                base = 64 * hh
                E.op("dve", "tensor_tensor", out=MIXc[4 + pp][base:base + 64, :], in0=accs[hh][0:64, :],
                     in1=FT[2 * hh + 1][0:64, :], op=ALU.mult)

        E.barrier()
        E.dma("sp", "d_gfin", out=GFIN[:], in_=V(d_gfin, "dram_gfin"))
        for hf in range(2):
            W = get_w()
            for t in range(4):
                p = psum("mm")
                for c in range(8):
                    E.op("pe", "matmul", out=p[:, :], lhsT=MIXc[c][:, t * 128:(t + 1) * 128], rhs=W[:, c, :],
                         start=(c == 0), stop=(c == 7))
                E.op("dve", "tensor_tensor", out=X1[t][:, hf * 512:(hf + 1) * 512], in0=p[:, :],
                     in1=X1[t][:, hf * 512:(hf + 1) * 512], op=ALU.add)
        rstd_batch([X1[t][:] for t in range(4)], [SS2[t][:] for t in range(4)], [RSTD2[t][:] for t in range(4)],
                   JUNK2[:], 1024.0)
        for t in range(4):
            E.op("dve", "tensor_scalar", out=HBE[t % 2][:], in0=X1[t][:], scalar1=RSTD2[t][:], scalar2=None, op0=ALU.mult)
            transposes_to(HT[:, :, t * 128:(t + 1) * 128], HBE[t % 2], 8)

        def ffn_conv(p, Y, U, ci):
            E.op("act", "activation", out=Y[:], in_=p[:, :], func=AF.Identity, scale=FCW[:, 3 * ci + 2:3 * ci + 3],
                 bias=FCB[:, ci:ci + 1])
            E.op("act", "activation", out=U[:, 2:514], in_=p[:, :], func=AF.Copy)
            if I > 0:
                E.op("act", "activation", out=U[:, 0:2], in_=HFh[ci][:], func=AF.Copy)
            else:
                E._emit("act", "memzero", dict(ap=U.ap[:, 0:2]), [], [U.key], "act", 1)
            E.op("act", "activation", out=HFh[ci][:], in_=U[:, 512:514], func=AF.Copy)
            E.op("dve", "scalar_tensor_tensor", out=Y[:], in0=U[:, 1:513], scalar=FCW[:, 3 * ci + 1:3 * ci + 2], in1=Y[:],
                 op0=ALU.mult, op1=ALU.add)
            E.op("dve", "scalar_tensor_tensor", out=Y[:], in0=U[:, 0:512], scalar=FCW[:, 3 * ci:3 * ci + 1], in1=Y[:],
                 op0=ALU.mult, op1=ALU.add)

        for g in range(11):
            W = get_w()
            for sub in range(2):
                c = 2 * g + sub
                pg_ = psum("mm")
                pv_ = psum("mm")
                for kc in range(8):
                    E.op("pe", "matmul", out=pg_[:, :], lhsT=W[:, kc, sub * 128:(sub + 1) * 128], rhs=HT[:, kc, :],
                         start=(kc == 0), stop=(kc == 7))
                for kc in range(8):
                    E.op("pe", "matmul", out=pv_[:, :], lhsT=W[:, kc, 256 + sub * 128:256 + (sub + 1) * 128],
                         rhs=HT[:, kc, :], start=(kc == 0), stop=(kc == 7))
                Yg, Yv = YE[(c % 2) * 2], YE[(c % 2) * 2 + 1]
                ffn_conv(pg_, Yg, UE[(c % 2) * 2], c)
                ffn_conv(pv_, Yv, UE[(c % 2) * 2 + 1], 22 + c)
                E.op("act", "activation", out=Yg[:], in_=Yg[:], func=AF.Silu)
                E.op("pool", "tensor_tensor", out=V(AT.ap[:, c, :], "AT%d" % c), in0=Yg[:], in1=Yv[:], op=ALU.mult)
        for hf in range(2):
            accs = [psum("acc") for _ in range(4)]
            for kg in range(3):
                W = get_w()
                nk = 8 if kg < 2 else 6
                for t in range(4):
                    for kk in range(nk):
                        c = 8 * kg + kk
                        E.op("pe", "matmul", out=accs[t][:, :], lhsT=V(AT.ap[:, c, t * 128:(t + 1) * 128], "AT%d" % c), rhs=W[:, kk, :],
                             start=(c == 0), stop=(c == 21), skip_group_check=True)
            for t in range(4):
                E.op("dve", "tensor_tensor", out=X2[t][:, hf * 512:(hf + 1) * 512], in0=accs[t][:, :],
                     in1=X1[t][:, hf * 512:(hf + 1) * 512], op=ALU.add)
        if I + 1 < NB:
            for t in range(4):
                E.dma("sp", "d_x%d" % t, out=X1[t][:],
                      in_=V(x[tok0 + 512 + t * 128:tok0 + 512 + (t + 1) * 128, :], "dram_x"))
        rstd_batch([X2[t][:] for t in range(4)], [SS3[t][:] for t in range(4)], [RSTD3[t][:] for t in range(4)],
                   JUNK2[:], 1024.0)
        for t in range(4):
            ob = OUTB[t % 2]
            E.op("dve", "scalar_tensor_tensor", out=ob[:], in0=X2[t][:], scalar=RSTD3[t][:], in1=GFIN[:],
                 op0=ALU.mult, op1=ALU.mult)
            E.dma("pool", "d_out%d" % (t % 2), out=V(y[tok0 + t * 128:tok0 + (t + 1) * 128, :], "dram_y"), in_=ob[:])
        E.barrier()

    with nc.Block() as block:
        @block.sync
        def _(eng):
            E.replay("sp", eng)

        @block.tensor
        def _(eng):
            E.replay("pe", eng)

        @block.scalar
        def _(eng):
            E.replay("act", eng)

        @block.vector
        def _(eng):
            E.replay("dve", eng)

        @block.gpsimd
        def _(eng):
            E.replay("pool", eng)
            E.final_wait(eng, "pool")
    stack.close()
    return nc


def _rel_bucket(d):
    d = np.maximum(d, 0)
    large = 16 + (np.log(np.maximum(d, 1).astype(np.float32) / 16) / math.log(128 / 16) * 16).astype(np.int32)
    large = np.minimum(large, 31)
    return np.where(d < 16, d, large)


def host_consts(inp):
    f32 = np.float32
    c = {}
    c["gmix"] = np.ascontiguousarray(inp["norm_mix"].reshape(8, 128).T).astype(f32)
    c["gffn"] = np.ascontiguousarray(inp["norm_ffn"].reshape(8, 128).T).astype(f32)
    c["gfin"] = np.ascontiguousarray(np.broadcast_to(inp["norm_final"].reshape(1, 1024), (128, 1024))).astype(f32)
    mcw = inp["mlstm_conv_w"].reshape(4, 4, 128)
    c["mcw"] = np.ascontiguousarray(mcw.transpose(2, 1, 0).reshape(128, 16)).astype(f32)
    c["mcb"] = np.ascontiguousarray(inp["mlstm_conv_b"].reshape(4, 128).T).astype(f32)
    gb = np.concatenate([inp["i_bias"].reshape(4), inp["f_bias"].reshape(4)])
    c["gbias"] = np.ascontiguousarray(np.broadcast_to(gb.reshape(1, 8), (128, 8))).astype(f32)
    c["mnorm"] = np.ascontiguousarray(np.broadcast_to(inp["mlstm_norm"].reshape(1, 512), (128, 512))).astype(f32)
    c["gk"] = np.ascontiguousarray(np.broadcast_to(inp["idx_k_norm"].reshape(1, 64), (128, 64))).astype(f32)
    c["relb"] = np.ascontiguousarray(inp["rel_bias"]).astype(f32)
    c["b31"] = np.ascontiguousarray(inp["rel_bias"][31].reshape(8, 1)).astype(f32)
    oh = np.zeros((32, 384), f32)
    for k in range(384):
        d = k - 127
        if d >= 0:
            oh[int(_rel_bucket(np.array([d]))[0]), k] = 1.0
    c["oh"] = oh
    fw = inp["ffn_conv_w"].reshape(3, 44, 128)
    c["fcw"] = np.ascontiguousarray(fw.transpose(2, 1, 0).reshape(128, 132)).astype(f32)
    c["fcb"] = np.ascontiguousarray(inp["ffn_conv_b"].reshape(44, 128).T).astype(f32)
    c["ident"] = np.eye(128, dtype=f32)
    c["jrev"] = np.ascontiguousarray(np.eye(128, dtype=f32)[::-1])
    s = np.arange(128)[:, None]
    l = np.arange(128)[None, :]
    same = (s // 64) == (l // 64)
    tri = (same & (s <= l)).astype(f32)
    blk = same.astype(f32)
    blka = np.broadcast_to((s < 64), (128, 128)).astype(f32)
    blkb = np.broadcast_to((s >= 64), (128, 128)).astype(f32)
    c["tri4"] = np.ascontiguousarray(np.concatenate([tri, blk, blka, blkb], axis=1))
    q = np.arange(128)[:, None]
    kk = np.arange(128)[None, :]
    c["cmask"] = np.where(kk <= q, 0.0, NEG).astype(f32)
    c["hkc"] = np.ascontiguousarray(np.broadcast_to((0.5 ** (np.arange(NITER + 1) + 1)).reshape(1, NITER + 1), (128, NITER + 1))).astype(f32)
    return c


_NC_CACHE = {}


def kernel(**inputs):
    inp = {k: np.asarray(v) for k, v in inputs.items()}
    x = inp["x"]
    B, S, D = x.shape
    NB = S // 512
    topk = min(256, S // 4)
    key = (NB, topk)
    if key not in _NC_CACHE:
        _NC_CACHE[key] = build(NB, topk)
    nc = _NC_CACHE[key]
    c = host_consts(inp)
    shared = dict(c)
    shared["w_in"] = np.ascontiguousarray(inp["w_in"][0])
    shared["w_out"] = np.ascontiguousarray(inp["w_out"][0])
    shared["w_up"] = np.ascontiguousarray(inp["w_up"][0])
    shared["w_down"] = np.ascontiguousarray(inp["w_down"][0])
    in_maps = []
    for b in range(B):
        m = dict(shared)
        m["x"] = np.ascontiguousarray(x[b])
        in_maps.append(m)
    res = run_bass_kernel_spmd(nc, in_maps, core_ids=list(range(B)))
    out = np.stack([np.asarray(r["y"]) for r in res.results], axis=0).astype(np.float32)
    return out
```
